# Optimizing a Trainium2 kernel written in Bass

```python
import math
import jax, jax.numpy as jnp
from jax import lax
import numpy as np

D_MODEL = 1024
BATCH = 8
SEQ = 4096
DEPTH = 1

HEAD_DIM = 64
BLOCK = 128
A_PAIRS = ((128, 1), (512, 4), (2048, 16))
A_GROUPS = 3
A_HEADS_PER_GROUP = 4
A_WIDTH = A_GROUPS * A_HEADS_PER_GROUP * HEAD_DIM
A_OUT_WIDTH = A_HEADS_PER_GROUP * HEAD_DIM
B_Q_HEADS = 8
B_KV_HEADS = 2
B_WINDOW = 128
B_Q_WIDTH = B_Q_HEADS * HEAD_DIM
B_KV_WIDTH = B_KV_HEADS * HEAD_DIM
C_HEADS = 4
C_HEAD_DIM = 128
C_WIDTH = C_HEADS * C_HEAD_DIM
MEM_LEN = 256
N_GATES = 3
IN_WIDTH = 3 * A_WIDTH + B_Q_WIDTH + 2 * B_KV_WIDTH + C_WIDTH + N_GATES * D_MODEL
N_BUCKETS = 32
MAX_DISTANCE = 2048
N_BIAS_HEADS = A_GROUPS * A_HEADS_PER_GROUP + B_Q_HEADS
PEER_HEADS = 8
N_KEYS = 128
N_EXPERTS = N_KEYS * N_KEYS
PEER_TOPK = 16
PEER_DKEY = 128
PEER_CHUNK = 128
ALPHA = (2.0 * DEPTH) ** 0.25
BETA = (8.0 * DEPTH) ** -0.25
LN_EPS = 1e-5
NEG = -1e30

kernel_name = "hybrid_dilated_swa_sink_mem_peer_deepnorm"


def _layer_norm(x, g, b):
    xf = x.astype(jnp.float32)
    mu = jnp.mean(xf, axis=-1, keepdims=True)
    xc = xf - mu
    var = jnp.mean(xc * xc, axis=-1, keepdims=True)
    y = xc * lax.rsqrt(var + LN_EPS) * g.astype(jnp.float32) + b.astype(jnp.float32)
    return y.astype(x.dtype)


def _t5_bucket(dist):
    n = np.asarray(dist, dtype=np.int32)
    max_exact = N_BUCKETS // 2
    nf = np.maximum(n, 1).astype(np.float32)
    scale = np.float32(math.log(MAX_DISTANCE / max_exact))
    large = max_exact + (np.log(nf / np.float32(max_exact)) / scale
                         * np.float32(N_BUCKETS - max_exact)).astype(np.int32)
    large = np.minimum(large, N_BUCKETS - 1)
    return np.where(n < max_exact, n, large).astype(np.int32)


def _with_prev(t, axis):
    pad = [(0, 0)] * t.ndim
    pad[axis] = (1, 0)
    prev = lax.slice_in_dim(jnp.pad(t, pad), 0, t.shape[axis], axis=axis)
    return jnp.concatenate([prev, t], axis=axis + 1)


def _dilated_group(q, k, v, table, window, dilation):
    Bn, S, H, dh = q.shape
    r = dilation
    L = S // r
    nblk = -(-L // BLOCK)
    Lp = nblk * BLOCK
    W = window // r

    def strided(t):
        t = t.reshape(Bn, L, r, H, dh).transpose(0, 2, 1, 3, 4)
        t = jnp.pad(t, ((0, 0), (0, 0), (0, Lp - L), (0, 0), (0, 0)))
        return t.reshape(Bn, r, nblk, BLOCK, H, dh)

    qb = strided(q)
    kc = _with_prev(strided(k), axis=2)
    vc = _with_prev(strided(v), axis=2)
    logits = jnp.einsum('brnqhd,brnkhd->brnhqk', qb, kc).astype(jnp.float32) * (dh ** -0.5)

    i = np.arange(BLOCK)[:, None]
    j = np.arange(2 * BLOCK)[None, :]
    off = BLOCK + i - j
    bucket = _t5_bucket(np.clip(off, 0, W) * r)
    bias = jnp.take(table.astype(jnp.float32), bucket, axis=0).transpose(2, 0, 1)
    valid = ((off >= 0) & (off <= W))[None] & ~((np.arange(nblk)[:, None, None] == 0) & (j[None] < BLOCK))
    logits = jnp.where(valid[None, None, :, None], logits + bias[None, None, None], NEG)

    lse = jax.nn.logsumexp(logits, axis=-1)
    p = jnp.exp(logits - lse[..., None]).astype(v.dtype)
    o = jnp.einsum('brnhqk,brnkhd->brnqhd', p, vc)
    o = o.reshape(Bn, r, Lp, H, dh)[:, :, :L].transpose(0, 2, 1, 3, 4).reshape(Bn, S, H, dh)
    lse = lse.transpose(0, 1, 2, 4, 3).reshape(Bn, r, Lp, H)[:, :, :L].transpose(0, 2, 1, 3).reshape(Bn, S, H)
    return o, lse


def _swa_sinks(q, k, v, table, sinks):
    Bn, S, Hq, dh = q.shape
    Hkv = k.shape[2]
    G = Hq // Hkv
    nb = S // BLOCK
    qb = q.reshape(Bn, nb, BLOCK, Hkv, G, dh)
    kc = _with_prev(k.reshape(Bn, nb, BLOCK, Hkv, dh), axis=1)
    vc = _with_prev(v.reshape(Bn, nb, BLOCK, Hkv, dh), axis=1)
    logits = jnp.einsum('bnqkgd,bnjkd->bnkgqj', qb, kc).astype(jnp.float32) * (dh ** -0.5)

    i = np.arange(BLOCK)[:, None]
    j = np.arange(2 * BLOCK)[None, :]
    off = BLOCK + i - j
    bucket = _t5_bucket(np.clip(off, 0, B_WINDOW - 1))
    bias = jnp.take(table.astype(jnp.float32), bucket, axis=0).transpose(2, 0, 1)
    bias = bias.reshape(Hkv, G, BLOCK, 2 * BLOCK)
    valid = ((off >= 0) & (off < B_WINDOW))[None] & ~((np.arange(nb)[:, None, None] == 0) & (j[None] < BLOCK))
    logits = jnp.where(valid[None, :, None, None], logits + bias[None, None], NEG)

    s = sinks.astype(jnp.float32).reshape(Hkv, G)[None, None, :, :, None, None]
    m = jnp.maximum(jnp.max(logits, axis=-1, keepdims=True), s)
    e = jnp.exp(logits - m)
    denom = jnp.sum(e, axis=-1, keepdims=True) + jnp.exp(s - m)
    p = (e / denom).astype(v.dtype)
    o = jnp.einsum('bnkgqj,bnjkd->bnqkgd', p, vc)
    return o.reshape(Bn, S, Hq * dh)


def _memory_attn(q, k_mem, v_mem):
    Bn, S, H, dc = q.shape
    logits = jnp.einsum('bshd,bmhd->bhsm', q, k_mem).astype(jnp.float32) * (dc ** -0.5)
    p = jax.nn.softmax(logits, axis=-1).astype(v_mem.dtype)
    return jnp.einsum('bhsm,bmhd->bshd', p, v_mem).reshape(Bn, S, H * dc)


def _peer(x, w_query, sub_keys, u_table, v_table):
    Bn, S, D = x.shape
    q = (x @ w_query).reshape(Bn, S, PEER_HEADS, 2, PEER_DKEY // 2)
    scores = jnp.einsum('bshcd,hckd->bshck', q, sub_keys).astype(jnp.float32)
    sv, si = lax.top_k(scores, PEER_TOPK)
    cand = (sv[..., 0, :, None] + sv[..., 1, None, :]).reshape(Bn, S, PEER_HEADS, PEER_TOPK * PEER_TOPK)
    cidx = (si[..., 0, :, None] * N_KEYS + si[..., 1, None, :]).reshape(Bn, S, PEER_HEADS, PEER_TOPK * PEER_TOPK)
    top, pos = lax.top_k(cand, PEER_TOPK)
    eidx = jnp.take_along_axis(cidx, pos, axis=-1)
    gate = jax.nn.softmax(top, axis=-1).astype(x.dtype)

    T = Bn * S
    nchunk = T // PEER_CHUNK
    xs = x.reshape(nchunk, PEER_CHUNK, D)
    es = eidx.reshape(nchunk, PEER_CHUNK, PEER_HEADS * PEER_TOPK)
    gs = gate.reshape(nchunk, PEER_CHUNK, PEER_HEADS * PEER_TOPK)

    def expert_chunk(args):
        xc, ec, gc = args
        u = jnp.take(u_table, ec, axis=0)
        h = jax.nn.gelu(jnp.einsum('td,tkd->tk', xc, u), approximate=False)
        vv = jnp.take(v_table, ec, axis=0)
        return jnp.einsum('tk,tkd->td', gc * h, vv)

    return lax.map(expert_chunk, (xs, es, gs)).reshape(Bn, S, D)


def setup_inputs(seed: int = 0) -> dict:
    key = jax.random.key(seed)
    ks = jax.random.split(key, 20)
    f32 = jnp.float32
    D = D_MODEL
    nrm = lambda k, shape, s: (jax.random.normal(k, shape, f32) * s)
    return {
        "x": nrm(ks[0], (BATCH, SEQ, D), 1.0),
        "mem": nrm(ks[1], (BATCH, MEM_LEN, D), 1.0),
        "rel_bias": nrm(ks[2], (N_BUCKETS, N_BIAS_HEADS), 0.5),
        "w_in": nrm(ks[3], (DEPTH, D, IN_WIDTH), D ** -0.5),
        "b_gate": nrm(ks[4], (DEPTH, N_GATES * D), 0.02),
        "w_mem_kv": nrm(ks[5], (DEPTH, D, 2 * C_WIDTH), D ** -0.5),
        "sinks": nrm(ks[6], (DEPTH, B_Q_HEADS), 0.5),
        "w_branch_a": nrm(ks[7], (DEPTH, A_OUT_WIDTH, D), A_OUT_WIDTH ** -0.5),
        "w_branch_b": nrm(ks[8], (DEPTH, B_Q_WIDTH, D), B_Q_WIDTH ** -0.5),
        "w_branch_c": nrm(ks[9], (DEPTH, C_WIDTH, D), C_WIDTH ** -0.5),
        "w_out": nrm(ks[10], (DEPTH, D, D), BETA * D ** -0.5),
        "ln1_g": 1.0 + nrm(ks[11], (DEPTH, D), 0.05),
        "ln1_b": nrm(ks[12], (DEPTH, D), 0.02),
        "peer_w_query": nrm(ks[13], (DEPTH, D, PEER_HEADS * PEER_DKEY), D ** -0.5),
        "peer_sub_keys": nrm(ks[14], (DEPTH, PEER_HEADS, 2, N_KEYS, PEER_DKEY // 2), (PEER_DKEY // 2) ** -0.5),
        "peer_u": nrm(ks[15], (DEPTH, N_EXPERTS, D), D ** -0.5),
        "peer_v": nrm(ks[16], (DEPTH, N_EXPERTS, D), BETA * 0.5),
        "ln2_g": 1.0 + nrm(ks[17], (DEPTH, D), 0.05),
        "ln2_b": nrm(ks[18], (DEPTH, D), 0.02),
    }


def reference(x, mem, rel_bias, w_in, b_gate, w_mem_kv, sinks, w_branch_a, w_branch_b, w_branch_c,
              w_out, ln1_g, ln1_b, peer_w_query, peer_sub_keys, peer_u, peer_v, ln2_g, ln2_b):
    Bn, S, D = x.shape
    M = mem.shape[1]
    widths = [A_WIDTH, A_WIDTH, A_WIDTH, B_Q_WIDTH, B_KV_WIDTH, B_KV_WIDTH, C_WIDTH, N_GATES * D_MODEL]
    split_idx = [int(c) for c in np.cumsum(widths)[:-1]]
    n_a = A_GROUPS * A_HEADS_PER_GROUP
    for l in range(DEPTH):
        h = x @ w_in[l]
        aq, ak, av, bq, bk, bv, cq, gates = jnp.split(h, split_idx, axis=-1)
        aq = aq.reshape(Bn, S, A_GROUPS, A_HEADS_PER_GROUP, HEAD_DIM)
        ak = ak.reshape(Bn, S, A_GROUPS, A_HEADS_PER_GROUP, HEAD_DIM)
        av = av.reshape(Bn, S, A_GROUPS, A_HEADS_PER_GROUP, HEAD_DIM)

        outs, lses = [], []
        for g, (win, dil) in enumerate(A_PAIRS):
            tbl = rel_bias[:, g * A_HEADS_PER_GROUP:(g + 1) * A_HEADS_PER_GROUP]
            o_g, l_g = _dilated_group(aq[:, :, g], ak[:, :, g], av[:, :, g], tbl, win, dil)
            outs.append(o_g)
            lses.append(l_g)
        mix = jax.nn.softmax(jnp.stack(lses, axis=0), axis=0).astype(x.dtype)
        y_a = jnp.sum(mix[..., None] * jnp.stack(outs, axis=0), axis=0).reshape(Bn, S, A_OUT_WIDTH)

        y_b = _swa_sinks(bq.reshape(Bn, S, B_Q_HEADS, HEAD_DIM),
                         bk.reshape(Bn, S, B_KV_HEADS, HEAD_DIM),
                         bv.reshape(Bn, S, B_KV_HEADS, HEAD_DIM),
                         rel_bias[:, n_a:n_a + B_Q_HEADS], sinks[l])

        kv_m = (mem @ w_mem_kv[l]).reshape(Bn, M, 2, C_HEADS, C_HEAD_DIM)
        y_c = _memory_attn(cq.reshape(Bn, S, C_HEADS, C_HEAD_DIM), kv_m[:, :, 0], kv_m[:, :, 1])

        gt = jax.nn.sigmoid((gates.reshape(Bn, S, N_GATES, D) + b_gate[l].reshape(N_GATES, D)).astype(jnp.float32)).astype(x.dtype)
        merged = (gt[:, :, 0] * (y_a @ w_branch_a[l])
                  + gt[:, :, 1] * (y_b @ w_branch_b[l])
                  + gt[:, :, 2] * (y_c @ w_branch_c[l]))
        x = _layer_norm(ALPHA * x + merged @ w_out[l], ln1_g[l], ln1_b[l])

        y_p = _peer(x, peer_w_query[l], peer_sub_keys[l], peer_u[l], peer_v[l])
        x = _layer_norm(ALPHA * x + y_p, ln2_g[l], ln2_b[l])
    return x
```

```python
import numpy as np
import concourse.bass as bass
import concourse.mybir as mybir
from concourse.bass_utils import run_bass_kernel_spmd
from contextlib import ExitStack

F32 = mybir.dt.float32
BF16 = mybir.dt.bfloat16
U32 = mybir.dt.uint32
AF = mybir.ActivationFunctionType
ALU = mybir.AluOpType
AX = mybir.AxisListType

S = 4096
D = 1024
NCORES = 8
ALPHA = 2.0 ** 0.25
LN_EPS = 1e-5
NEG = -30000.0
N_EXP = 16384
NG = 12
ND = 4
LAG = 2
NP = 4
FRONT_SPAN = 110


class Buf:
    __slots__ = ("w", "r")

    def __init__(self):
        self.w = None
        self.r = {}


class Q:
    def __init__(self, nc, eng, st, name, is_pe=False):
        self.nc = nc
        self.eng = eng
        self.sem = st.enter_context(nc.semaphore("q_" + name))
        self.n = 0
        self.seen = {}
        self.is_pe = is_pe

    def wait(self, ev):
        if ev is None:
            return
        sem, val = ev
        if sem is self.sem and self.is_pe:
            return
        k = id(sem)
        if self.seen.get(k, -1) >= val:
            return
        self.eng.wait_ge(sem, val)
        self.seen[k] = val

    def deps(self, reads, writes, extra, skip_sem=None):
        for b in reads:
            if b.w is not None and b.w[0] is not skip_sem:
                self.wait(b.w)
        for b in writes:
            if b.w is not None and b.w[0] is not skip_sem:
                self.wait(b.w)
            for ev in b.r.values():
                self.wait(ev)
        for ev in extra:
            self.wait(ev)

    @staticmethod
    def mark(ev, reads, writes):
        for b in reads:
            k = id(ev[0])
            old = b.r.get(k)
            if old is None or old[1] < ev[1]:
                b.r[k] = ev
        for b in writes:
            b.w = ev
            b.r = {}

    def do(self, fn, reads=(), writes=(), extra=()):
        self.deps(reads, writes, extra)
        ins = fn()
        self.n += 1
        ins.then_inc(self.sem, 1)
        ev = (self.sem, self.n)
        self.mark(ev, reads, writes)
        return ev


class DmaChan:
    def __init__(self, nc, st, name):
        self.sem = st.enter_context(nc.semaphore("c_" + name))
        self.n = 0


def dma(q, chan, fn, reads=(), writes=(), extra=()):
    q.deps(reads, writes, extra, skip_sem=chan.sem)
    ins = fn()
    chan.n += 16
    ins.then_inc(chan.sem, 16)
    ev = (chan.sem, chan.n)
    Q.mark(ev, reads, writes)
    return ev


class SpQ(Q):
    def __init__(self, nc, eng):
        self.nc = nc
        self.eng = eng
        self.sem = None
        self.n = 0
        self.seen = {}
        self.is_pe = False


def _t5_bucket(dist):
    n = np.asarray(dist, dtype=np.int32)
    max_exact = 16
    nf = np.maximum(n, 1).astype(np.float32)
    scale = np.float32(np.log(2048 / max_exact))
    large = max_exact + (np.log(nf / np.float32(max_exact)) / scale * np.float32(32 - max_exact)).astype(np.int32)
    large = np.minimum(large, 31)
    return np.where(n < max_exact, n, large).astype(np.int32)


def _bias_tables():
    i = np.arange(128)[None, :]
    j = np.arange(128)[:, None]
    off_prev = 128 + i - j
    off_cur = i - j
    pairs = []
    for g, r in enumerate((1, 4, 16)):
        for m in range(2):
            pairs.append(("A", r, g * 4 + 2 * m))
    for m in range(4):
        pairs.append(("B", 1, 12 + 2 * m))
    bucket = np.zeros((10, 2, 128, 128), np.int32)
    mask = np.zeros((10, 2, 128, 128), np.float32)
    heads = []
    for p, (kind, r, h0) in enumerate(pairs):
        W = 128 if kind == "A" else 127
        for kb, off in enumerate((off_prev, off_cur)):
            bucket[p, kb] = _t5_bucket(np.clip(off, 0, W) * r)
            valid = (off >= 0) & (off <= W)
            mask[p, kb] = np.where(valid, 0.0, NEG)
        heads.append(h0)
    return pairs, bucket, mask, heads


def build_nc(debug=False, upto="all", ntiles=None, njobs=None, do_c=True, stage=99, jobsel=None):
    nc = bass.Bass("TRN2", target_bir_lowering=False)
    dt_in = lambda n, s, t=F32: nc.dram_tensor(n, s, t, kind="ExternalInput").ap()
    xT_d = dt_in("xT", [D, S])
    x_d = dt_in("x", [S, D])
    memT_d = dt_in("memT", [D, 256])
    w_in_d = dt_in("w_in", [D, 6656])
    w_kv_d = dt_in("w_mem_kv", [D, 1024])
    w_a_d = dt_in("w_a", [256, D])
    w_b_d = dt_in("w_b", [512, D])
    w_c_d = dt_in("w_c", [512, D])
    w_out_d = dt_in("w_out", [D, D])
    w_q_d = dt_in("w_q", [D, D])
    skT_d = dt_in("skT", [128, 8 * 128])
    pu_d = dt_in("peer_u", [N_EXP, D])
    pv_d = dt_in("peer_v", [N_EXP, D])
    biasT_d = dt_in("biasT", [128, 10 * 512])
    maskT_d = dt_in("maskT", [128, 10 * 512])
    bgate_d = dt_in("bgate", [128, 24])
    sinks_d = dt_in("sinksP", [128, 4])
    lnp_d = dt_in("lnp", [128, 4 * D])
    ident_d = dt_in("ident", [128, 128])
    iota_d = dt_in("iota16", [128, 16])
    out_d = nc.dram_tensor("out", [S, D], F32, kind="ExternalOutput").ap()
    skind = "ExternalOutput" if debug else "Internal"
    yT_s = nc.dram_tensor("yT_scr", [1280, S], BF16, kind=skind).ap()
    mT_s = nc.dram_tensor("mT_scr", [D, S], BF16, kind=skind).ap()
    uv_s = nc.dram_tensor("uv_scr", [N_EXP, 2 * D], BF16, kind="Internal").ap()
    if debug:
        x1_dbg = nc.dram_tensor("x1_dbg", [S, D], F32, kind="ExternalOutput").ap()
        yp_dbg = nc.dram_tensor("yp_dbg", [S, D], F32, kind="ExternalOutput").ap()

    with ExitStack() as gst:
        pe = Q(nc, nc.tensor, gst, "pe", is_pe=True)
        act = Q(nc, nc.scalar, gst, "act")
        dve = Q(nc, nc.vector, gst, "dve")
        pool = Q(nc, nc.gpsimd, gst, "pool")
        sp = SpQ(nc, nc.sync)
        queues = [pe, act, dve, pool, sp]
        chans = []

        def chan(name):
            c = DmaChan(nc, gst, name)
            chans.append(c)
            return c

        def barrier(skip=()):
            for q in queues:
                for o in (pe, act, dve, pool):
                    if o is not q and o.n > 0:
                        q.wait((o.sem, o.n))
                for c in chans:
                    if c.n > 0 and c not in skip:
                        q.wait((c.sem, c.n))

        psall = gst.enter_context(nc.psum_tensor("psall", [128, 4096], F32))

        def bank(i, n=1):
            return psall[:, i * 512:(i + n) * 512]

        ident = gst.enter_context(nc.sbuf_tensor("s_ident", [128, 128], F32))
        ones_b = gst.enter_context(nc.sbuf_tensor("s_ones_b", [128, 128], BF16))
        c_const = chan("const")
        b_const = Buf()
        dma(sp, c_const, lambda: nc.sync.dma_start(out=ident[:], in_=ident_d[:, :]), writes=[b_const])
        dve.do(lambda: nc.vector.memset(ones_b[:], 1.0), writes=[b_const])

        w_in_v = w_in_d.rearrange("(kc p) n -> p kc n", p=128)

        c_cv = chan("cv")
        cv_list = [(tab, c0, c) for (tab, c0) in ((pu_d, 0), (pv_d, D)) for c in range(16)]

        def convert_some(k):
            for _ in range(k):
                if not cv_list:
                    return
                tab, c0, c = cv_list.pop(0)
                src = tab[c * 1024:(c + 1) * 1024, :].rearrange("(p r) d -> p r d", p=128)
                dst = uv_s[c * 1024:(c + 1) * 1024, c0:c0 + D].rearrange("(p r) d -> p r d", p=128)
                dma(pool, c_cv, lambda: nc.gpsimd.dma_start(out=dst, in_=src))

        with ExitStack() as st:
            sb = lambda n, s, t: st.enter_context(nc.sbuf_tensor("s_" + n, s, t))
            XT = sb("XT", [128, 8, S], BF16)
            b_XTc = [Buf() for _ in range(4)]
            c_xtc = [chan("xc%d" % i) for i in range(4)]
            st01 = st.enter_context(ExitStack())
            sb1 = lambda n, s, t: st01.enter_context(nc.sbuf_tensor("s_" + n, s, t))
            BT = sb1("BT", [128, 10, 512], BF16)
            es = sb1("es", [128, 4], F32)
            KmT = sb1("KmT", [128, 4, 256], BF16)
            Vm = sb1("Vm", [128, 2, 512], BF16)
            b_BT, b_es, b_KmT, b_Vm = Buf(), Buf(), Buf(), Buf()
            b_ps = [Buf() for _ in range(8)]
            c_misc = chan("misc")

            Wt = [sb1("Wt%d" % i, [128, 8, 384], BF16) for i in range(2)]
            b_Wt = [Buf(), Buf()]
            c_wt, c_ys = [chan("wt0"), chan("wt1")], [chan("ys0"), chan("ys1")]
            jobs = []
            for m in range(2):
                for g, r in enumerate((1, 4, 16)):
                    c = g * 256 + 2 * m * 64
                    jobs.append(dict(q=c, k=(768 + c, 768 + c + 64), v=(1536 + c, 1536 + c + 64), r=r, bp=g * 2 + m,
                                     first=(g == 0), last=(g == 2), sink=None, row=m * 128))
            for kv in range(2):
                for m in range(2):
                    hq = kv * 4 + 2 * m
                    jobs.append(dict(q=2304 + hq * 64, k=(2816 + kv * 64,) * 2, v=(2944 + kv * 64,) * 2, r=1, bp=6 + kv * 2 + m,
                                     first=True, last=True, sink=kv * 2 + m, row=256 + (kv * 2 + m) * 128))

            def load_w(ji):
                jb = jobs[ji]
                s = ji % 2
                dma(pool, c_wt[s], lambda: nc.gpsimd.dma_start(out=Wt[s][:, :, 0:128], in_=w_in_v[:, :, jb["q"]:jb["q"] + 128]),
                    writes=[b_Wt[s]])
                for t, key in ((0, "k"), (1, "v")):
                    for hh in range(2):
                        c0 = jb[key][hh]
                        o0 = 128 + t * 128 + hh * 64
                        dma(pool, c_wt[s], lambda c0=c0, o0=o0: nc.gpsimd.dma_start(out=Wt[s][:, :, o0:o0 + 64], in_=w_in_v[:, :, c0:c0 + 64]),
                            writes=[b_Wt[s]])


            xT_v = xT_d.rearrange("(kc p) t -> p kc t", p=128)
            with ExitStack() as st0:
                sb0 = lambda n, s, t: st0.enter_context(nc.sbuf_tensor("s_" + n, s, t))
                bt_f = sb0("bt_f", [128, 5120], F32)
                mk_f = sb0("mk_f", [128, 5120], F32)
                sk_in = sb0("sk_in", [128, 4], F32)
                memTb = sb0("memTb", [128, 8, 256], BF16)
                Wkv = sb0("Wkv", [128, 8, 1024], BF16)
                b_btf = b_mkf = b_skin = b_memTb = b_Wkv = Buf()
                dma(sp, c_misc, lambda: nc.sync.dma_start(out=bt_f[:], in_=biasT_d[:, :]), writes=[b_btf])
                dma(sp, c_misc, lambda: nc.sync.dma_start(out=mk_f[:], in_=maskT_d[:, :]), writes=[b_mkf])
                dma(sp, c_misc, lambda: nc.sync.dma_start(out=sk_in[:], in_=sinks_d[:, :]), writes=[b_skin])
                dma(pool, c_misc, lambda: nc.gpsimd.dma_start(out=memTb[:], in_=memT_d.rearrange("(kc p) m -> p kc m", p=128)),
                    writes=[b_memTb])
                dma(pool, c_misc, lambda: nc.gpsimd.dma_start(out=Wkv[:], in_=w_kv_d.rearrange("(kc p) n -> p kc n", p=128)),
                    writes=[b_Wkv])
                if upto != "p0" and njobs != 0:
                    load_w(0)
                for i in range(4):
                    dma(pool, c_xtc[i], lambda i=i: nc.gpsimd.dma_start(out=XT[:, :, i * 1024:(i + 1) * 1024],
                                                                      in_=xT_v[:, :, i * 1024:(i + 1) * 1024]), writes=[b_XTc[i]])
                dve.do(lambda: nc.vector.tensor_tensor(out=BT[:].rearrange("p a b -> p (a b)"), in0=bt_f[:], in1=mk_f[:], op=ALU.add),
                       reads=[b_btf, b_mkf], writes=[b_BT])
                act.do(lambda: nc.scalar.activation(out=es[:], in_=sk_in[:], func=AF.Exp), reads=[b_skin], writes=[b_es])
                for h in range(4):
                    pb = h % 2
                    for kc in range(8):
                        pe.do(lambda h=h, kc=kc, pb=pb: nc.tensor.matmul(bank(pb)[:, 0:256], lhsT=Wkv[:, kc, h * 128:(h + 1) * 128],
                                                                          rhs=memTb[:, kc, :], start=(kc == 0), stop=(kc == 7)),
                              reads=[b_Wkv, b_memTb], writes=[b_ps[pb]])
                    act.do(lambda h=h, pb=pb: nc.scalar.activation(out=KmT[:, h, :], in_=bank(pb)[:, 0:256], func=AF.Identity),
                           reads=[b_ps[pb]], writes=[b_KmT])
                for kb in range(2):
                    for kc in range(8):
                        pe.do(lambda kb=kb, kc=kc: nc.tensor.matmul(bank(kb), lhsT=memTb[:, kc, kb * 128:(kb + 1) * 128],
                                                                     rhs=Wkv[:, kc, 512:1024], start=(kc == 0), stop=(kc == 7)),
                              reads=[b_Wkv, b_memTb], writes=[b_ps[kb]])
                    act.do(lambda kb=kb: nc.scalar.activation(out=Vm[:, kb, :], in_=bank(kb), func=AF.Identity),
                           reads=[b_ps[kb]], writes=[b_Vm])
                barrier(skip=c_xtc + c_wt)

            QT = sb1("QT", [128, S], BF16)
            KT = sb1("KT", [128, S], BF16)
            Vt = sb1("Vt", [128, 32, 128], BF16)
            Acc = sb1("Acc", [128, 2, S], F32)
            Yt = [sb1("Yt%d" % i, [128, S], BF16) for i in range(2)]
            Pf = [sb1("Pf%d" % i, [128, 512], F32) for i in range(2)]
            PT = [sb1("PT%d" % i, [128, 512], BF16) for i in range(2)]
            b_QT, b_KT, b_Vt, b_Acc = Buf(), Buf(), Buf(), Buf()
            b_Yt = [Buf(), Buf()]
            b_Pf = [Buf(), Buf()]
            b_PT = [Buf(), Buf()]
            b_ps = [Buf() for _ in range(8)]
            b_nz = [[Buf(), Buf()] for _ in range(4)]
            b_S = [Buf(), Buf()]

            if njobs is not None:
                jobs = jobs[:njobs]
            if jobsel is not None:
                jobs = [jobs[i] for i in jobsel]
            if upto == "p0":
                jobs = []
            ycount = 0
            for ji, jb in enumerate(jobs):
                s = ji % 2
                if ji + 1 < len(jobs):
                    load_w(ji + 1)
                convert_some(4)
                r = jb["r"]
                nb = 32 // r
                for which, dst, bdst in ((0, QT, b_QT), (1, KT, b_KT)):
                    for tc in range(8):
                        pb = tc % 2
                        for kc in range(8):
                            pe.do(lambda kc=kc, tc=tc, pb=pb, which=which: nc.tensor.matmul(
                                bank(pb), lhsT=Wt[s][:, kc, which * 128:(which + 1) * 128], rhs=XT[:, kc, tc * 512:(tc + 1) * 512],
                                start=(kc == 0), stop=(kc == 7)), reads=[b_Wt[s], b_XTc[tc // 2]], writes=[b_ps[pb]])
                        act.do(lambda tc=tc, pb=pb, dst=dst: nc.scalar.activation(out=dst[:, tc * 512:(tc + 1) * 512], in_=bank(pb), func=AF.Identity),
                               reads=[b_ps[pb]], writes=[bdst])

                def tokset(blk):
                    rho, n = blk // nb, blk % nb
                    st_ = r * 128 * n + rho
                    return st_, n

                for bg in (range(8) if stage >= 2 else ()):
                    pb = bg % 2
                    for j in range(4):
                        blk = bg * 4 + j
                        st_, n = tokset(blk)
                        for kc in range(8):
                            pe.do(lambda kc=kc, j=j, pb=pb, st_=st_: nc.tensor.matmul(
                                bank(pb)[:, j * 128:(j + 1) * 128], lhsT=XT[:, kc, st_:st_ + 127 * r + 1:r], rhs=Wt[s][:, kc, 256:384],
                                start=(kc == 0), stop=(kc == 7)), reads=[b_Wt[s]] + b_XTc, writes=[b_ps[pb]])
                    act.do(lambda bg=bg, pb=pb: nc.scalar.activation(out=Vt[:, bg * 4:(bg + 1) * 4, :].rearrange("p a b -> p (a b)"),
                                                                   in_=bank(pb), func=AF.Identity), reads=[b_ps[pb]], writes=[b_Vt])
                bp = jb["bp"]

                def qk(blk):
                    st_, n = tokset(blk)
                    pi = blk % 2
                    qs = slice(st_, st_ + 127 * r + 1, r)
                    ks_c = qs
                    ks_p = slice(st_ - 128 * r, st_ - r + 1, r)
                    sb0 = 2 if pi == 0 else 0
                    bS = [b_ps[sb0], b_ps[sb0 + 1]]
                    for hh in range(2):
                        ps_ = slice(hh * 64, (hh + 1) * 64)
                        if n > 0:
                            pe.do(lambda: nc.tensor.matmul(
                                bank(sb0 + hh)[:, 0:128], lhsT=KT[ps_, ks_p], rhs=QT[ps_, qs], start=True, stop=True),
                                reads=[b_KT, b_QT], writes=bS)
                        pe.do(lambda: nc.tensor.matmul(
                            bank(sb0 + hh)[:, 128:256], lhsT=KT[ps_, ks_c], rhs=QT[ps_, qs], start=True, stop=True),
                            reads=[b_KT, b_QT], writes=bS)

                def softmax(blk):
                    st_, n = tokset(blk)
                    pi = blk % 2
                    sb0 = 2 if pi == 0 else 0
                    bS = [b_ps[sb0], b_ps[sb0 + 1]]
                    Sv = bank(sb0, 2).rearrange("p (b c) -> p b c", c=512)[:, :, 0:256]
                    h3 = lambda ap: ap.rearrange("p (b c) -> p b c", c=256)
                    if n > 0:
                        dve.do(lambda: nc.vector.scalar_tensor_tensor(
                            out=h3(Pf[pi][:]), in0=Sv, scalar=0.125, in1=h3(BT[:, bp, :]), op0=ALU.mult, op1=ALU.add),
                            reads=bS + [b_BT], writes=[b_Pf[pi]])
                        act.do(lambda: nc.scalar.activation(out=PT[pi][:], in_=Pf[pi][:], func=AF.Exp),
                               reads=[b_Pf[pi]], writes=[b_PT[pi]])
                    else:
                        dve.do(lambda: nc.vector.scalar_tensor_tensor(
                            out=h3(Pf[pi][:])[:, :, 128:256], in0=Sv[:, :, 128:256], scalar=0.125, in1=h3(BT[:, bp, :])[:, :, 128:256],
                            op0=ALU.mult, op1=ALU.add), reads=bS + [b_BT], writes=[b_Pf[pi]])
                        act.do(lambda: nc.scalar.activation(out=h3(PT[pi][:])[:, :, 128:256], in_=h3(Pf[pi][:])[:, :, 128:256], func=AF.Exp),
                               reads=[b_Pf[pi]], writes=[b_PT[pi]])

                def pv(blk):
                    st_, n = tokset(blk)
                    pi = blk % 2
                    nzb = blk % 4
                    half = 0
                    co = 0
                    for hh in range(2):
                        ps_ = slice(hh * 64, (hh + 1) * 64)
                        for (oc, lhs_fn) in ((co, lambda b_: Vt[:, b_, ps_]), (co + 128, lambda b_: ones_b[:, 0:64])):
                            if n > 0:
                                pe.do(lambda: nc.tensor.matmul(
                                    bank(4 + nzb)[ps_, oc:oc + 128], lhsT=lhs_fn(blk - 1), rhs=PT[pi][:, (2 * hh) * 128:(2 * hh + 1) * 128],
                                    start=True, stop=False), reads=[b_Vt, b_PT[pi]], writes=[b_nz[nzb][half]])
                            pe.do(lambda: nc.tensor.matmul(
                                bank(4 + nzb)[ps_, oc:oc + 128], lhsT=lhs_fn(blk), rhs=PT[pi][:, (2 * hh + 1) * 128:(2 * hh + 2) * 128],
                                start=(n == 0), stop=True), reads=[b_Vt, b_PT[pi]], writes=[b_nz[nzb][half]])

                def evac(blk):
                    st_, n = tokset(blk)
                    nzb = blk % 4
                    half = 0
                    co = 0
                    accv = Acc[:, :, st_:st_ + 127 * r + 1:r]
                    nzv = bank(4 + nzb)[:, co:co + 256].rearrange("p (a q) -> p a q", q=128)
                    if jb["first"]:
                        dve.do(lambda: nc.vector.tensor_copy(out=accv, in_=nzv), reads=[b_nz[nzb][half]], writes=[b_Acc])
                    else:
                        dve.do(lambda: nc.vector.tensor_tensor(out=accv, in0=nzv, in1=accv, op=ALU.add),
                               reads=[b_nz[nzb][half]], writes=[b_Acc])

                qk(0)
                qk(1)
                softmax(0)
                for blk in range(32):
                    if blk + 2 < 32:
                        qk(blk + 2)
                    if blk + 1 < 32:
                        softmax(blk + 1)
                    pv(blk)
                    evac(blk)
                if jb["last"]:
                    ys = ycount % 2
                    ycount += 1
                    if jb["sink"] is not None:
                        sk = jb["sink"]
                        dve.do(lambda sk=sk: nc.vector.tensor_scalar(out=Acc[:, 1, :], in0=Acc[:, 1, :], scalar1=es[:, sk:sk + 1], scalar2=None, op0=ALU.add),
                               reads=[b_es], writes=[b_Acc])
                    dve.do(lambda: nc.vector.reciprocal(out=Acc[:, 1, :], in_=Acc[:, 1, :]), reads=[], writes=[b_Acc])
                    dve.do(lambda ys=ys: nc.vector.tensor_tensor(out=Yt[ys][:], in0=Acc[:, 0, :], in1=Acc[:, 1, :], op=ALU.mult),
                           reads=[b_Acc], writes=[b_Yt[ys]])
                    row = jb["row"]
                    dma(sp, c_ys[ys], lambda ys=ys, row=row: nc.sync.dma_start(out=yT_s[row:row + 128, :], in_=Yt[ys][:]), reads=[b_Yt[ys]])

            convert_some(64)
            barrier()
            for h in (range(4) if (do_c and upto != "p0") else ()):
                s = h % 2
                c0 = 3072 + h * 128
                dma(pool, c_wt[s], lambda s=s, c0=c0: nc.gpsimd.dma_start(out=Wt[s][:, :, 0:128], in_=w_in_v[:, :, c0:c0 + 128]), writes=[b_Wt[s]])
                for tc in range(8):
                    pb = tc % 2
                    for kc in range(8):
                        pe.do(lambda kc=kc, tc=tc, pb=pb, s=s: nc.tensor.matmul(
                            bank(pb), lhsT=Wt[s][:, kc, 0:128], rhs=XT[:, kc, tc * 512:(tc + 1) * 512], start=(kc == 0), stop=(kc == 7)),
                            reads=[b_Wt[s], b_XTc[tc // 2]], writes=[b_ps[pb]])
                    act.do(lambda tc=tc, pb=pb: nc.scalar.activation(out=QT[:, tc * 512:(tc + 1) * 512], in_=bank(pb), func=AF.Identity),
                           reads=[b_ps[pb]], writes=[b_QT])
                ys = ycount % 2
                ycount += 1
                for tc in range(8):
                    for kb in range(2):
                        sbk = 2 + kb
                        pe.do(lambda kb=kb, tc=tc, sbk=sbk, h=h: nc.tensor.matmul(
                            bank(sbk), lhsT=KmT[:, h, kb * 128:(kb + 1) * 128], rhs=QT[:, tc * 512:(tc + 1) * 512], start=True, stop=True),
                            reads=[b_KmT, b_QT], writes=[b_ps[sbk]])
                        act.do(lambda kb=kb, sbk=sbk: nc.scalar.activation(out=PT[kb][:], in_=bank(sbk), func=AF.Exp, scale=float(128 ** -0.5)),
                               reads=[b_ps[sbk]], writes=[b_PT[kb]])
                    nb_ = 4 + (tc % 2) * 2
                    for kb in range(2):
                        pe.do(lambda kb=kb, nb_=nb_, h=h: nc.tensor.matmul(bank(nb_), lhsT=Vm[:, kb, h * 128:(h + 1) * 128], rhs=PT[kb][:],
                                                                        start=(kb == 0), stop=(kb == 1)), reads=[b_Vm, b_PT[kb]], writes=[b_ps[nb_]])
                    for kb in range(2):
                        pe.do(lambda kb=kb, nb_=nb_: nc.tensor.matmul(bank(nb_ + 1), lhsT=ones_b[:, :], rhs=PT[kb][:],
                                                                    start=(kb == 0), stop=(kb == 1)), reads=[b_PT[kb]], writes=[b_ps[nb_ + 1]])
                    pi = tc % 2
                    dve.do(lambda pi=pi, nb_=nb_: nc.vector.reciprocal(out=Pf[pi][:], in_=bank(nb_ + 1)), reads=[b_ps[nb_ + 1]], writes=[b_Pf[pi]])
                    dve.do(lambda pi=pi, nb_=nb_, tc=tc, ys=ys: nc.vector.tensor_tensor(out=Yt[ys][:, tc * 512:(tc + 1) * 512], in0=bank(nb_), in1=Pf[pi][:], op=ALU.mult),
                           reads=[b_ps[nb_], b_Pf[pi]], writes=[b_Yt[ys]])
                row = 768 + h * 128
                dma(sp, c_ys[ys], lambda ys=ys, row=row: nc.sync.dma_start(out=yT_s[row:row + 128, :], in_=Yt[ys][:]), reads=[b_Yt[ys]])
            barrier()
            st01.close()

            with ExitStack() as st2:
                sb2 = lambda n, s_, t: st2.enter_context(nc.sbuf_tensor("s_" + n, s_, t))
                Wg = sb2("Wg", [128, 8, 3072], BF16)
                Wbr = sb2("Wbr", [128, 10, D], BF16)
                bg = sb2("bg", [128, 24], F32)
                Ych = [sb2("Ych%d" % i, [128, 10, 512], BF16) for i in range(2)]
                mch = [sb2("mch%d" % i, [128, 8, 512], BF16) for i in range(2)]
                gt = [sb2("gt%d" % i, [128, 512], F32) for i in range(3)]
                tt_ = [sb2("tt%d" % i, [128, 512], F32) for i in range(3)]
                b_Wg = b_Wbr = b_bg = Buf()
                b_Ych, b_mch = [Buf(), Buf()], [Buf(), Buf()]
                b_gt = [Buf() for _ in range(3)]
                b_tt = [Buf() for _ in range(3)]
                b_ps = [Buf() for _ in range(8)]
                c_w2, c_ych, c_mst = chan("w2"), [chan("ych0"), chan("ych1")], [chan("mst0"), chan("mst1")]
                for br in range(3):
                    dma(pool, c_w2, lambda br=br: nc.gpsimd.dma_start(out=Wg[:, :, br * 1024:(br + 1) * 1024],
                                                                      in_=w_in_v[:, :, 3584 + br * 1024:3584 + (br + 1) * 1024]), writes=[b_Wg])
                dma(pool, c_w2, lambda: nc.gpsimd.dma_start(out=Wbr[:, 0:2, :], in_=w_a_d.rearrange("(kc p) n -> p kc n", p=128)), writes=[b_Wbr])
                dma(pool, c_w2, lambda: nc.gpsimd.dma_start(out=Wbr[:, 2:6, :], in_=w_b_d.rearrange("(kc p) n -> p kc n", p=128)), writes=[b_Wbr])
                dma(pool, c_w2, lambda: nc.gpsimd.dma_start(out=Wbr[:, 6:10, :], in_=w_c_d.rearrange("(kc p) n -> p kc n", p=128)), writes=[b_Wbr])
                dma(sp, c_w2, lambda: nc.sync.dma_start(out=bg[:], in_=bgate_d[:, :]), writes=[b_bg])
                yT_v = yT_s.rearrange("(c p) t -> p c t", p=128)
                mT_v = mT_s.rearrange("(c p) t -> p c t", p=128)
                brk = ((0, 2), (2, 6), (6, 10))

                def load_y(tc):
                    dma(sp, c_ych[tc % 2], lambda: nc.sync.dma_start(out=Ych[tc % 2][:], in_=yT_v[:, :, tc * 512:(tc + 1) * 512]),
                        writes=[b_Ych[tc % 2]])

                if upto not in ("p0", "p1"):
                    load_y(0)
                for tc in (range(8) if upto not in ("p0", "p1") else ()):
                    s = tc % 2
                    if tc + 1 < 8:
                        load_y(tc + 1)
                    tsl = slice(tc * 512, (tc + 1) * 512)
                    for f in range(8):
                        for br in range(3):
                            gb = br
                            for kc in range(8):
                                pe.do(lambda kc=kc, br=br, f=f, gb=gb: nc.tensor.matmul(
                                    bank(gb), lhsT=Wg[:, kc, br * 1024 + f * 128:br * 1024 + (f + 1) * 128], rhs=XT[:, kc, tsl],
                                    start=(kc == 0), stop=(kc == 7)), reads=[b_Wg, b_XTc[tc // 2]], writes=[b_ps[gb]])
                            act.do(lambda br=br, f=f, gb=gb: nc.scalar.activation(out=gt[br][:], in_=bank(gb), func=AF.Sigmoid,
                                                                                   bias=bg[:, br * 8 + f:br * 8 + f + 1]),
                                   reads=[b_ps[gb], b_bg], writes=[b_gt[br]])
                            k0, k1 = brk[br]
                            for kc in range(k0, k1):
                                pe.do(lambda kc=kc, br=br, f=f, k0=k0, k1=k1: nc.tensor.matmul(
                                    bank(3 + br), lhsT=Wbr[:, kc, f * 128:(f + 1) * 128], rhs=Ych[s][:, kc, :],
                                    start=(kc == k0), stop=(kc == k1 - 1)), reads=[b_Wbr, b_Ych[s]], writes=[b_ps[3 + br]])
                            dve.do(lambda br=br: nc.vector.tensor_tensor(out=tt_[br][:], in0=bank(3 + br), in1=gt[br][:], op=ALU.mult),
                                   reads=[b_ps[3 + br], b_gt[br]], writes=[b_tt[br]])
                        pool.do(lambda: nc.gpsimd.tensor_tensor(out=tt_[0][:], in0=tt_[0][:], in1=tt_[1][:], op=ALU.add),
                                reads=[b_tt[1]], writes=[b_tt[0]])
                        pool.do(lambda f=f: nc.gpsimd.tensor_tensor(out=mch[s][:, f, :], in0=tt_[0][:], in1=tt_[2][:], op=ALU.add),
                                reads=[b_tt[0], b_tt[2]], writes=[b_mch[s]])
                    dma(sp, c_mst[s], lambda s=s: nc.sync.dma_start(out=mT_v[:, :, tsl], in_=mch[s][:]), reads=[b_mch[s]])
                barrier()
        barrier()

        with ExitStack() as st:
            sb = lambda n, s_, t: st.enter_context(nc.sbuf_tensor("s_" + n, s_, t))
            Wout = sb("Wout", [128, 8, D], BF16)
            Wq = sb("Wq", [128, 8, D], BF16)
            skT = sb("skT", [128, 8, 128], F32)
            lnp = sb("lnp", [128, 4, D], F32)
            iota16 = sb("iota16", [128, 16], F32)
            mTc = [sb("mTc%d" % i, [128, 8, 512], BF16) for i in range(2)]
            xt = [sb("xt%d" % i, [128, D], F32) for i in range(2)]
            r1 = sb("r1", [128, D], F32)
            x1 = [sb("x1_%d" % i, [128, D], F32) for i in range(2)]
            x1T = sb("x1T", [128, 8, 128], BF16)
            qT = sb("qT", [128, 8, 128], F32)
            stats = sb("stats", [128, 2, 6], F32)
            mv = sb("mv", [128, 2], F32)
            rstd = sb("rstd", [128, 1], F32)
            nmr = sb("nmr", [128, 1], F32)
            sv = sb("sv", [128, 16, 16], F32)
            si_u = sb("si_u", [128, 16, 16], U32)
            si_f = sb("si_f", [128, 16, 16], F32)
            work = sb("work", [128, 128], F32)
            cand = sb("cand", [128, 8, 256], F32)
            work2 = sb("work2", [128, 256], F32)
            top = sb("top", [128, 8, 16], F32)
            pos_u = sb("pos_u", [128, 8, 16], U32)
            ab_u = sb("ab_u", [128, 2, 128], U32)
            ab_f = sb("ab_f", [128, 2, 128], F32)
            oh = sb("oh", [128, 8, 16, 16], BF16)
            ab_b = sb("ab_b", [128, 2, 128], BF16)
            si_b = sb("si_b", [128, 16, 16], BF16)
            iota_b = sb("iota_b", [128, 16], BF16)
            ij = sb("ij", [128, 2, 128], F32)
            eidx_f = sb("eidx_f", [128, 128], F32)
            eidx_u = [sb("eidx_u%d" % i, [128, 128], U32) for i in range(2)]
            gate = [sb("gate%d" % i, [128, 8, 16], F32) for i in range(2)]
            gsum = sb("gsum", [128, 8], F32)
            hpre = bank(4)[:, 0:128]
            gl = sb("gl", [128, 128], F32)
            x1b = [sb("x1b%d" % i, [128, D], BF16) for i in range(2)]
            prod = [sb("prod%d" % i, [128, D], BF16) for i in range(NP)]
            junk = sb("junk", [128, D], BF16)
            G = [sb("G%d" % i, [128, 2 * D], BF16) for i in range(NG)]
            diag = [sb("diag%d" % i, [128, 128], BF16) for i in range(ND)]
            ident_b = sb("ident_b", [128, 128], BF16)
            alphaI = sb("alphaI", [128, 128], F32)
            r2 = sb("r2", [128, D], F32)
            ot = [sb("ot%d" % i, [128, D], F32) for i in range(2)]
            b_w3 = Buf()
            b_mTc, b_xt, b_ot, b_x1, b_eu, b_gate = ([Buf(), Buf()] for _ in range(6))
            b_r1, b_x1T, b_qT, b_st, b_mv, b_rstd, b_nmr = (Buf() for _ in range(7))
            b_sv, b_siu, b_sif, b_work, b_cand, b_work2, b_top, b_pos = (Buf() for _ in range(8))
            b_abu, b_abf, b_oh, b_ij, b_ef, b_gsum, b_r2, b_idb = (Buf() for _ in range(8))
            b_G = [Buf() for _ in range(NG)]
            b_diag = [Buf() for _ in range(ND)]
            b_hp = [Buf() for _ in range(128)]
            b_gl = [Buf() for _ in range(128)]
            b_x1b = [Buf(), Buf()]
            b_prod = [Buf() for _ in range(NP)]
            b_ps = [Buf() for _ in range(8)]
            c_w3 = chan("w3")
            c_mtc, c_xt_, c_ot = [chan("mtc0"), chan("mtc1")], [chan("xt0"), chan("xt1")], [chan("ot0"), chan("ot1")]
            c_g = [chan("g%d" % i) for i in range(NG)]
            c_dbg = chan("dbg")
            dve.do(lambda: nc.vector.tensor_copy(out=ident_b[:], in_=ident[:]), reads=[b_const], writes=[b_idb])
            dve.do(lambda: nc.vector.tensor_scalar(out=alphaI[:], in0=ident[:], scalar1=float(ALPHA), scalar2=None, op0=ALU.mult), reads=[b_const], writes=[b_idb])
            dma(pool, c_w3, lambda: nc.gpsimd.dma_start(out=Wout[:], in_=w_out_d.rearrange("(kc p) n -> p kc n", p=128)), writes=[b_w3])
            dma(pool, c_w3, lambda: nc.gpsimd.dma_start(out=Wq[:], in_=w_q_d.rearrange("(kc p) n -> p kc n", p=128)), writes=[b_w3])
            dma(sp, c_w3, lambda: nc.sync.dma_start(out=skT[:].rearrange("p a b -> p (a b)"), in_=skT_d[:, :]), writes=[b_w3])
            dma(sp, c_w3, lambda: nc.sync.dma_start(out=lnp[:].rearrange("p a b -> p (a b)"), in_=lnp_d[:, :]), writes=[b_w3])
            dma(sp, c_w3, lambda: nc.sync.dma_start(out=iota16[:], in_=iota_d[:, :]), writes=[b_w3])
            dve.do(lambda: nc.vector.tensor_copy(out=iota_b[:], in_=iota16[:]), reads=[b_w3], writes=[b_w3])
            mT_v = mT_s.rearrange("(c p) t -> p c t", p=128)
            accp = bank(6, 2)
            NTILES = S // 128 if ntiles is None else ntiles
            if upto != "all":
                NTILES = 0

            def layer_norm_steps(L, src, b_src, dst, b_dst, gi):
                rs = b_src if isinstance(b_src, list) else [b_src]

                def s_stats():
                    for hf in range(2):
                        dve.do(lambda hf=hf: nc.vector.bn_stats(out=stats[:, hf, :], in_=src[:, hf * 512:(hf + 1) * 512]), reads=rs, writes=[b_st])
                    dve.do(lambda: nc.vector.bn_aggr(out=mv[:], in_=stats[:].rearrange("p a b -> p (a b)")), reads=[b_st], writes=[b_mv])
                    act.do(lambda: nc.scalar.activation(out=rstd[:], in_=mv[:, 1:2], func=AF.Sqrt, bias=float(LN_EPS), scale=1.0), reads=[b_mv], writes=[b_rstd])
                def s_norm():
                    dve.do(lambda: nc.vector.reciprocal(out=rstd[:], in_=rstd[:]), reads=[], writes=[b_rstd])
                    dve.do(lambda: nc.vector.scalar_tensor_tensor(out=nmr[:], in0=mv[:, 0:1], scalar=-1.0, in1=rstd[:], op0=ALU.mult, op1=ALU.mult),
                           reads=[b_mv, b_rstd], writes=[b_nmr])
                    act.do(lambda: nc.scalar.activation(out=dst[:], in_=src[:, 0:D], func=AF.Identity, bias=nmr[:, 0:1], scale=rstd[:, 0:1]),
                           reads=rs + [b_rstd, b_nmr], writes=[b_dst])
                def s_g():
                    dve.do(lambda: nc.vector.tensor_tensor(out=dst[:], in0=dst[:], in1=lnp[:, gi, :], op=ALU.mult), reads=[b_w3], writes=[b_dst])
                def s_b():
                    dve.do(lambda: nc.vector.tensor_tensor(out=dst[:], in0=dst[:], in1=lnp[:, gi + 1, :], op=ALU.add), reads=[b_w3], writes=[b_dst])
                L.extend([s_stats, s_norm, s_g, s_b])

            def load_tile(tt):
                if tt >= NTILES:
                    return
                if tt % 4 == 0:
                    cc = (tt // 4) % 2
                    dma(sp, c_mtc[cc], lambda: nc.sync.dma_start(out=mTc[cc][:], in_=mT_v[:, :, (tt // 4) * 512:(tt // 4 + 1) * 512]),
                        writes=[b_mTc[cc]])
                dma(sp, c_xt_[tt % 2], lambda: nc.sync.dma_start(out=xt[tt % 2][:], in_=x_d[tt * 128:(tt + 1) * 128, :]), writes=[b_xt[tt % 2]])

            def topk16(L, src_fn, b_src, vals, b_vals, idxs, b_idxs, wk, b_wk):
                def f():
                    src = src_fn()
                    dve.do(lambda: nc.vector.max(out=vals[:, 0:8], in_=src), reads=[b_src], writes=[b_vals])
                    dve.do(lambda: nc.vector.max_index(out=idxs[:, 0:8], in_max=vals[:, 0:8], in_values=src), reads=[b_src, b_vals], writes=[b_idxs])
                    dve.do(lambda: nc.vector.match_replace(out=wk, in_to_replace=vals[:, 0:8], in_values=src, imm_value=-1e30),
                           reads=[b_src, b_vals], writes=[b_wk])
                    dve.do(lambda: nc.vector.max(out=vals[:, 8:16], in_=wk), reads=[b_wk], writes=[b_vals])
                    dve.do(lambda: nc.vector.max_index(out=idxs[:, 8:16], in_max=vals[:, 8:16], in_values=wk), reads=[b_wk, b_vals], writes=[b_idxs])
                L.append(f)

            def front_steps(tt):
                L = []
                if tt >= NTILES:
                    return L
                par = tt % 2
                cc = (tt // 4) % 2
                sub = tt % 4
                xs = tt % 2
                X1 = x1[par]
                bX1 = b_x1[par]
                for hf in range(2):
                    for f0 in (0, 4):
                        def s_wout(hf=hf, f0=f0):
                            for f in range(f0, f0 + 4):
                                pe.do(lambda f=f: nc.tensor.matmul(bank(hf), lhsT=mTc[cc][:, f, sub * 128:(sub + 1) * 128],
                                                                  rhs=Wout[:, f, hf * 512:(hf + 1) * 512], start=(f == 0), stop=False),
                                      reads=[b_mTc[cc], b_w3], writes=[b_ps[hf]])
                            if f0 == 4:
                                pe.do(lambda: nc.tensor.matmul(bank(hf), lhsT=alphaI[:], rhs=xt[xs][:, hf * 512:(hf + 1) * 512], start=False, stop=True),
                                      reads=[b_xt[xs], b_idb], writes=[b_ps[hf]])
                        L.append(s_wout)
                layer_norm_steps(L, bank(0, 2), [b_ps[0], b_ps[1]], X1, bX1, 0)
                if debug:
                    L.append(lambda: dma(sp, c_dbg, lambda: nc.sync.dma_start(out=x1_dbg[tt * 128:(tt + 1) * 128, :], in_=X1[:]), reads=[bX1]))
                L.append(lambda: act.do(lambda: nc.scalar.activation(out=x1b[par][:], in_=X1[:], func=AF.Identity), reads=[bX1], writes=[b_x1b[par]]))
                for f0 in (0, 4):
                    def s_tr(f0=f0):
                        for f in range(f0, f0 + 4):
                            pe.do(lambda f=f: nc.tensor.transpose(out=bank(2, 2)[:, f * 128:(f + 1) * 128], in_=X1[:, f * 128:(f + 1) * 128], identity=ident[:]),
                                  reads=[bX1, b_const], writes=[b_ps[2 + f // 4]])
                    L.append(s_tr)
                L.append(lambda: act.do(lambda: nc.scalar.activation(out=x1T[:].rearrange("p a b -> p (a b)"), in_=bank(2, 2), func=AF.Identity),
                                        reads=[b_ps[2], b_ps[3]], writes=[b_x1T]))
                for h in range(8):
                    def s_q(h=h):
                        for kc in range(8):
                            pe.do(lambda kc=kc: nc.tensor.matmul(bank(0, 2)[:, h * 128:(h + 1) * 128], lhsT=Wq[:, kc, h * 128:(h + 1) * 128],
                                                                rhs=x1T[:, kc, :], start=(kc == 0), stop=(kc == 7)),
                                  reads=[b_w3, b_x1T], writes=[b_ps[h // 4]])
                    L.append(s_q)
                L.append(lambda: act.do(lambda: nc.scalar.activation(out=qT[:].rearrange("p a b -> p (a b)"), in_=bank(0, 2), func=AF.Identity),
                                        reads=[b_ps[0], b_ps[1]], writes=[b_qT]))
                for rnd, b0 in ((0, 2), (1, 0)):
                    def s_sc(rnd=rnd, b0=b0):
                        for hh in range(4):
                            h = rnd * 4 + hh
                            for c in range(2):
                                pr = slice(c * 64, (c + 1) * 64)
                                pe.do(lambda h=h, hh=hh, c=c, pr=pr: nc.tensor.matmul(bank(b0 + c)[:, hh * 128:(hh + 1) * 128], lhsT=qT[pr, h, :],
                                                                                    rhs=skT[pr, h, :], start=True, stop=True),
                                      reads=[b_qT, b_w3], writes=[b_ps[b0 + c]])
                    L.append(s_sc)
                    for hh in range(4):
                        for c in range(2):
                            hc = c * 8 + rnd * 4 + hh
                            topk16(L, (lambda b0=b0, c=c, hh=hh: bank(b0 + c)[:, hh * 128:(hh + 1) * 128]), b_ps[b0 + c],
                                   sv[:, hc, :], b_sv, si_u[:, hc, :], b_siu, work[:], b_work)
                sv4 = sv[:].rearrange("p (c h) k -> p c h k", c=2)
                si4 = si_b[:].rearrange("p (c h) k -> p c h k", c=2)

                def s_cand():
                    dve.do(lambda: nc.vector.tensor_copy(out=si_b[:], in_=si_u[:]), reads=[b_siu], writes=[b_sif])
                    dve.do(lambda: nc.vector.tensor_tensor(out=cand[:].rearrange("p h (a b) -> p h a b", b=16),
                                                           in0=sv4[:, 0, :, :].unsqueeze(3).broadcast_to([128, 8, 16, 16]),
                                                           in1=sv4[:, 1, :, :].unsqueeze(2).broadcast_to([128, 8, 16, 16]), op=ALU.add),
                           reads=[b_sv], writes=[b_cand])
                L.append(s_cand)
                for h in range(8):
                    topk16(L, (lambda h=h: cand[:, h, :]), b_cand, top[:, h, :], b_top, pos_u[:, h, :], b_pos, work2[:], b_work2)
                posf = pos_u[:].rearrange("p h k -> p (h k)")

                def s_ab():
                    dve.do(lambda: nc.vector.tensor_single_scalar(out=ab_u[:, 0, :], in_=posf, scalar=4, op=ALU.logical_shift_right), reads=[b_pos], writes=[b_abu])
                    dve.do(lambda: nc.vector.tensor_single_scalar(out=ab_u[:, 1, :], in_=posf, scalar=15, op=ALU.bitwise_and), reads=[b_pos], writes=[b_abu])
                    dve.do(lambda: nc.vector.tensor_copy(out=ab_b[:], in_=ab_u[:]), reads=[b_abu], writes=[b_abf])
                L.append(s_ab)
                for c in range(2):
                    def s_lk(c=c):
                        abv = ab_b[:, c, :].rearrange("p (h k) -> p h k", k=16)
                        dve.do(lambda: nc.vector.tensor_tensor(out=oh[:], in0=abv.unsqueeze(3).broadcast_to([128, 8, 16, 16]),
                                                               in1=iota_b[:].unsqueeze(1).unsqueeze(1).broadcast_to([128, 8, 16, 16]), op=ALU.is_equal),
                               reads=[b_abf, b_w3], writes=[b_oh])
                        dve.do(lambda: nc.vector.tensor_tensor(out=oh[:], in0=oh[:], in1=si4[:, c, :, :].unsqueeze(2).broadcast_to([128, 8, 16, 16]), op=ALU.mult),
                               reads=[b_sif], writes=[b_oh])
                        dve.do(lambda: nc.vector.tensor_reduce(out=ij[:, c, :], in_=oh[:].rearrange("p h k a -> p (h k) a"), axis=AX.X, op=ALU.add),
                               reads=[b_oh], writes=[b_ij])
                    L.append(s_lk)

                def s_eidx():
                    dve.do(lambda: nc.vector.scalar_tensor_tensor(out=eidx_f[:], in0=ij[:, 0, :], scalar=128.0, in1=ij[:, 1, :], op0=ALU.mult, op1=ALU.add),
                           reads=[b_ij], writes=[b_ef])
                    dve.do(lambda: nc.vector.tensor_copy(out=eidx_u[par][:], in_=eidx_f[:]), reads=[b_ef], writes=[b_eu[par]])
                L.append(s_eidx)
                GT = gate[par]
                bGT = b_gate[par]

                def s_gate1():
                    dve.do(lambda: nc.vector.tensor_tensor(out=GT[:], in0=top[:], in1=top[:, :, 0:1].broadcast_to([128, 8, 16]), op=ALU.subtract),
                           reads=[b_top], writes=[bGT])
                    act.do(lambda: nc.scalar.activation(out=GT[:], in_=GT[:], func=AF.Exp), reads=[], writes=[bGT])
                def s_gate2():
                    dve.do(lambda: nc.vector.tensor_reduce(out=gsum[:], in_=GT[:], axis=AX.X, op=ALU.add), reads=[bGT], writes=[b_gsum])
                    dve.do(lambda: nc.vector.reciprocal(out=gsum[:], in_=gsum[:]), reads=[], writes=[b_gsum])
                    dve.do(lambda: nc.vector.tensor_tensor(out=GT[:], in0=GT[:], in1=gsum[:].unsqueeze(2).broadcast_to([128, 8, 16]), op=ALU.mult),
                           reads=[b_gsum], writes=[bGT])
                L.extend([s_gate1, s_gate2])
                return L

            def slot_head(tt, s_):
                par = tt % 2
                g = s_ % NG
                pb = s_ % NP
                dma(pool, c_g[g], lambda: nc.gpsimd.indirect_dma_start(
                    out=G[g][:], out_offset=None, in_=uv_s[:, :], in_offset=bass.IndirectOffsetOnAxis(ap=eidx_u[par][:, s_:s_ + 1], axis=0)),
                    reads=[b_eu[par]], writes=[b_G[g]])
                dve.do(lambda: nc.vector.tensor_tensor(out=prod[pb][:], in0=G[g][:, 0:D], in1=x1b[par][:], op=ALU.mult),
                       reads=[b_G[g], b_x1b[par]], writes=[b_prod[pb]])
                act.do(lambda: nc.scalar.activation(out=junk[:], in_=prod[pb][:], func=AF.Identity, accum_out=hpre[:, s_:s_ + 1]),
                       reads=[b_prod[pb]], writes=[b_hp[s_]])
                act.do(lambda: nc.scalar.activation(out=gl[:, s_:s_ + 1], in_=hpre[:, s_:s_ + 1], func=AF.Gelu), reads=[b_hp[s_]], writes=[b_gl[s_]])

            def slot_tail(tt, s_):
                par = tt % 2
                g = s_ % NG
                d_ = s_ % ND
                gatef = gate[par][:].rearrange("p h k -> p (h k)")
                dve.do(lambda: nc.vector.tensor_scalar(out=diag[d_][:], in0=ident_b[:], scalar1=gl[:, s_:s_ + 1], scalar2=gatef[:, s_:s_ + 1],
                                                       op0=ALU.mult, op1=ALU.mult),
                       reads=[b_gl[s_], b_gate[par], b_idb], writes=[b_diag[d_]])
                for hf in range(2):
                    pe.do(lambda hf=hf: nc.tensor.matmul(bank(6 + hf), lhsT=diag[d_][:], rhs=G[g][:, D + hf * 512:D + (hf + 1) * 512],
                                                        start=(s_ == 0), stop=(s_ == 127)),
                          reads=[b_diag[d_], b_G[g]], writes=[b_ps[6 + hf]])

            def tail(tt):
                par = tt % 2
                os_ = tt % 2
                if debug:
                    dve.do(lambda: nc.vector.tensor_copy(out=r2[:], in_=accp), reads=[b_ps[6], b_ps[7]], writes=[b_r2])
                    dma(sp, c_dbg, lambda: nc.sync.dma_start(out=yp_dbg[tt * 128:(tt + 1) * 128, :], in_=r2[:]), reads=[b_r2])
                dve.do(lambda: nc.vector.scalar_tensor_tensor(out=r2[:], in0=x1[par][:], scalar=float(ALPHA), in1=accp, op0=ALU.mult, op1=ALU.add),
                       reads=[b_x1[par], b_ps[6], b_ps[7]], writes=[b_r2])
                L = []
                layer_norm_steps(L, r2, b_r2, ot[os_], b_ot[os_], 2)
                for f in L:
                    f()
                dma(sp, c_ot[os_], lambda: nc.sync.dma_start(out=out_d[tt * 128:(tt + 1) * 128, :], in_=ot[os_][:]), reads=[b_ot[os_]])

            load_tile(0)
            load_tile(1)
            for f in front_steps(0):
                f()
            for tt in range(NTILES):
                load_tile(tt + 2)
                nxt = front_steps(tt + 1)
                k = 0
                for s_ in range(128 + LAG):
                    if s_ < 128:
                        slot_head(tt, s_)
                    if s_ >= LAG:
                        slot_tail(tt, s_ - LAG)
                    if s_ < 128:
                        tgt = min(len(nxt), ((s_ + 1) * len(nxt)) // FRONT_SPAN)
                        while k < tgt:
                            nxt[k]()
                            k += 1
                tail(tt)
            barrier()
    return nc


_NC_CACHE = {}


def _host_inputs(x, mem, rel_bias, w_in, b_gate, w_mem_kv, sinks, w_branch_a, w_branch_b, w_branch_c, w_out, ln1_g, ln1_b,
                 peer_w_query, peer_sub_keys, peer_u, peer_v, ln2_g, ln2_b):
    f = lambda a: np.ascontiguousarray(np.asarray(a, dtype=np.float32))
    pairs, bucket, mask, heads = _bias_tables()
    rb = np.asarray(rel_bias, np.float32)
    biasT = np.zeros((128, 10, 4, 128), np.float32)
    maskT = np.zeros((128, 10, 4, 128), np.float32)
    for p in range(10):
        for hh in range(2):
            for kb in range(2):
                biasT[:, p, 2 * hh + kb, :] = rb[bucket[p, kb], heads[p] + hh]
                maskT[:, p, 2 * hh + kb, :] = mask[p, kb]
    sk = np.asarray(sinks, np.float32)[0]
    sinksP = np.zeros((128, 4), np.float32)
    for p in range(4):
        sinksP[0:64, p] = sk[2 * p]
        sinksP[64:128, p] = sk[2 * p + 1]
    lnp = np.stack([np.asarray(a, np.float32)[0] for a in (ln1_g, ln1_b, ln2_g, ln2_b)], 0)
    lnp = np.ascontiguousarray(np.broadcast_to(lnp.reshape(1, 4 * D), (128, 4 * D)))
    skT = np.asarray(peer_sub_keys, np.float32)[0].transpose(1, 3, 0, 2).reshape(128, 8 * 128)
    shared = {
        "w_in": f(w_in[0]), "w_mem_kv": f(w_mem_kv[0]), "w_a": f(w_branch_a[0]), "w_b": f(w_branch_b[0]), "w_c": f(w_branch_c[0]),
        "w_out": f(w_out[0]), "w_q": f(peer_w_query[0]), "skT": f(skT), "peer_u": f(peer_u[0]), "peer_v": f(peer_v[0]),
        "biasT": f(biasT.reshape(128, 5120)), "maskT": f(maskT.reshape(128, 5120)),
        "bgate": f(np.asarray(b_gate, np.float32)[0].reshape(24, 128).T), "sinksP": f(sinksP), "lnp": f(lnp),
        "ident": np.eye(128, dtype=np.float32), "iota16": f(np.broadcast_to(np.arange(16, dtype=np.float32), (128, 16))),
    }
    x = np.asarray(x, np.float32)
    mem = np.asarray(mem, np.float32)
    in_maps = []
    for b in range(x.shape[0]):
        m = dict(shared)
        m["x"] = f(x[b])
        m["xT"] = f(x[b].T)
        m["memT"] = f(mem[b].T)
        in_maps.append(m)
    return in_maps


def kernel(**inputs):
    in_maps = _host_inputs(**inputs)
    if "nc" not in _NC_CACHE:
        _NC_CACHE["nc"] = build_nc()
    nc = _NC_CACHE["nc"]
    res = run_bass_kernel_spmd(nc, in_maps, core_ids=list(range(NCORES)))
    return np.stack([np.asarray(r["out"], dtype=np.float32) for r in res.results], 0)
```

```python
import numpy as np
import concourse.bass as bass
import concourse.mybir as mybir
from concourse.bass_utils import run_bass_kernel_spmd
from contextlib import ExitStack

F32 = mybir.dt.float32
BF16 = mybir.dt.bfloat16
U32 = mybir.dt.uint32
AF = mybir.ActivationFunctionType
ALU = mybir.AluOpType
AX = mybir.AxisListType

S = 4096
D = 1024
NCORES = 8
ALPHA = 2.0 ** 0.25
LN_EPS = 1e-5
NEG = -30000.0
N_EXP = 16384
NG = 12
ND = 8
LAG = 2
NP = 6


class Buf:
    __slots__ = ("w", "r")

    def __init__(self):
        self.w = None
        self.r = {}


class Q:
    def __init__(self, nc, eng, st, name, is_pe=False):
        self.nc = nc
        self.eng = eng
        self.sem = st.enter_context(nc.semaphore("q_" + name))
        self.n = 0
        self.seen = {}
        self.is_pe = is_pe

    def wait(self, ev):
        if ev is None:
            return
        sem, val = ev
        if sem is self.sem and self.is_pe:
            return
        k = id(sem)
        if self.seen.get(k, -1) >= val:
            return
        self.eng.wait_ge(sem, val)
        self.seen[k] = val

    def deps(self, reads, writes, extra, skip_sem=None):
        for b in reads:
            if b.w is not None and b.w[0] is not skip_sem:
                self.wait(b.w)
        for b in writes:
            if b.w is not None and b.w[0] is not skip_sem:
                self.wait(b.w)
            for ev in b.r.values():
                self.wait(ev)
        for ev in extra:
            self.wait(ev)

    @staticmethod
    def mark(ev, reads, writes):
        for b in reads:
            k = id(ev[0])
            old = b.r.get(k)
            if old is None or old[1] < ev[1]:
                b.r[k] = ev
        for b in writes:
            b.w = ev
            b.r = {}

    def do(self, fn, reads=(), writes=(), extra=()):
        self.deps(reads, writes, extra)
        ins = fn()
        self.n += 1
        ins.then_inc(self.sem, 1)
        ev = (self.sem, self.n)
        self.mark(ev, reads, writes)
        return ev


class DmaChan:
    def __init__(self, nc, st, name):
        self.sem = st.enter_context(nc.semaphore("c_" + name))
        self.n = 0


def dma(q, chan, fn, reads=(), writes=(), extra=()):
    q.deps(reads, writes, extra, skip_sem=chan.sem)
    ins = fn()
    chan.n += 16
    ins.then_inc(chan.sem, 16)
    ev = (chan.sem, chan.n)
    Q.mark(ev, reads, writes)
    return ev


class SpQ(Q):
    def __init__(self, nc, eng):
        self.nc = nc
        self.eng = eng
        self.sem = None
        self.n = 0
        self.seen = {}
        self.is_pe = False


def _t5_bucket(dist):
    n = np.asarray(dist, dtype=np.int32)
    max_exact = 16
    nf = np.maximum(n, 1).astype(np.float32)
    scale = np.float32(np.log(2048 / max_exact))
    large = max_exact + (np.log(nf / np.float32(max_exact)) / scale * np.float32(32 - max_exact)).astype(np.int32)
    large = np.minimum(large, 31)
    return np.where(n < max_exact, n, large).astype(np.int32)


def _bias_tables():
    i = np.arange(128)[None, :]
    j = np.arange(128)[:, None]
    off_prev = 128 + i - j
    off_cur = i - j
    pairs = []
    for g, r in enumerate((1, 4, 16)):
        for m in range(2):
            pairs.append(("A", r, g * 4 + 2 * m))
    for m in range(4):
        pairs.append(("B", 1, 12 + 2 * m))
    bucket = np.zeros((10, 2, 128, 128), np.int32)
    mask = np.zeros((10, 2, 128, 128), np.float32)
    heads = []
    for p, (kind, r, h0) in enumerate(pairs):
        W = 128 if kind == "A" else 127
        for kb, off in enumerate((off_prev, off_cur)):
            bucket[p, kb] = _t5_bucket(np.clip(off, 0, W) * r)
            valid = (off >= 0) & (off <= W)
            mask[p, kb] = np.where(valid, 0.0, NEG)
        heads.append(h0)
    return pairs, bucket, mask, heads


def build_nc(debug=False, upto="all", ntiles=None, njobs=None, do_c=True, stage=99, jobsel=None):
    nc = bass.Bass("TRN2", target_bir_lowering=False)
    dt_in = lambda n, s, t=F32: nc.dram_tensor(n, s, t, kind="ExternalInput").ap()
    xT_d = dt_in("xT", [D, S])
    x_d = dt_in("x", [S, D])
    memT_d = dt_in("memT", [D, 256])
    w_in_d = dt_in("w_in", [D, 6656])
    w_kv_d = dt_in("w_mem_kv", [D, 1024])
    w_a_d = dt_in("w_a", [256, D])
    w_b_d = dt_in("w_b", [512, D])
    w_c_d = dt_in("w_c", [512, D])
    w_out_d = dt_in("w_out", [D, D])
    w_q_d = dt_in("w_q", [D, D])
    skT_d = dt_in("skT", [128, 8 * 128])
    pu_d = dt_in("peer_u", [N_EXP, D])
    pv_d = dt_in("peer_v", [N_EXP, D])
    biasT_d = dt_in("biasT", [128, 10 * 512])
    maskT_d = dt_in("maskT", [128, 10 * 512])
    bgate_d = dt_in("bgate", [128, 24])
    sinks_d = dt_in("sinksP", [128, 4])
    lnp_d = dt_in("lnp", [128, 4 * D])
    ident_d = dt_in("ident", [128, 128])
    iota_d = dt_in("iota16", [128, 16])
    out_d = nc.dram_tensor("out", [S, D], F32, kind="ExternalOutput").ap()
    skind = "ExternalOutput" if debug else "Internal"
    yT_s = nc.dram_tensor("yT_scr", [1280, S], BF16, kind=skind).ap()
    mT_s = nc.dram_tensor("mT_scr", [D, S], BF16, kind=skind).ap()
    uv_s = nc.dram_tensor("uv_scr", [N_EXP, 2 * D], BF16, kind="Internal").ap()
    if debug:
        x1_dbg = nc.dram_tensor("x1_dbg", [S, D], F32, kind="ExternalOutput").ap()
        yp_dbg = nc.dram_tensor("yp_dbg", [S, D], F32, kind="ExternalOutput").ap()

    with ExitStack() as gst:
        pe = Q(nc, nc.tensor, gst, "pe", is_pe=True)
        act = Q(nc, nc.scalar, gst, "act")
        dve = Q(nc, nc.vector, gst, "dve")
        pool = Q(nc, nc.gpsimd, gst, "pool")
        sp = SpQ(nc, nc.sync)
        queues = [pe, act, dve, pool, sp]
        chans = []

        def chan(name):
            c = DmaChan(nc, gst, name)
            chans.append(c)
            return c

        def barrier(skip=()):
            for q in queues:
                for o in (pe, act, dve, pool):
                    if o is not q and o.n > 0:
                        q.wait((o.sem, o.n))
                for c in chans:
                    if c.n > 0 and c not in skip:
                        q.wait((c.sem, c.n))

        psall = gst.enter_context(nc.psum_tensor("psall", [128, 4096], F32))

        def bank(i, n=1):
            return psall[:, i * 512:(i + n) * 512]

        ident = gst.enter_context(nc.sbuf_tensor("s_ident", [128, 128], F32))
        ones_b = gst.enter_context(nc.sbuf_tensor("s_ones_b", [128, 128], BF16))
        c_const = chan("const")
        b_const = Buf()
        dma(sp, c_const, lambda: nc.sync.dma_start(out=ident[:], in_=ident_d[:, :]), writes=[b_const])
        dve.do(lambda: nc.vector.memset(ones_b[:], 1.0), writes=[b_const])

        w_in_v = w_in_d.rearrange("(kc p) n -> p kc n", p=128)

        c_cv = chan("cv")
        cv_list = [(tab, c0, c) for (tab, c0) in ((pu_d, 0), (pv_d, D)) for c in range(16)]

        def convert_some(k):
            for _ in range(k):
                if not cv_list:
                    return
                tab, c0, c = cv_list.pop(0)
                src = tab[c * 1024:(c + 1) * 1024, :].rearrange("(p r) d -> p r d", p=128)
                dst = uv_s[c * 1024:(c + 1) * 1024, c0:c0 + D].rearrange("(p r) d -> p r d", p=128)
                dma(pool, c_cv, lambda: nc.gpsimd.dma_start(out=dst, in_=src))

        with ExitStack() as st:
            sb = lambda n, s, t: st.enter_context(nc.sbuf_tensor("s_" + n, s, t))
            XT = sb("XT", [128, 8, S], BF16)
            b_XTc = [Buf() for _ in range(4)]
            c_xtc = [chan("xc%d" % i) for i in range(4)]
            st01 = st.enter_context(ExitStack())
            sb1 = lambda n, s, t: st01.enter_context(nc.sbuf_tensor("s_" + n, s, t))
            BT = sb1("BT", [128, 10, 512], BF16)
            es = sb1("es", [128, 4], F32)
            KmT = sb1("KmT", [128, 4, 256], BF16)
            Vm = sb1("Vm", [128, 2, 512], BF16)
            b_BT, b_es, b_KmT, b_Vm = Buf(), Buf(), Buf(), Buf()
            b_ps = [Buf() for _ in range(8)]
            c_misc = chan("misc")

            Wt = [sb1("Wt%d" % i, [128, 8, 384], BF16) for i in range(2)]
            b_Wt = [Buf(), Buf()]
            c_wt, c_ys = [chan("wt0"), chan("wt1")], [chan("ys0"), chan("ys1")]
            jobs = []
            for m in range(2):
                for g, r in enumerate((1, 4, 16)):
                    c = g * 256 + 2 * m * 64
                    jobs.append(dict(q=c, k=(768 + c, 768 + c + 64), v=(1536 + c, 1536 + c + 64), r=r, bp=g * 2 + m,
                                     first=(g == 0), last=(g == 2), sink=None, row=m * 128))
            for kv in range(2):
                for m in range(2):
                    hq = kv * 4 + 2 * m
                    jobs.append(dict(q=2304 + hq * 64, k=(2816 + kv * 64,) * 2, v=(2944 + kv * 64,) * 2, r=1, bp=6 + kv * 2 + m,
                                     first=True, last=True, sink=kv * 2 + m, row=256 + (kv * 2 + m) * 128))

            def load_w(ji):
                jb = jobs[ji]
                s = ji % 2
                dma(pool, c_wt[s], lambda: nc.gpsimd.dma_start(out=Wt[s][:, :, 0:128], in_=w_in_v[:, :, jb["q"]:jb["q"] + 128]),
                    writes=[b_Wt[s]])
                for t, key in ((0, "k"), (1, "v")):
                    for hh in range(2):
                        c0 = jb[key][hh]
                        o0 = 128 + t * 128 + hh * 64
                        dma(pool, c_wt[s], lambda c0=c0, o0=o0: nc.gpsimd.dma_start(out=Wt[s][:, :, o0:o0 + 64], in_=w_in_v[:, :, c0:c0 + 64]),
                            writes=[b_Wt[s]])


            xT_v = xT_d.rearrange("(kc p) t -> p kc t", p=128)
            with ExitStack() as st0:
                sb0 = lambda n, s, t: st0.enter_context(nc.sbuf_tensor("s_" + n, s, t))
                bt_f = sb0("bt_f", [128, 5120], F32)
                mk_f = sb0("mk_f", [128, 5120], F32)
                sk_in = sb0("sk_in", [128, 4], F32)
                memTb = sb0("memTb", [128, 8, 256], BF16)
                Wkv = sb0("Wkv", [128, 8, 1024], BF16)
                b_btf = b_mkf = b_skin = b_memTb = b_Wkv = Buf()
                dma(sp, c_misc, lambda: nc.sync.dma_start(out=bt_f[:], in_=biasT_d[:, :]), writes=[b_btf])
                dma(sp, c_misc, lambda: nc.sync.dma_start(out=mk_f[:], in_=maskT_d[:, :]), writes=[b_mkf])
                dma(sp, c_misc, lambda: nc.sync.dma_start(out=sk_in[:], in_=sinks_d[:, :]), writes=[b_skin])
                dma(pool, c_misc, lambda: nc.gpsimd.dma_start(out=memTb[:], in_=memT_d.rearrange("(kc p) m -> p kc m", p=128)),
                    writes=[b_memTb])
                dma(pool, c_misc, lambda: nc.gpsimd.dma_start(out=Wkv[:], in_=w_kv_d.rearrange("(kc p) n -> p kc n", p=128)),
                    writes=[b_Wkv])
                if upto != "p0" and njobs != 0:
                    load_w(0)
                for i in range(4):
                    dma(pool, c_xtc[i], lambda i=i: nc.gpsimd.dma_start(out=XT[:, :, i * 1024:(i + 1) * 1024],
                                                                      in_=xT_v[:, :, i * 1024:(i + 1) * 1024]), writes=[b_XTc[i]])
                dve.do(lambda: nc.vector.tensor_tensor(out=BT[:].rearrange("p a b -> p (a b)"), in0=bt_f[:], in1=mk_f[:], op=ALU.add),
                       reads=[b_btf, b_mkf], writes=[b_BT])
                act.do(lambda: nc.scalar.activation(out=es[:], in_=sk_in[:], func=AF.Exp), reads=[b_skin], writes=[b_es])
                for h in range(4):
                    pb = h % 2
                    for kc in range(8):
                        pe.do(lambda h=h, kc=kc, pb=pb: nc.tensor.matmul(bank(pb)[:, 0:256], lhsT=Wkv[:, kc, h * 128:(h + 1) * 128],
                                                                          rhs=memTb[:, kc, :], start=(kc == 0), stop=(kc == 7)),
                              reads=[b_Wkv, b_memTb], writes=[b_ps[pb]])
                    act.do(lambda h=h, pb=pb: nc.scalar.activation(out=KmT[:, h, :], in_=bank(pb)[:, 0:256], func=AF.Identity),
                           reads=[b_ps[pb]], writes=[b_KmT])
                for kb in range(2):
                    for kc in range(8):
                        pe.do(lambda kb=kb, kc=kc: nc.tensor.matmul(bank(kb), lhsT=memTb[:, kc, kb * 128:(kb + 1) * 128],
                                                                     rhs=Wkv[:, kc, 512:1024], start=(kc == 0), stop=(kc == 7)),
                              reads=[b_Wkv, b_memTb], writes=[b_ps[kb]])
                    act.do(lambda kb=kb: nc.scalar.activation(out=Vm[:, kb, :], in_=bank(kb), func=AF.Identity),
                           reads=[b_ps[kb]], writes=[b_Vm])
                barrier(skip=c_xtc + c_wt)

            QT = sb1("QT", [128, S], BF16)
            KT = sb1("KT", [128, S], BF16)
            Vt = sb1("Vt", [128, 32, 128], BF16)
            Acc = sb1("Acc", [128, 2, S], F32)
            Yt = [sb1("Yt%d" % i, [128, S], BF16) for i in range(2)]
            Pf = [sb1("Pf%d" % i, [128, 512], F32) for i in range(2)]
            PT = [sb1("PT%d" % i, [128, 512], BF16) for i in range(2)]
            b_QT, b_KT, b_Vt, b_Acc = Buf(), Buf(), Buf(), Buf()
            b_Yt = [Buf(), Buf()]
            b_Pf = [Buf(), Buf()]
            b_PT = [Buf(), Buf()]
            b_ps = [Buf() for _ in range(8)]
            b_nz = [[Buf(), Buf()] for _ in range(4)]
            b_S = [Buf(), Buf()]

            if njobs is not None:
                jobs = jobs[:njobs]
            if jobsel is not None:
                jobs = [jobs[i] for i in jobsel]
            if upto == "p0":
                jobs = []
            ycount = 0
            for ji, jb in enumerate(jobs):
                s = ji % 2
                if ji + 1 < len(jobs):
                    load_w(ji + 1)
                convert_some(4)
                r = jb["r"]
                nb = 32 // r
                for which, dst, bdst in ((0, QT, b_QT), (1, KT, b_KT)):
                    for tc in range(8):
                        pb = tc % 2
                        for kc in range(8):
                            pe.do(lambda kc=kc, tc=tc, pb=pb, which=which: nc.tensor.matmul(
                                bank(pb), lhsT=Wt[s][:, kc, which * 128:(which + 1) * 128], rhs=XT[:, kc, tc * 512:(tc + 1) * 512],
                                start=(kc == 0), stop=(kc == 7)), reads=[b_Wt[s], b_XTc[tc // 2]], writes=[b_ps[pb]])
                        act.do(lambda tc=tc, pb=pb, dst=dst: nc.scalar.activation(out=dst[:, tc * 512:(tc + 1) * 512], in_=bank(pb), func=AF.Identity),
                               reads=[b_ps[pb]], writes=[bdst])

                def tokset(blk):
                    rho, n = blk // nb, blk % nb
                    st_ = r * 128 * n + rho
                    return st_, n

                for bg in (range(8) if stage >= 2 else ()):
                    pb = bg % 2
                    for j in range(4):
                        blk = bg * 4 + j
                        st_, n = tokset(blk)
                        for kc in range(8):
                            pe.do(lambda kc=kc, j=j, pb=pb, st_=st_: nc.tensor.matmul(
                                bank(pb)[:, j * 128:(j + 1) * 128], lhsT=XT[:, kc, st_:st_ + 127 * r + 1:r], rhs=Wt[s][:, kc, 256:384],
                                start=(kc == 0), stop=(kc == 7)), reads=[b_Wt[s]] + b_XTc, writes=[b_ps[pb]])
                    act.do(lambda bg=bg, pb=pb: nc.scalar.activation(out=Vt[:, bg * 4:(bg + 1) * 4, :].rearrange("p a b -> p (a b)"),
                                                                   in_=bank(pb), func=AF.Identity), reads=[b_ps[pb]], writes=[b_Vt])
                bp = jb["bp"]

                def qk(blk):
                    st_, n = tokset(blk)
                    pi = blk % 2
                    qs = slice(st_, st_ + 127 * r + 1, r)
                    ks_c = qs
                    ks_p = slice(st_ - 128 * r, st_ - r + 1, r)
                    sb0 = 2 if pi == 0 else 0
                    bS = [b_ps[sb0], b_ps[sb0 + 1]]
                    for hh in range(2):
                        ps_ = slice(hh * 64, (hh + 1) * 64)
                        if n > 0:
                            pe.do(lambda: nc.tensor.matmul(
                                bank(sb0 + hh)[:, 0:128], lhsT=KT[ps_, ks_p], rhs=QT[ps_, qs], start=True, stop=True),
                                reads=[b_KT, b_QT], writes=bS)
                        pe.do(lambda: nc.tensor.matmul(
                            bank(sb0 + hh)[:, 128:256], lhsT=KT[ps_, ks_c], rhs=QT[ps_, qs], start=True, stop=True),
                            reads=[b_KT, b_QT], writes=bS)

                def softmax(blk):
                    st_, n = tokset(blk)
                    pi = blk % 2
                    sb0 = 2 if pi == 0 else 0
                    bS = [b_ps[sb0], b_ps[sb0 + 1]]
                    Sv = bank(sb0, 2).rearrange("p (b c) -> p b c", c=512)[:, :, 0:256]
                    h3 = lambda ap: ap.rearrange("p (b c) -> p b c", c=256)
                    if n > 0:
                        dve.do(lambda: nc.vector.scalar_tensor_tensor(
                            out=h3(Pf[pi][:]), in0=Sv, scalar=0.125, in1=h3(BT[:, bp, :]), op0=ALU.mult, op1=ALU.add),
                            reads=bS + [b_BT], writes=[b_Pf[pi]])
                        act.do(lambda: nc.scalar.activation(out=PT[pi][:], in_=Pf[pi][:], func=AF.Exp),
                               reads=[b_Pf[pi]], writes=[b_PT[pi]])
                    else:
                        dve.do(lambda: nc.vector.scalar_tensor_tensor(
                            out=h3(Pf[pi][:])[:, :, 128:256], in0=Sv[:, :, 128:256], scalar=0.125, in1=h3(BT[:, bp, :])[:, :, 128:256],
                            op0=ALU.mult, op1=ALU.add), reads=bS + [b_BT], writes=[b_Pf[pi]])
                        act.do(lambda: nc.scalar.activation(out=h3(PT[pi][:])[:, :, 128:256], in_=h3(Pf[pi][:])[:, :, 128:256], func=AF.Exp),
                               reads=[b_Pf[pi]], writes=[b_PT[pi]])

                def pv(blk):
                    st_, n = tokset(blk)
                    pi = blk % 2
                    nzb = blk % 4
                    half = 0
                    co = 0
                    for hh in range(2):
                        ps_ = slice(hh * 64, (hh + 1) * 64)
                        for (oc, lhs_fn) in ((co, lambda b_: Vt[:, b_, ps_]), (co + 128, lambda b_: ones_b[:, 0:64])):
                            if n > 0:
                                pe.do(lambda: nc.tensor.matmul(
                                    bank(4 + nzb)[ps_, oc:oc + 128], lhsT=lhs_fn(blk - 1), rhs=PT[pi][:, (2 * hh) * 128:(2 * hh + 1) * 128],
                                    start=True, stop=False), reads=[b_Vt, b_PT[pi]], writes=[b_nz[nzb][half]])
                            pe.do(lambda: nc.tensor.matmul(
                                bank(4 + nzb)[ps_, oc:oc + 128], lhsT=lhs_fn(blk), rhs=PT[pi][:, (2 * hh + 1) * 128:(2 * hh + 2) * 128],
                                start=(n == 0), stop=True), reads=[b_Vt, b_PT[pi]], writes=[b_nz[nzb][half]])

                def evac(blk):
                    st_, n = tokset(blk)
                    nzb = blk % 4
                    half = 0
                    co = 0
                    accv = Acc[:, :, st_:st_ + 127 * r + 1:r]
                    nzv = bank(4 + nzb)[:, co:co + 256].rearrange("p (a q) -> p a q", q=128)
                    if jb["first"]:
                        dve.do(lambda: nc.vector.tensor_copy(out=accv, in_=nzv), reads=[b_nz[nzb][half]], writes=[b_Acc])
                    else:
                        dve.do(lambda: nc.vector.tensor_tensor(out=accv, in0=nzv, in1=accv, op=ALU.add),
                               reads=[b_nz[nzb][half]], writes=[b_Acc])

                qk(0)
                qk(1)
                softmax(0)
                for blk in range(32):
                    if blk + 2 < 32:
                        qk(blk + 2)
                    if blk + 1 < 32:
                        softmax(blk + 1)
                    pv(blk)
                    evac(blk)
                if jb["last"]:
                    ys = ycount % 2
                    ycount += 1
                    if jb["sink"] is not None:
                        sk = jb["sink"]
                        dve.do(lambda sk=sk: nc.vector.tensor_scalar(out=Acc[:, 1, :], in0=Acc[:, 1, :], scalar1=es[:, sk:sk + 1], scalar2=None, op0=ALU.add),
                               reads=[b_es], writes=[b_Acc])
                    dve.do(lambda: nc.vector.reciprocal(out=Acc[:, 1, :], in_=Acc[:, 1, :]), reads=[], writes=[b_Acc])
                    dve.do(lambda ys=ys: nc.vector.tensor_tensor(out=Yt[ys][:], in0=Acc[:, 0, :], in1=Acc[:, 1, :], op=ALU.mult),
                           reads=[b_Acc], writes=[b_Yt[ys]])
                    row = jb["row"]
                    dma(sp, c_ys[ys], lambda ys=ys, row=row: nc.sync.dma_start(out=yT_s[row:row + 128, :], in_=Yt[ys][:]), reads=[b_Yt[ys]])

            convert_some(64)
            barrier()
            for h in (range(4) if (do_c and upto != "p0") else ()):
                s = h % 2
                c0 = 3072 + h * 128
                dma(pool, c_wt[s], lambda s=s, c0=c0: nc.gpsimd.dma_start(out=Wt[s][:, :, 0:128], in_=w_in_v[:, :, c0:c0 + 128]), writes=[b_Wt[s]])
                for tc in range(8):
                    pb = tc % 2
                    for kc in range(8):
                        pe.do(lambda kc=kc, tc=tc, pb=pb, s=s: nc.tensor.matmul(
                            bank(pb), lhsT=Wt[s][:, kc, 0:128], rhs=XT[:, kc, tc * 512:(tc + 1) * 512], start=(kc == 0), stop=(kc == 7)),
                            reads=[b_Wt[s], b_XTc[tc // 2]], writes=[b_ps[pb]])
                    act.do(lambda tc=tc, pb=pb: nc.scalar.activation(out=QT[:, tc * 512:(tc + 1) * 512], in_=bank(pb), func=AF.Identity),
                           reads=[b_ps[pb]], writes=[b_QT])
                ys = ycount % 2
                ycount += 1
                for tc in range(8):
                    for kb in range(2):
                        sbk = 2 + kb
                        pe.do(lambda kb=kb, tc=tc, sbk=sbk, h=h: nc.tensor.matmul(
                            bank(sbk), lhsT=KmT[:, h, kb * 128:(kb + 1) * 128], rhs=QT[:, tc * 512:(tc + 1) * 512], start=True, stop=True),
                            reads=[b_KmT, b_QT], writes=[b_ps[sbk]])
                        act.do(lambda kb=kb, sbk=sbk: nc.scalar.activation(out=PT[kb][:], in_=bank(sbk), func=AF.Exp, scale=float(128 ** -0.5)),
                               reads=[b_ps[sbk]], writes=[b_PT[kb]])
                    nb_ = 4 + (tc % 2) * 2
                    for kb in range(2):
                        pe.do(lambda kb=kb, nb_=nb_, h=h: nc.tensor.matmul(bank(nb_), lhsT=Vm[:, kb, h * 128:(h + 1) * 128], rhs=PT[kb][:],
                                                                        start=(kb == 0), stop=(kb == 1)), reads=[b_Vm, b_PT[kb]], writes=[b_ps[nb_]])
                    for kb in range(2):
                        pe.do(lambda kb=kb, nb_=nb_: nc.tensor.matmul(bank(nb_ + 1), lhsT=ones_b[:, :], rhs=PT[kb][:],
                                                                    start=(kb == 0), stop=(kb == 1)), reads=[b_PT[kb]], writes=[b_ps[nb_ + 1]])
                    pi = tc % 2
                    dve.do(lambda pi=pi, nb_=nb_: nc.vector.reciprocal(out=Pf[pi][:], in_=bank(nb_ + 1)), reads=[b_ps[nb_ + 1]], writes=[b_Pf[pi]])
                    dve.do(lambda pi=pi, nb_=nb_, tc=tc, ys=ys: nc.vector.tensor_tensor(out=Yt[ys][:, tc * 512:(tc + 1) * 512], in0=bank(nb_), in1=Pf[pi][:], op=ALU.mult),
                           reads=[b_ps[nb_], b_Pf[pi]], writes=[b_Yt[ys]])
                row = 768 + h * 128
                dma(sp, c_ys[ys], lambda ys=ys, row=row: nc.sync.dma_start(out=yT_s[row:row + 128, :], in_=Yt[ys][:]), reads=[b_Yt[ys]])
            barrier()
            st01.close()

            with ExitStack() as st2:
                sb2 = lambda n, s_, t: st2.enter_context(nc.sbuf_tensor("s_" + n, s_, t))
                Wg = sb2("Wg", [128, 8, 3072], BF16)
                Wbr = sb2("Wbr", [128, 10, D], BF16)
                bg = sb2("bg", [128, 24], F32)
                Ych = [sb2("Ych%d" % i, [128, 10, 512], BF16) for i in range(2)]
                mch = [sb2("mch%d" % i, [128, 8, 512], BF16) for i in range(2)]
                gt = [sb2("gt%d" % i, [128, 512], F32) for i in range(3)]
                tt_ = [sb2("tt%d" % i, [128, 512], F32) for i in range(3)]
                b_Wg = b_Wbr = b_bg = Buf()
                b_Ych, b_mch = [Buf(), Buf()], [Buf(), Buf()]
                b_gt = [Buf() for _ in range(3)]
                b_tt = [Buf() for _ in range(3)]
                b_ps = [Buf() for _ in range(8)]
                c_w2, c_ych, c_mst = chan("w2"), [chan("ych0"), chan("ych1")], [chan("mst0"), chan("mst1")]
                for br in range(3):
                    dma(pool, c_w2, lambda br=br: nc.gpsimd.dma_start(out=Wg[:, :, br * 1024:(br + 1) * 1024],
                                                                      in_=w_in_v[:, :, 3584 + br * 1024:3584 + (br + 1) * 1024]), writes=[b_Wg])
                dma(pool, c_w2, lambda: nc.gpsimd.dma_start(out=Wbr[:, 0:2, :], in_=w_a_d.rearrange("(kc p) n -> p kc n", p=128)), writes=[b_Wbr])
                dma(pool, c_w2, lambda: nc.gpsimd.dma_start(out=Wbr[:, 2:6, :], in_=w_b_d.rearrange("(kc p) n -> p kc n", p=128)), writes=[b_Wbr])
                dma(pool, c_w2, lambda: nc.gpsimd.dma_start(out=Wbr[:, 6:10, :], in_=w_c_d.rearrange("(kc p) n -> p kc n", p=128)), writes=[b_Wbr])
                dma(sp, c_w2, lambda: nc.sync.dma_start(out=bg[:], in_=bgate_d[:, :]), writes=[b_bg])
                yT_v = yT_s.rearrange("(c p) t -> p c t", p=128)
                mT_v = mT_s.rearrange("(c p) t -> p c t", p=128)
                brk = ((0, 2), (2, 6), (6, 10))

                def load_y(tc):
                    dma(sp, c_ych[tc % 2], lambda: nc.sync.dma_start(out=Ych[tc % 2][:], in_=yT_v[:, :, tc * 512:(tc + 1) * 512]),
                        writes=[b_Ych[tc % 2]])

                if upto not in ("p0", "p1"):
                    load_y(0)
                for tc in (range(8) if upto not in ("p0", "p1") else ()):
                    s = tc % 2
                    if tc + 1 < 8:
                        load_y(tc + 1)
                    tsl = slice(tc * 512, (tc + 1) * 512)
                    for f in range(8):
                        for br in range(3):
                            gb = br
                            for kc in range(8):
                                pe.do(lambda kc=kc, br=br, f=f, gb=gb: nc.tensor.matmul(
                                    bank(gb), lhsT=Wg[:, kc, br * 1024 + f * 128:br * 1024 + (f + 1) * 128], rhs=XT[:, kc, tsl],
                                    start=(kc == 0), stop=(kc == 7)), reads=[b_Wg, b_XTc[tc // 2]], writes=[b_ps[gb]])
                            act.do(lambda br=br, f=f, gb=gb: nc.scalar.activation(out=gt[br][:], in_=bank(gb), func=AF.Sigmoid,
                                                                                   bias=bg[:, br * 8 + f:br * 8 + f + 1]),
                                   reads=[b_ps[gb], b_bg], writes=[b_gt[br]])
                            k0, k1 = brk[br]
                            for kc in range(k0, k1):
                                pe.do(lambda kc=kc, br=br, f=f, k0=k0, k1=k1: nc.tensor.matmul(
                                    bank(3 + br), lhsT=Wbr[:, kc, f * 128:(f + 1) * 128], rhs=Ych[s][:, kc, :],
                                    start=(kc == k0), stop=(kc == k1 - 1)), reads=[b_Wbr, b_Ych[s]], writes=[b_ps[3 + br]])
                            dve.do(lambda br=br: nc.vector.tensor_tensor(out=tt_[br][:], in0=bank(3 + br), in1=gt[br][:], op=ALU.mult),
                                   reads=[b_ps[3 + br], b_gt[br]], writes=[b_tt[br]])
                        pool.do(lambda: nc.gpsimd.tensor_tensor(out=tt_[0][:], in0=tt_[0][:], in1=tt_[1][:], op=ALU.add),
                                reads=[b_tt[1]], writes=[b_tt[0]])
                        pool.do(lambda f=f: nc.gpsimd.tensor_tensor(out=mch[s][:, f, :], in0=tt_[0][:], in1=tt_[2][:], op=ALU.add),
                                reads=[b_tt[0], b_tt[2]], writes=[b_mch[s]])
                    dma(sp, c_mst[s], lambda s=s: nc.sync.dma_start(out=mT_v[:, :, tsl], in_=mch[s][:]), reads=[b_mch[s]])
                barrier()
        barrier()

        with ExitStack() as st:
            sb = lambda n, s_, t: st.enter_context(nc.sbuf_tensor("s_" + n, s_, t))
            Wout = sb("Wout", [128, 8, D], BF16)
            Wq = sb("Wq", [128, 8, D], BF16)
            skT = sb("skT", [128, 8, 128], F32)
            lnp = sb("lnp", [128, 4, D], F32)
            iota16 = sb("iota16", [128, 16], F32)
            mTc = [sb("mTc%d" % i, [128, 8, 512], BF16) for i in range(2)]
            xt = [sb("xt%d" % i, [128, D], F32) for i in range(2)]
            r1 = sb("r1", [128, D], F32)
            x1 = [sb("x1_%d" % i, [128, D], F32) for i in range(2)]
            x1T = sb("x1T", [128, 8, 128], BF16)
            qT = sb("qT", [128, 8, 128], F32)
            stats = sb("stats", [128, 2, 6], F32)
            mv = sb("mv", [128, 2], F32)
            rstd = sb("rstd", [128, 1], F32)
            nmr = sb("nmr", [128, 1], F32)
            sv = sb("sv", [128, 16, 16], F32)
            si_u = sb("si_u", [128, 16, 16], U32)
            si_f = sb("si_f", [128, 16, 16], F32)
            work = sb("work", [128, 128], F32)
            cand = sb("cand", [128, 8, 256], F32)
            work2 = sb("work2", [128, 256], F32)
            top = sb("top", [128, 8, 16], F32)
            pos_u = sb("pos_u", [128, 8, 16], U32)
            ab_u = sb("ab_u", [128, 2, 128], U32)
            ab_f = sb("ab_f", [128, 2, 128], F32)
            oh = sb("oh", [128, 8, 16, 16], BF16)
            ab_b = sb("ab_b", [128, 2, 128], BF16)
            si_b = sb("si_b", [128, 16, 16], BF16)
            iota_b = sb("iota_b", [128, 16], BF16)
            ij = sb("ij", [128, 2, 128], F32)
            eidx_f = sb("eidx_f", [128, 128], F32)
            eidx_u = [sb("eidx_u%d" % i, [128, 128], U32) for i in range(2)]
            gate = [sb("gate%d" % i, [128, 8, 16], F32) for i in range(2)]
            gsum = sb("gsum", [128, 8], F32)
            hpre = bank(4)[:, 0:128]
            gl = sb("gl", [128, 128], F32)
            x1b = [sb("x1b%d" % i, [128, D], BF16) for i in range(2)]
            prod = [sb("prod%d" % i, [128, D], BF16) for i in range(NP)]
            junk = sb("junk", [128, D], BF16)
            G = [sb("G%d" % i, [128, 2 * D], BF16) for i in range(NG)]
            diag = [sb("diag%d" % i, [128, 128], BF16) for i in range(ND)]
            ident_b = sb("ident_b", [128, 128], BF16)
            r2 = sb("r2", [128, D], F32)
            ot = [sb("ot%d" % i, [128, D], F32) for i in range(2)]
            b_w3 = Buf()
            b_mTc, b_xt, b_ot, b_x1, b_eu, b_gate = ([Buf(), Buf()] for _ in range(6))
            b_r1, b_x1T, b_qT, b_st, b_mv, b_rstd, b_nmr = (Buf() for _ in range(7))
            b_sv, b_siu, b_sif, b_work, b_cand, b_work2, b_top, b_pos = (Buf() for _ in range(8))
            b_abu, b_abf, b_oh, b_ij, b_ef, b_gsum, b_r2, b_idb = (Buf() for _ in range(8))
            b_G = [Buf() for _ in range(NG)]
            b_diag = [Buf() for _ in range(ND)]
            b_hp = [Buf() for _ in range(128)]
            b_gl = [Buf() for _ in range(128)]
            b_x1b = [Buf(), Buf()]
            b_prod = [Buf() for _ in range(NP)]
            b_ps = [Buf() for _ in range(8)]
            c_w3 = chan("w3")
            c_mtc, c_xt_, c_ot = [chan("mtc0"), chan("mtc1")], [chan("xt0"), chan("xt1")], [chan("ot0"), chan("ot1")]
            c_g = [chan("g%d" % i) for i in range(NG)]
            c_dbg = chan("dbg")
            dve.do(lambda: nc.vector.tensor_copy(out=ident_b[:], in_=ident[:]), reads=[b_const], writes=[b_idb])
            dma(pool, c_w3, lambda: nc.gpsimd.dma_start(out=Wout[:], in_=w_out_d.rearrange("(kc p) n -> p kc n", p=128)), writes=[b_w3])
            dma(pool, c_w3, lambda: nc.gpsimd.dma_start(out=Wq[:], in_=w_q_d.rearrange("(kc p) n -> p kc n", p=128)), writes=[b_w3])
            dma(sp, c_w3, lambda: nc.sync.dma_start(out=skT[:].rearrange("p a b -> p (a b)"), in_=skT_d[:, :]), writes=[b_w3])
            dma(sp, c_w3, lambda: nc.sync.dma_start(out=lnp[:].rearrange("p a b -> p (a b)"), in_=lnp_d[:, :]), writes=[b_w3])
            dma(sp, c_w3, lambda: nc.sync.dma_start(out=iota16[:], in_=iota_d[:, :]), writes=[b_w3])
            dve.do(lambda: nc.vector.tensor_copy(out=iota_b[:], in_=iota16[:]), reads=[b_w3], writes=[b_w3])
            mT_v = mT_s.rearrange("(c p) t -> p c t", p=128)
            accp = bank(6, 2)
            NTILES = S // 128 if ntiles is None else ntiles
            if upto != "all":
                NTILES = 0

            def layer_norm_steps(L, src, b_src, dst, b_dst, gi):
                def s_stats():
                    for hf in range(2):
                        dve.do(lambda hf=hf: nc.vector.bn_stats(out=stats[:, hf, :], in_=src[:, hf * 512:(hf + 1) * 512]), reads=[b_src], writes=[b_st])
                    dve.do(lambda: nc.vector.bn_aggr(out=mv[:], in_=stats[:].rearrange("p a b -> p (a b)")), reads=[b_st], writes=[b_mv])
                    act.do(lambda: nc.scalar.activation(out=rstd[:], in_=mv[:, 1:2], func=AF.Sqrt, bias=float(LN_EPS), scale=1.0), reads=[b_mv], writes=[b_rstd])
                def s_norm():
                    dve.do(lambda: nc.vector.reciprocal(out=rstd[:], in_=rstd[:]), reads=[], writes=[b_rstd])
                    dve.do(lambda: nc.vector.scalar_tensor_tensor(out=nmr[:], in0=mv[:, 0:1], scalar=-1.0, in1=rstd[:], op0=ALU.mult, op1=ALU.mult),
                           reads=[b_mv, b_rstd], writes=[b_nmr])
                    act.do(lambda: nc.scalar.activation(out=dst[:], in_=src[:], func=AF.Identity, bias=nmr[:, 0:1], scale=rstd[:, 0:1]),
                           reads=[b_src, b_rstd, b_nmr], writes=[b_dst])
                def s_g():
                    dve.do(lambda: nc.vector.tensor_tensor(out=dst[:], in0=dst[:], in1=lnp[:, gi, :], op=ALU.mult), reads=[b_w3], writes=[b_dst])
                def s_b():
                    dve.do(lambda: nc.vector.tensor_tensor(out=dst[:], in0=dst[:], in1=lnp[:, gi + 1, :], op=ALU.add), reads=[b_w3], writes=[b_dst])
                L.extend([s_stats, s_norm, s_g, s_b])

            def load_tile(tt):
                if tt >= NTILES:
                    return
                if tt % 4 == 0:
                    cc = (tt // 4) % 2
                    dma(sp, c_mtc[cc], lambda: nc.sync.dma_start(out=mTc[cc][:], in_=mT_v[:, :, (tt // 4) * 512:(tt // 4 + 1) * 512]),
                        writes=[b_mTc[cc]])
                dma(sp, c_xt_[tt % 2], lambda: nc.sync.dma_start(out=xt[tt % 2][:], in_=x_d[tt * 128:(tt + 1) * 128, :]), writes=[b_xt[tt % 2]])

            def topk16(L, src_fn, b_src, vals, b_vals, idxs, b_idxs, wk, b_wk):
                def f():
                    src = src_fn()
                    dve.do(lambda: nc.vector.max(out=vals[:, 0:8], in_=src), reads=[b_src], writes=[b_vals])
                    dve.do(lambda: nc.vector.max_index(out=idxs[:, 0:8], in_max=vals[:, 0:8], in_values=src), reads=[b_src, b_vals], writes=[b_idxs])
                    dve.do(lambda: nc.vector.match_replace(out=wk, in_to_replace=vals[:, 0:8], in_values=src, imm_value=-1e30),
                           reads=[b_src, b_vals], writes=[b_wk])
                    dve.do(lambda: nc.vector.max(out=vals[:, 8:16], in_=wk), reads=[b_wk], writes=[b_vals])
                    dve.do(lambda: nc.vector.max_index(out=idxs[:, 8:16], in_max=vals[:, 8:16], in_values=wk), reads=[b_wk, b_vals], writes=[b_idxs])
                L.append(f)

            def front_steps(tt):
                L = []
                if tt >= NTILES:
                    return L
                par = tt % 2
                cc = (tt // 4) % 2
                sub = tt % 4
                xs = tt % 2
                X1 = x1[par]
                bX1 = b_x1[par]
                for hf in range(2):
                    for f0 in (0, 4):
                        def s_wout(hf=hf, f0=f0):
                            for f in range(f0, f0 + 4):
                                pe.do(lambda f=f: nc.tensor.matmul(bank(hf), lhsT=mTc[cc][:, f, sub * 128:(sub + 1) * 128],
                                                                  rhs=Wout[:, f, hf * 512:(hf + 1) * 512], start=(f == 0), stop=(f == 7)),
                                      reads=[b_mTc[cc], b_w3], writes=[b_ps[hf]])
                        L.append(s_wout)
                L.append(lambda: dve.do(lambda: nc.vector.scalar_tensor_tensor(out=r1[:], in0=xt[xs][:], scalar=float(ALPHA), in1=bank(0, 2),
                                                                                op0=ALU.mult, op1=ALU.add),
                                        reads=[b_xt[xs], b_ps[0], b_ps[1]], writes=[b_r1]))
                layer_norm_steps(L, r1, b_r1, X1, bX1, 0)
                if debug:
                    L.append(lambda: dma(sp, c_dbg, lambda: nc.sync.dma_start(out=x1_dbg[tt * 128:(tt + 1) * 128, :], in_=X1[:]), reads=[bX1]))
                L.append(lambda: act.do(lambda: nc.scalar.activation(out=x1b[par][:], in_=X1[:], func=AF.Identity), reads=[bX1], writes=[b_x1b[par]]))
                for f0 in (0, 4):
                    def s_tr(f0=f0):
                        for f in range(f0, f0 + 4):
                            pe.do(lambda f=f: nc.tensor.transpose(out=bank(2, 2)[:, f * 128:(f + 1) * 128], in_=X1[:, f * 128:(f + 1) * 128], identity=ident[:]),
                                  reads=[bX1, b_const], writes=[b_ps[2 + f // 4]])
                    L.append(s_tr)
                L.append(lambda: act.do(lambda: nc.scalar.activation(out=x1T[:].rearrange("p a b -> p (a b)"), in_=bank(2, 2), func=AF.Identity),
                                        reads=[b_ps[2], b_ps[3]], writes=[b_x1T]))
                for h in range(8):
                    def s_q(h=h):
                        for kc in range(8):
                            pe.do(lambda kc=kc: nc.tensor.matmul(bank(0, 2)[:, h * 128:(h + 1) * 128], lhsT=Wq[:, kc, h * 128:(h + 1) * 128],
                                                                rhs=x1T[:, kc, :], start=(kc == 0), stop=(kc == 7)),
                                  reads=[b_w3, b_x1T], writes=[b_ps[h // 4]])
                    L.append(s_q)
                L.append(lambda: act.do(lambda: nc.scalar.activation(out=qT[:].rearrange("p a b -> p (a b)"), in_=bank(0, 2), func=AF.Identity),
                                        reads=[b_ps[0], b_ps[1]], writes=[b_qT]))
                for rnd, b0 in ((0, 2), (1, 0)):
                    def s_sc(rnd=rnd, b0=b0):
                        for hh in range(4):
                            h = rnd * 4 + hh
                            for c in range(2):
                                pr = slice(c * 64, (c + 1) * 64)
                                pe.do(lambda h=h, hh=hh, c=c, pr=pr: nc.tensor.matmul(bank(b0 + c)[:, hh * 128:(hh + 1) * 128], lhsT=qT[pr, h, :],
                                                                                    rhs=skT[pr, h, :], start=True, stop=True),
                                      reads=[b_qT, b_w3], writes=[b_ps[b0 + c]])
                    L.append(s_sc)
                    for hh in range(4):
                        for c in range(2):
                            hc = c * 8 + rnd * 4 + hh
                            topk16(L, (lambda b0=b0, c=c, hh=hh: bank(b0 + c)[:, hh * 128:(hh + 1) * 128]), b_ps[b0 + c],
                                   sv[:, hc, :], b_sv, si_u[:, hc, :], b_siu, work[:], b_work)
                sv4 = sv[:].rearrange("p (c h) k -> p c h k", c=2)
                si4 = si_b[:].rearrange("p (c h) k -> p c h k", c=2)

                def s_cand():
                    dve.do(lambda: nc.vector.tensor_copy(out=si_b[:], in_=si_u[:]), reads=[b_siu], writes=[b_sif])
                    dve.do(lambda: nc.vector.tensor_tensor(out=cand[:].rearrange("p h (a b) -> p h a b", b=16),
                                                           in0=sv4[:, 0, :, :].unsqueeze(3).broadcast_to([128, 8, 16, 16]),
                                                           in1=sv4[:, 1, :, :].unsqueeze(2).broadcast_to([128, 8, 16, 16]), op=ALU.add),
                           reads=[b_sv], writes=[b_cand])
                L.append(s_cand)
                for h in range(8):
                    topk16(L, (lambda h=h: cand[:, h, :]), b_cand, top[:, h, :], b_top, pos_u[:, h, :], b_pos, work2[:], b_work2)
                posf = pos_u[:].rearrange("p h k -> p (h k)")

                def s_ab():
                    dve.do(lambda: nc.vector.tensor_single_scalar(out=ab_u[:, 0, :], in_=posf, scalar=4, op=ALU.logical_shift_right), reads=[b_pos], writes=[b_abu])
                    dve.do(lambda: nc.vector.tensor_single_scalar(out=ab_u[:, 1, :], in_=posf, scalar=15, op=ALU.bitwise_and), reads=[b_pos], writes=[b_abu])
                    dve.do(lambda: nc.vector.tensor_copy(out=ab_b[:], in_=ab_u[:]), reads=[b_abu], writes=[b_abf])
                L.append(s_ab)
                for c in range(2):
                    def s_lk(c=c):
                        abv = ab_b[:, c, :].rearrange("p (h k) -> p h k", k=16)
                        dve.do(lambda: nc.vector.tensor_tensor(out=oh[:], in0=abv.unsqueeze(3).broadcast_to([128, 8, 16, 16]),
                                                               in1=iota_b[:].unsqueeze(1).unsqueeze(1).broadcast_to([128, 8, 16, 16]), op=ALU.is_equal),
                               reads=[b_abf, b_w3], writes=[b_oh])
                        dve.do(lambda: nc.vector.tensor_tensor(out=oh[:], in0=oh[:], in1=si4[:, c, :, :].unsqueeze(2).broadcast_to([128, 8, 16, 16]), op=ALU.mult),
                               reads=[b_sif], writes=[b_oh])
                        dve.do(lambda: nc.vector.tensor_reduce(out=ij[:, c, :], in_=oh[:].rearrange("p h k a -> p (h k) a"), axis=AX.X, op=ALU.add),
                               reads=[b_oh], writes=[b_ij])
                    L.append(s_lk)

                def s_eidx():
                    dve.do(lambda: nc.vector.scalar_tensor_tensor(out=eidx_f[:], in0=ij[:, 0, :], scalar=128.0, in1=ij[:, 1, :], op0=ALU.mult, op1=ALU.add),
                           reads=[b_ij], writes=[b_ef])
                    dve.do(lambda: nc.vector.tensor_copy(out=eidx_u[par][:], in_=eidx_f[:]), reads=[b_ef], writes=[b_eu[par]])
                L.append(s_eidx)
                GT = gate[par]
                bGT = b_gate[par]

                def s_gate1():
                    dve.do(lambda: nc.vector.tensor_tensor(out=GT[:], in0=top[:], in1=top[:, :, 0:1].broadcast_to([128, 8, 16]), op=ALU.subtract),
                           reads=[b_top], writes=[bGT])
                    act.do(lambda: nc.scalar.activation(out=GT[:], in_=GT[:], func=AF.Exp), reads=[], writes=[bGT])
                def s_gate2():
                    dve.do(lambda: nc.vector.tensor_reduce(out=gsum[:], in_=GT[:], axis=AX.X, op=ALU.add), reads=[bGT], writes=[b_gsum])
                    dve.do(lambda: nc.vector.reciprocal(out=gsum[:], in_=gsum[:]), reads=[], writes=[b_gsum])
                    dve.do(lambda: nc.vector.tensor_tensor(out=GT[:], in0=GT[:], in1=gsum[:].unsqueeze(2).broadcast_to([128, 8, 16]), op=ALU.mult),
                           reads=[b_gsum], writes=[bGT])
                L.extend([s_gate1, s_gate2])
                return L

            def slot_head(tt, s_):
                par = tt % 2
                g = s_ % NG
                pb = s_ % NP
                dma(pool, c_g[g], lambda: nc.gpsimd.indirect_dma_start(
                    out=G[g][:], out_offset=None, in_=uv_s[:, :], in_offset=bass.IndirectOffsetOnAxis(ap=eidx_u[par][:, s_:s_ + 1], axis=0)),
                    reads=[b_eu[par]], writes=[b_G[g]])
                dve.do(lambda: nc.vector.tensor_tensor(out=prod[pb][:], in0=G[g][:, 0:D], in1=x1b[par][:], op=ALU.mult),
                       reads=[b_G[g], b_x1b[par]], writes=[b_prod[pb]])
                act.do(lambda: nc.scalar.activation(out=junk[:], in_=prod[pb][:], func=AF.Identity, accum_out=hpre[:, s_:s_ + 1]),
                       reads=[b_prod[pb]], writes=[b_hp[s_]])
                act.do(lambda: nc.scalar.activation(out=gl[:, s_:s_ + 1], in_=hpre[:, s_:s_ + 1], func=AF.Gelu), reads=[b_hp[s_]], writes=[b_gl[s_]])

            def slot_tail(tt, s_):
                par = tt % 2
                g = s_ % NG
                d_ = s_ % ND
                gatef = gate[par][:].rearrange("p h k -> p (h k)")
                dve.do(lambda: nc.vector.tensor_scalar(out=diag[d_][:], in0=ident_b[:], scalar1=gl[:, s_:s_ + 1], scalar2=gatef[:, s_:s_ + 1],
                                                       op0=ALU.mult, op1=ALU.mult),
                       reads=[b_gl[s_], b_gate[par], b_idb], writes=[b_diag[d_]])
                for hf in range(2):
                    pe.do(lambda hf=hf: nc.tensor.matmul(bank(6 + hf), lhsT=diag[d_][:], rhs=G[g][:, D + hf * 512:D + (hf + 1) * 512],
                                                        start=(s_ == 0), stop=(s_ == 127)),
                          reads=[b_diag[d_], b_G[g]], writes=[b_ps[6 + hf]])

            def tail(tt):
                par = tt % 2
                os_ = tt % 2
                if debug:
                    dve.do(lambda: nc.vector.tensor_copy(out=r2[:], in_=accp), reads=[b_ps[6], b_ps[7]], writes=[b_r2])
                    dma(sp, c_dbg, lambda: nc.sync.dma_start(out=yp_dbg[tt * 128:(tt + 1) * 128, :], in_=r2[:]), reads=[b_r2])
                dve.do(lambda: nc.vector.scalar_tensor_tensor(out=r2[:], in0=x1[par][:], scalar=float(ALPHA), in1=accp, op0=ALU.mult, op1=ALU.add),
                       reads=[b_x1[par], b_ps[6], b_ps[7]], writes=[b_r2])
                L = []
                layer_norm_steps(L, r2, b_r2, ot[os_], b_ot[os_], 2)
                for f in L:
                    f()
                dma(sp, c_ot[os_], lambda: nc.sync.dma_start(out=out_d[tt * 128:(tt + 1) * 128, :], in_=ot[os_][:]), reads=[b_ot[os_]])

            load_tile(0)
            load_tile(1)
            for f in front_steps(0):
                f()
            for tt in range(NTILES):
                load_tile(tt + 2)
                nxt = front_steps(tt + 1)
                k = 0
                for s_ in range(128 + LAG):
                    if s_ < 128:
                        slot_head(tt, s_)
                    if s_ >= LAG:
                        slot_tail(tt, s_ - LAG)
                    if s_ < 128:
                        tgt = ((s_ + 1) * len(nxt)) // 128
                        while k < tgt:
                            nxt[k]()
                            k += 1
                tail(tt)
            barrier()
    return nc


_NC_CACHE = {}


def _host_inputs(x, mem, rel_bias, w_in, b_gate, w_mem_kv, sinks, w_branch_a, w_branch_b, w_branch_c, w_out, ln1_g, ln1_b,
                 peer_w_query, peer_sub_keys, peer_u, peer_v, ln2_g, ln2_b):
    f = lambda a: np.ascontiguousarray(np.asarray(a, dtype=np.float32))
    pairs, bucket, mask, heads = _bias_tables()
    rb = np.asarray(rel_bias, np.float32)
    biasT = np.zeros((128, 10, 4, 128), np.float32)
    maskT = np.zeros((128, 10, 4, 128), np.float32)
    for p in range(10):
        for hh in range(2):
            for kb in range(2):
                biasT[:, p, 2 * hh + kb, :] = rb[bucket[p, kb], heads[p] + hh]
                maskT[:, p, 2 * hh + kb, :] = mask[p, kb]
    sk = np.asarray(sinks, np.float32)[0]
    sinksP = np.zeros((128, 4), np.float32)
    for p in range(4):
        sinksP[0:64, p] = sk[2 * p]
        sinksP[64:128, p] = sk[2 * p + 1]
    lnp = np.stack([np.asarray(a, np.float32)[0] for a in (ln1_g, ln1_b, ln2_g, ln2_b)], 0)
    lnp = np.ascontiguousarray(np.broadcast_to(lnp.reshape(1, 4 * D), (128, 4 * D)))
    skT = np.asarray(peer_sub_keys, np.float32)[0].transpose(1, 3, 0, 2).reshape(128, 8 * 128)
    shared = {
        "w_in": f(w_in[0]), "w_mem_kv": f(w_mem_kv[0]), "w_a": f(w_branch_a[0]), "w_b": f(w_branch_b[0]), "w_c": f(w_branch_c[0]),
        "w_out": f(w_out[0]), "w_q": f(peer_w_query[0]), "skT": f(skT), "peer_u": f(peer_u[0]), "peer_v": f(peer_v[0]),
        "biasT": f(biasT.reshape(128, 5120)), "maskT": f(maskT.reshape(128, 5120)),
        "bgate": f(np.asarray(b_gate, np.float32)[0].reshape(24, 128).T), "sinksP": f(sinksP), "lnp": f(lnp),
        "ident": np.eye(128, dtype=np.float32), "iota16": f(np.broadcast_to(np.arange(16, dtype=np.float32), (128, 16))),
    }
    x = np.asarray(x, np.float32)
    mem = np.asarray(mem, np.float32)
    in_maps = []
    for b in range(x.shape[0]):
        m = dict(shared)
        m["x"] = f(x[b])
        m["xT"] = f(x[b].T)
        m["memT"] = f(mem[b].T)
        in_maps.append(m)
    return in_maps


def kernel(**inputs):
    in_maps = _host_inputs(**inputs)
    if "nc" not in _NC_CACHE:
        _NC_CACHE["nc"] = build_nc()
    nc = _NC_CACHE["nc"]
    res = run_bass_kernel_spmd(nc, in_maps, core_ids=list(range(NCORES)))
    return np.stack([np.asarray(r["out"], dtype=np.float32) for r in res.results], 0)
```

```python
import numpy as np
import concourse.bass as bass
import concourse.mybir as mybir
from concourse.bass_utils import run_bass_kernel_spmd
from contextlib import ExitStack

F32 = mybir.dt.float32
BF16 = mybir.dt.bfloat16
U32 = mybir.dt.uint32
AF = mybir.ActivationFunctionType
ALU = mybir.AluOpType
AX = mybir.AxisListType

S = 4096
D = 1024
NCORES = 8
ALPHA = 2.0 ** 0.25
LN_EPS = 1e-5
NEG = -30000.0
N_EXP = 16384
NG = 12
ND = 4
LAG = 3
NP = 4


class Buf:
    __slots__ = ("w", "r")

    def __init__(self):
        self.w = None
        self.r = {}


class Q:
    def __init__(self, nc, eng, st, name, is_pe=False):
        self.nc = nc
        self.eng = eng
        self.sem = st.enter_context(nc.semaphore("q_" + name))
        self.n = 0
        self.seen = {}
        self.is_pe = is_pe

    def wait(self, ev):
        if ev is None:
            return
        sem, val = ev
        if sem is self.sem and self.is_pe:
            return
        k = id(sem)
        if self.seen.get(k, -1) >= val:
            return
        self.eng.wait_ge(sem, val)
        self.seen[k] = val

    def deps(self, reads, writes, extra, skip_sem=None):
        for b in reads:
            if b.w is not None and b.w[0] is not skip_sem:
                self.wait(b.w)
        for b in writes:
            if b.w is not None and b.w[0] is not skip_sem:
                self.wait(b.w)
            for ev in b.r.values():
                self.wait(ev)
        for ev in extra:
            self.wait(ev)

    @staticmethod
    def mark(ev, reads, writes):
        for b in reads:
            k = id(ev[0])
            old = b.r.get(k)
            if old is None or old[1] < ev[1]:
                b.r[k] = ev
        for b in writes:
            b.w = ev
            b.r = {}

    def do(self, fn, reads=(), writes=(), extra=()):
        self.deps(reads, writes, extra)
        ins = fn()
        self.n += 1
        ins.then_inc(self.sem, 1)
        ev = (self.sem, self.n)
        self.mark(ev, reads, writes)
        return ev


class DmaChan:
    def __init__(self, nc, st, name):
        self.sem = st.enter_context(nc.semaphore("c_" + name))
        self.n = 0


def dma(q, chan, fn, reads=(), writes=(), extra=()):
    q.deps(reads, writes, extra, skip_sem=chan.sem)
    ins = fn()
    chan.n += 16
    ins.then_inc(chan.sem, 16)
    ev = (chan.sem, chan.n)
    Q.mark(ev, reads, writes)
    return ev


class SpQ(Q):
    def __init__(self, nc, eng):
        self.nc = nc
        self.eng = eng
        self.sem = None
        self.n = 0
        self.seen = {}
        self.is_pe = False


def _t5_bucket(dist):
    n = np.asarray(dist, dtype=np.int32)
    max_exact = 16
    nf = np.maximum(n, 1).astype(np.float32)
    scale = np.float32(np.log(2048 / max_exact))
    large = max_exact + (np.log(nf / np.float32(max_exact)) / scale * np.float32(32 - max_exact)).astype(np.int32)
    large = np.minimum(large, 31)
    return np.where(n < max_exact, n, large).astype(np.int32)


def _bias_tables():
    i = np.arange(128)[None, :]
    j = np.arange(128)[:, None]
    off_prev = 128 + i - j
    off_cur = i - j
    pairs = []
    for g, r in enumerate((1, 4, 16)):
        for m in range(2):
            pairs.append(("A", r, g * 4 + 2 * m))
    for m in range(4):
        pairs.append(("B", 1, 12 + 2 * m))
    bucket = np.zeros((10, 2, 128, 128), np.int32)
    mask = np.zeros((10, 2, 128, 128), np.float32)
    heads = []
    for p, (kind, r, h0) in enumerate(pairs):
        W = 128 if kind == "A" else 127
        for kb, off in enumerate((off_prev, off_cur)):
            bucket[p, kb] = _t5_bucket(np.clip(off, 0, W) * r)
            valid = (off >= 0) & (off <= W)
            mask[p, kb] = np.where(valid, 0.0, NEG)
        heads.append(h0)
    return pairs, bucket, mask, heads


def build_nc(debug=False, upto="all", ntiles=None, njobs=None, do_c=True, stage=99, jobsel=None):
    nc = bass.Bass("TRN2", target_bir_lowering=False)
    dt_in = lambda n, s, t=F32: nc.dram_tensor(n, s, t, kind="ExternalInput").ap()
    xT_d = dt_in("xT", [D, S])
    x_d = dt_in("x", [S, D])
    memT_d = dt_in("memT", [D, 256])
    w_in_d = dt_in("w_in", [D, 6656])
    w_kv_d = dt_in("w_mem_kv", [D, 1024])
    w_a_d = dt_in("w_a", [256, D])
    w_b_d = dt_in("w_b", [512, D])
    w_c_d = dt_in("w_c", [512, D])
    w_out_d = dt_in("w_out", [D, D])
    w_q_d = dt_in("w_q", [D, D])
    skT_d = dt_in("skT", [128, 8 * 128])
    pu_d = dt_in("peer_u", [N_EXP, D])
    pv_d = dt_in("peer_v", [N_EXP, D])
    biasT_d = dt_in("biasT", [128, 10 * 512])
    maskT_d = dt_in("maskT", [128, 10 * 512])
    bgate_d = dt_in("bgate", [128, 24])
    sinks_d = dt_in("sinksP", [128, 4])
    lnp_d = dt_in("lnp", [128, 4 * D])
    ident_d = dt_in("ident", [128, 128])
    iota_d = dt_in("iota16", [128, 16])
    out_d = nc.dram_tensor("out", [S, D], F32, kind="ExternalOutput").ap()
    skind = "ExternalOutput" if debug else "Internal"
    yT_s = nc.dram_tensor("yT_scr", [1280, S], BF16, kind=skind).ap()
    mT_s = nc.dram_tensor("mT_scr", [D, S], BF16, kind=skind).ap()
    uv_s = nc.dram_tensor("uv_scr", [N_EXP, 2 * D], BF16, kind="Internal").ap()
    if debug:
        x1_dbg = nc.dram_tensor("x1_dbg", [S, D], F32, kind="ExternalOutput").ap()
        yp_dbg = nc.dram_tensor("yp_dbg", [S, D], F32, kind="ExternalOutput").ap()

    with ExitStack() as gst:
        pe = Q(nc, nc.tensor, gst, "pe", is_pe=True)
        act = Q(nc, nc.scalar, gst, "act")
        dve = Q(nc, nc.vector, gst, "dve")
        pool = Q(nc, nc.gpsimd, gst, "pool")
        sp = SpQ(nc, nc.sync)
        queues = [pe, act, dve, pool, sp]
        chans = []

        def chan(name):
            c = DmaChan(nc, gst, name)
            chans.append(c)
            return c

        def barrier(skip=()):
            for q in queues:
                for o in (pe, act, dve, pool):
                    if o is not q and o.n > 0:
                        q.wait((o.sem, o.n))
                for c in chans:
                    if c.n > 0 and c not in skip:
                        q.wait((c.sem, c.n))

        psall = gst.enter_context(nc.psum_tensor("psall", [128, 4096], F32))

        def bank(i, n=1):
            return psall[:, i * 512:(i + n) * 512]

        ident = gst.enter_context(nc.sbuf_tensor("s_ident", [128, 128], F32))
        ones_b = gst.enter_context(nc.sbuf_tensor("s_ones_b", [128, 128], BF16))
        c_const = chan("const")
        b_const = Buf()
        dma(sp, c_const, lambda: nc.sync.dma_start(out=ident[:], in_=ident_d[:, :]), writes=[b_const])
        dve.do(lambda: nc.vector.memset(ones_b[:], 1.0), writes=[b_const])

        w_in_v = w_in_d.rearrange("(kc p) n -> p kc n", p=128)

        c_cv = chan("cv")
        cv_list = [(tab, c0, c) for (tab, c0) in ((pu_d, 0), (pv_d, D)) for c in range(16)]

        def convert_some(k):
            for _ in range(k):
                if not cv_list:
                    return
                tab, c0, c = cv_list.pop(0)
                src = tab[c * 1024:(c + 1) * 1024, :].rearrange("(p r) d -> p r d", p=128)
                dst = uv_s[c * 1024:(c + 1) * 1024, c0:c0 + D].rearrange("(p r) d -> p r d", p=128)
                dma(pool, c_cv, lambda: nc.gpsimd.dma_start(out=dst, in_=src))

        with ExitStack() as st:
            sb = lambda n, s, t: st.enter_context(nc.sbuf_tensor("s_" + n, s, t))
            XT = sb("XT", [128, 8, S], BF16)
            b_XTc = [Buf() for _ in range(4)]
            c_xtc = [chan("xc%d" % i) for i in range(4)]
            st01 = st.enter_context(ExitStack())
            sb1 = lambda n, s, t: st01.enter_context(nc.sbuf_tensor("s_" + n, s, t))
            BT = sb1("BT", [128, 10, 512], BF16)
            es = sb1("es", [128, 4], F32)
            KmT = sb1("KmT", [128, 4, 256], BF16)
            Vm = sb1("Vm", [128, 2, 512], BF16)
            b_BT, b_es, b_KmT, b_Vm = Buf(), Buf(), Buf(), Buf()
            b_ps = [Buf() for _ in range(8)]
            c_misc = chan("misc")

            Wt = [sb1("Wt%d" % i, [128, 8, 384], BF16) for i in range(2)]
            b_Wt = [Buf(), Buf()]
            c_wt, c_ys = [chan("wt0"), chan("wt1")], [chan("ys0"), chan("ys1")]
            jobs = []
            for m in range(2):
                for g, r in enumerate((1, 4, 16)):
                    c = g * 256 + 2 * m * 64
                    jobs.append(dict(q=c, k=(768 + c, 768 + c + 64), v=(1536 + c, 1536 + c + 64), r=r, bp=g * 2 + m,
                                     first=(g == 0), last=(g == 2), sink=None, row=m * 128))
            for kv in range(2):
                for m in range(2):
                    hq = kv * 4 + 2 * m
                    jobs.append(dict(q=2304 + hq * 64, k=(2816 + kv * 64,) * 2, v=(2944 + kv * 64,) * 2, r=1, bp=6 + kv * 2 + m,
                                     first=True, last=True, sink=kv * 2 + m, row=256 + (kv * 2 + m) * 128))

            def load_w(ji):
                jb = jobs[ji]
                s = ji % 2
                dma(pool, c_wt[s], lambda: nc.gpsimd.dma_start(out=Wt[s][:, :, 0:128], in_=w_in_v[:, :, jb["q"]:jb["q"] + 128]),
                    writes=[b_Wt[s]])
                for t, key in ((0, "k"), (1, "v")):
                    for hh in range(2):
                        c0 = jb[key][hh]
                        o0 = 128 + t * 128 + hh * 64
                        dma(pool, c_wt[s], lambda c0=c0, o0=o0: nc.gpsimd.dma_start(out=Wt[s][:, :, o0:o0 + 64], in_=w_in_v[:, :, c0:c0 + 64]),
                            writes=[b_Wt[s]])


            xT_v = xT_d.rearrange("(kc p) t -> p kc t", p=128)
            with ExitStack() as st0:
                sb0 = lambda n, s, t: st0.enter_context(nc.sbuf_tensor("s_" + n, s, t))
                bt_f = sb0("bt_f", [128, 5120], F32)
                mk_f = sb0("mk_f", [128, 5120], F32)
                sk_in = sb0("sk_in", [128, 4], F32)
                memTb = sb0("memTb", [128, 8, 256], BF16)
                Wkv = sb0("Wkv", [128, 8, 1024], BF16)
                b_btf = b_mkf = b_skin = b_memTb = b_Wkv = Buf()
                dma(sp, c_misc, lambda: nc.sync.dma_start(out=bt_f[:], in_=biasT_d[:, :]), writes=[b_btf])
                dma(sp, c_misc, lambda: nc.sync.dma_start(out=mk_f[:], in_=maskT_d[:, :]), writes=[b_mkf])
                dma(sp, c_misc, lambda: nc.sync.dma_start(out=sk_in[:], in_=sinks_d[:, :]), writes=[b_skin])
                dma(pool, c_misc, lambda: nc.gpsimd.dma_start(out=memTb[:], in_=memT_d.rearrange("(kc p) m -> p kc m", p=128)),
                    writes=[b_memTb])
                dma(pool, c_misc, lambda: nc.gpsimd.dma_start(out=Wkv[:], in_=w_kv_d.rearrange("(kc p) n -> p kc n", p=128)),
                    writes=[b_Wkv])
                if upto != "p0" and njobs != 0:
                    load_w(0)
                for i in range(4):
                    dma(pool, c_xtc[i], lambda i=i: nc.gpsimd.dma_start(out=XT[:, :, i * 1024:(i + 1) * 1024],
                                                                      in_=xT_v[:, :, i * 1024:(i + 1) * 1024]), writes=[b_XTc[i]])
                dve.do(lambda: nc.vector.tensor_tensor(out=BT[:].rearrange("p a b -> p (a b)"), in0=bt_f[:], in1=mk_f[:], op=ALU.add),
                       reads=[b_btf, b_mkf], writes=[b_BT])
                act.do(lambda: nc.scalar.activation(out=es[:], in_=sk_in[:], func=AF.Exp), reads=[b_skin], writes=[b_es])
                for h in range(4):
                    pb = h % 2
                    for kc in range(8):
                        pe.do(lambda h=h, kc=kc, pb=pb: nc.tensor.matmul(bank(pb)[:, 0:256], lhsT=Wkv[:, kc, h * 128:(h + 1) * 128],
                                                                          rhs=memTb[:, kc, :], start=(kc == 0), stop=(kc == 7)),
                              reads=[b_Wkv, b_memTb], writes=[b_ps[pb]])
                    act.do(lambda h=h, pb=pb: nc.scalar.activation(out=KmT[:, h, :], in_=bank(pb)[:, 0:256], func=AF.Identity),
                           reads=[b_ps[pb]], writes=[b_KmT])
                for kb in range(2):
                    for kc in range(8):
                        pe.do(lambda kb=kb, kc=kc: nc.tensor.matmul(bank(kb), lhsT=memTb[:, kc, kb * 128:(kb + 1) * 128],
                                                                     rhs=Wkv[:, kc, 512:1024], start=(kc == 0), stop=(kc == 7)),
                              reads=[b_Wkv, b_memTb], writes=[b_ps[kb]])
                    act.do(lambda kb=kb: nc.scalar.activation(out=Vm[:, kb, :], in_=bank(kb), func=AF.Identity),
                           reads=[b_ps[kb]], writes=[b_Vm])
                barrier(skip=c_xtc + c_wt)

            QT = sb1("QT", [128, S], BF16)
            KT = sb1("KT", [128, S], BF16)
            Vt = sb1("Vt", [128, 32, 128], BF16)
            Acc = sb1("Acc", [128, 2, S], F32)
            Yt = [sb1("Yt%d" % i, [128, S], BF16) for i in range(2)]
            Pf = [sb1("Pf%d" % i, [128, 512], F32) for i in range(2)]
            PT = [sb1("PT%d" % i, [128, 512], BF16) for i in range(2)]
            b_QT, b_KT, b_Vt, b_Acc = Buf(), Buf(), Buf(), Buf()
            b_Yt = [Buf(), Buf()]
            b_Pf = [Buf(), Buf()]
            b_PT = [Buf(), Buf()]
            b_ps = [Buf() for _ in range(8)]
            b_nz = [[Buf(), Buf()] for _ in range(4)]
            b_S = [Buf(), Buf()]

            if njobs is not None:
                jobs = jobs[:njobs]
            if jobsel is not None:
                jobs = [jobs[i] for i in jobsel]
            if upto == "p0":
                jobs = []
            ycount = 0
            for ji, jb in enumerate(jobs):
                s = ji % 2
                if ji + 1 < len(jobs):
                    load_w(ji + 1)
                convert_some(4)
                r = jb["r"]
                nb = 32 // r
                for which, dst, bdst in ((0, QT, b_QT), (1, KT, b_KT)):
                    for tc in range(8):
                        pb = tc % 2
                        for kc in range(8):
                            pe.do(lambda kc=kc, tc=tc, pb=pb, which=which: nc.tensor.matmul(
                                bank(pb), lhsT=Wt[s][:, kc, which * 128:(which + 1) * 128], rhs=XT[:, kc, tc * 512:(tc + 1) * 512],
                                start=(kc == 0), stop=(kc == 7)), reads=[b_Wt[s], b_XTc[tc // 2]], writes=[b_ps[pb]])
                        act.do(lambda tc=tc, pb=pb, dst=dst: nc.scalar.activation(out=dst[:, tc * 512:(tc + 1) * 512], in_=bank(pb), func=AF.Identity),
                               reads=[b_ps[pb]], writes=[bdst])

                def tokset(blk):
                    rho, n = blk // nb, blk % nb
                    st_ = r * 128 * n + rho
                    return st_, n

                for bg in (range(8) if stage >= 2 else ()):
                    pb = bg % 2
                    for j in range(4):
                        blk = bg * 4 + j
                        st_, n = tokset(blk)
                        for kc in range(8):
                            pe.do(lambda kc=kc, j=j, pb=pb, st_=st_: nc.tensor.matmul(
                                bank(pb)[:, j * 128:(j + 1) * 128], lhsT=XT[:, kc, st_:st_ + 127 * r + 1:r], rhs=Wt[s][:, kc, 256:384],
                                start=(kc == 0), stop=(kc == 7)), reads=[b_Wt[s]] + b_XTc, writes=[b_ps[pb]])
                    act.do(lambda bg=bg, pb=pb: nc.scalar.activation(out=Vt[:, bg * 4:(bg + 1) * 4, :].rearrange("p a b -> p (a b)"),
                                                                   in_=bank(pb), func=AF.Identity), reads=[b_ps[pb]], writes=[b_Vt])
                bp = jb["bp"]

                def qk(blk):
                    st_, n = tokset(blk)
                    pi = blk % 2
                    qs = slice(st_, st_ + 127 * r + 1, r)
                    ks_c = qs
                    ks_p = slice(st_ - 128 * r, st_ - r + 1, r)
                    sb0 = 2 if pi == 0 else 0
                    bS = [b_ps[sb0], b_ps[sb0 + 1]]
                    for hh in range(2):
                        ps_ = slice(hh * 64, (hh + 1) * 64)
                        if n > 0:
                            pe.do(lambda: nc.tensor.matmul(
                                bank(sb0 + hh)[:, 0:128], lhsT=KT[ps_, ks_p], rhs=QT[ps_, qs], start=True, stop=True),
                                reads=[b_KT, b_QT], writes=bS)
                        pe.do(lambda: nc.tensor.matmul(
                            bank(sb0 + hh)[:, 128:256], lhsT=KT[ps_, ks_c], rhs=QT[ps_, qs], start=True, stop=True),
                            reads=[b_KT, b_QT], writes=bS)

                def softmax(blk):
                    st_, n = tokset(blk)
                    pi = blk % 2
                    sb0 = 2 if pi == 0 else 0
                    bS = [b_ps[sb0], b_ps[sb0 + 1]]
                    Sv = bank(sb0, 2).rearrange("p (b c) -> p b c", c=512)[:, :, 0:256]
                    h3 = lambda ap: ap.rearrange("p (b c) -> p b c", c=256)
                    if n > 0:
                        dve.do(lambda: nc.vector.scalar_tensor_tensor(
                            out=h3(Pf[pi][:]), in0=Sv, scalar=0.125, in1=h3(BT[:, bp, :]), op0=ALU.mult, op1=ALU.add),
                            reads=bS + [b_BT], writes=[b_Pf[pi]])
                        act.do(lambda: nc.scalar.activation(out=PT[pi][:], in_=Pf[pi][:], func=AF.Exp),
                               reads=[b_Pf[pi]], writes=[b_PT[pi]])
                    else:
                        dve.do(lambda: nc.vector.scalar_tensor_tensor(
                            out=h3(Pf[pi][:])[:, :, 128:256], in0=Sv[:, :, 128:256], scalar=0.125, in1=h3(BT[:, bp, :])[:, :, 128:256],
                            op0=ALU.mult, op1=ALU.add), reads=bS + [b_BT], writes=[b_Pf[pi]])
                        act.do(lambda: nc.scalar.activation(out=h3(PT[pi][:])[:, :, 128:256], in_=h3(Pf[pi][:])[:, :, 128:256], func=AF.Exp),
                               reads=[b_Pf[pi]], writes=[b_PT[pi]])

                def pv(blk):
                    st_, n = tokset(blk)
                    pi = blk % 2
                    nzb = blk % 4
                    half = 0
                    co = 0
                    for hh in range(2):
                        ps_ = slice(hh * 64, (hh + 1) * 64)
                        for (oc, lhs_fn) in ((co, lambda b_: Vt[:, b_, ps_]), (co + 128, lambda b_: ones_b[:, 0:64])):
                            if n > 0:
                                pe.do(lambda: nc.tensor.matmul(
                                    bank(4 + nzb)[ps_, oc:oc + 128], lhsT=lhs_fn(blk - 1), rhs=PT[pi][:, (2 * hh) * 128:(2 * hh + 1) * 128],
                                    start=True, stop=False), reads=[b_Vt, b_PT[pi]], writes=[b_nz[nzb][half]])
                            pe.do(lambda: nc.tensor.matmul(
                                bank(4 + nzb)[ps_, oc:oc + 128], lhsT=lhs_fn(blk), rhs=PT[pi][:, (2 * hh + 1) * 128:(2 * hh + 2) * 128],
                                start=(n == 0), stop=True), reads=[b_Vt, b_PT[pi]], writes=[b_nz[nzb][half]])

                def evac(blk):
                    st_, n = tokset(blk)
                    nzb = blk % 4
                    half = 0
                    co = 0
                    accv = Acc[:, :, st_:st_ + 127 * r + 1:r]
                    nzv = bank(4 + nzb)[:, co:co + 256].rearrange("p (a q) -> p a q", q=128)
                    if jb["first"]:
                        dve.do(lambda: nc.vector.tensor_copy(out=accv, in_=nzv), reads=[b_nz[nzb][half]], writes=[b_Acc])
                    else:
                        dve.do(lambda: nc.vector.tensor_tensor(out=accv, in0=nzv, in1=accv, op=ALU.add),
                               reads=[b_nz[nzb][half]], writes=[b_Acc])

                qk(0)
                qk(1)
                softmax(0)
                for blk in range(32):
                    if blk + 2 < 32:
                        qk(blk + 2)
                    if blk + 1 < 32:
                        softmax(blk + 1)
                    pv(blk)
                    evac(blk)
                if jb["last"]:
                    ys = ycount % 2
                    ycount += 1
                    if jb["sink"] is not None:
                        sk = jb["sink"]
                        dve.do(lambda sk=sk: nc.vector.tensor_scalar(out=Acc[:, 1, :], in0=Acc[:, 1, :], scalar1=es[:, sk:sk + 1], scalar2=None, op0=ALU.add),
                               reads=[b_es], writes=[b_Acc])
                    dve.do(lambda: nc.vector.reciprocal(out=Acc[:, 1, :], in_=Acc[:, 1, :]), reads=[], writes=[b_Acc])
                    dve.do(lambda ys=ys: nc.vector.tensor_tensor(out=Yt[ys][:], in0=Acc[:, 0, :], in1=Acc[:, 1, :], op=ALU.mult),
                           reads=[b_Acc], writes=[b_Yt[ys]])
                    row = jb["row"]
                    dma(sp, c_ys[ys], lambda ys=ys, row=row: nc.sync.dma_start(out=yT_s[row:row + 128, :], in_=Yt[ys][:]), reads=[b_Yt[ys]])

            convert_some(64)
            barrier()
            for h in (range(4) if (do_c and upto != "p0") else ()):
                s = h % 2
                c0 = 3072 + h * 128
                dma(pool, c_wt[s], lambda s=s, c0=c0: nc.gpsimd.dma_start(out=Wt[s][:, :, 0:128], in_=w_in_v[:, :, c0:c0 + 128]), writes=[b_Wt[s]])
                for tc in range(8):
                    pb = tc % 2
                    for kc in range(8):
                        pe.do(lambda kc=kc, tc=tc, pb=pb, s=s: nc.tensor.matmul(
                            bank(pb), lhsT=Wt[s][:, kc, 0:128], rhs=XT[:, kc, tc * 512:(tc + 1) * 512], start=(kc == 0), stop=(kc == 7)),
                            reads=[b_Wt[s], b_XTc[tc // 2]], writes=[b_ps[pb]])
                    act.do(lambda tc=tc, pb=pb: nc.scalar.activation(out=QT[:, tc * 512:(tc + 1) * 512], in_=bank(pb), func=AF.Identity),
                           reads=[b_ps[pb]], writes=[b_QT])
                ys = ycount % 2
                ycount += 1
                for tc in range(8):
                    for kb in range(2):
                        sbk = 2 + kb
                        pe.do(lambda kb=kb, tc=tc, sbk=sbk, h=h: nc.tensor.matmul(
                            bank(sbk), lhsT=KmT[:, h, kb * 128:(kb + 1) * 128], rhs=QT[:, tc * 512:(tc + 1) * 512], start=True, stop=True),
                            reads=[b_KmT, b_QT], writes=[b_ps[sbk]])
                        act.do(lambda kb=kb, sbk=sbk: nc.scalar.activation(out=PT[kb][:], in_=bank(sbk), func=AF.Exp, scale=float(128 ** -0.5)),
                               reads=[b_ps[sbk]], writes=[b_PT[kb]])
                    nb_ = 4 + (tc % 2) * 2
                    for kb in range(2):
                        pe.do(lambda kb=kb, nb_=nb_, h=h: nc.tensor.matmul(bank(nb_), lhsT=Vm[:, kb, h * 128:(h + 1) * 128], rhs=PT[kb][:],
                                                                        start=(kb == 0), stop=(kb == 1)), reads=[b_Vm, b_PT[kb]], writes=[b_ps[nb_]])
                    for kb in range(2):
                        pe.do(lambda kb=kb, nb_=nb_: nc.tensor.matmul(bank(nb_ + 1), lhsT=ones_b[:, :], rhs=PT[kb][:],
                                                                    start=(kb == 0), stop=(kb == 1)), reads=[b_PT[kb]], writes=[b_ps[nb_ + 1]])
                    pi = tc % 2
                    dve.do(lambda pi=pi, nb_=nb_: nc.vector.reciprocal(out=Pf[pi][:], in_=bank(nb_ + 1)), reads=[b_ps[nb_ + 1]], writes=[b_Pf[pi]])
                    dve.do(lambda pi=pi, nb_=nb_, tc=tc, ys=ys: nc.vector.tensor_tensor(out=Yt[ys][:, tc * 512:(tc + 1) * 512], in0=bank(nb_), in1=Pf[pi][:], op=ALU.mult),
                           reads=[b_ps[nb_], b_Pf[pi]], writes=[b_Yt[ys]])
                row = 768 + h * 128
                dma(sp, c_ys[ys], lambda ys=ys, row=row: nc.sync.dma_start(out=yT_s[row:row + 128, :], in_=Yt[ys][:]), reads=[b_Yt[ys]])
            barrier()
            st01.close()

            with ExitStack() as st2:
                sb2 = lambda n, s_, t: st2.enter_context(nc.sbuf_tensor("s_" + n, s_, t))
                Wg = sb2("Wg", [128, 8, 3072], BF16)
                Wbr = sb2("Wbr", [128, 10, D], BF16)
                bg = sb2("bg", [128, 24], F32)
                Ych = [sb2("Ych%d" % i, [128, 10, 512], BF16) for i in range(2)]
                mch = [sb2("mch%d" % i, [128, 8, 512], BF16) for i in range(2)]
                gt = [sb2("gt%d" % i, [128, 512], F32) for i in range(3)]
                tt_ = [sb2("tt%d" % i, [128, 512], F32) for i in range(3)]
                b_Wg = b_Wbr = b_bg = Buf()
                b_Ych, b_mch = [Buf(), Buf()], [Buf(), Buf()]
                b_gt = [Buf() for _ in range(3)]
                b_tt = [Buf() for _ in range(3)]
                b_ps = [Buf() for _ in range(8)]
                c_w2, c_ych, c_mst = chan("w2"), [chan("ych0"), chan("ych1")], [chan("mst0"), chan("mst1")]
                for br in range(3):
                    dma(pool, c_w2, lambda br=br: nc.gpsimd.dma_start(out=Wg[:, :, br * 1024:(br + 1) * 1024],
                                                                      in_=w_in_v[:, :, 3584 + br * 1024:3584 + (br + 1) * 1024]), writes=[b_Wg])
                dma(pool, c_w2, lambda: nc.gpsimd.dma_start(out=Wbr[:, 0:2, :], in_=w_a_d.rearrange("(kc p) n -> p kc n", p=128)), writes=[b_Wbr])
                dma(pool, c_w2, lambda: nc.gpsimd.dma_start(out=Wbr[:, 2:6, :], in_=w_b_d.rearrange("(kc p) n -> p kc n", p=128)), writes=[b_Wbr])
                dma(pool, c_w2, lambda: nc.gpsimd.dma_start(out=Wbr[:, 6:10, :], in_=w_c_d.rearrange("(kc p) n -> p kc n", p=128)), writes=[b_Wbr])
                dma(sp, c_w2, lambda: nc.sync.dma_start(out=bg[:], in_=bgate_d[:, :]), writes=[b_bg])
                yT_v = yT_s.rearrange("(c p) t -> p c t", p=128)
                mT_v = mT_s.rearrange("(c p) t -> p c t", p=128)
                brk = ((0, 2), (2, 6), (6, 10))

                def load_y(tc):
                    dma(sp, c_ych[tc % 2], lambda: nc.sync.dma_start(out=Ych[tc % 2][:], in_=yT_v[:, :, tc * 512:(tc + 1) * 512]),
                        writes=[b_Ych[tc % 2]])

                if upto not in ("p0", "p1"):
                    load_y(0)
                for tc in (range(8) if upto not in ("p0", "p1") else ()):
                    s = tc % 2
                    if tc + 1 < 8:
                        load_y(tc + 1)
                    tsl = slice(tc * 512, (tc + 1) * 512)
                    for f in range(8):
                        for br in range(3):
                            gb = br
                            for kc in range(8):
                                pe.do(lambda kc=kc, br=br, f=f, gb=gb: nc.tensor.matmul(
                                    bank(gb), lhsT=Wg[:, kc, br * 1024 + f * 128:br * 1024 + (f + 1) * 128], rhs=XT[:, kc, tsl],
                                    start=(kc == 0), stop=(kc == 7)), reads=[b_Wg, b_XTc[tc // 2]], writes=[b_ps[gb]])
                            act.do(lambda br=br, f=f, gb=gb: nc.scalar.activation(out=gt[br][:], in_=bank(gb), func=AF.Sigmoid,
                                                                                   bias=bg[:, br * 8 + f:br * 8 + f + 1]),
                                   reads=[b_ps[gb], b_bg], writes=[b_gt[br]])
                            k0, k1 = brk[br]
                            for kc in range(k0, k1):
                                pe.do(lambda kc=kc, br=br, f=f, k0=k0, k1=k1: nc.tensor.matmul(
                                    bank(3 + br), lhsT=Wbr[:, kc, f * 128:(f + 1) * 128], rhs=Ych[s][:, kc, :],
                                    start=(kc == k0), stop=(kc == k1 - 1)), reads=[b_Wbr, b_Ych[s]], writes=[b_ps[3 + br]])
                            dve.do(lambda br=br: nc.vector.tensor_tensor(out=tt_[br][:], in0=bank(3 + br), in1=gt[br][:], op=ALU.mult),
                                   reads=[b_ps[3 + br], b_gt[br]], writes=[b_tt[br]])
                        pool.do(lambda: nc.gpsimd.tensor_tensor(out=tt_[0][:], in0=tt_[0][:], in1=tt_[1][:], op=ALU.add),
                                reads=[b_tt[1]], writes=[b_tt[0]])
                        pool.do(lambda f=f: nc.gpsimd.tensor_tensor(out=mch[s][:, f, :], in0=tt_[0][:], in1=tt_[2][:], op=ALU.add),
                                reads=[b_tt[0], b_tt[2]], writes=[b_mch[s]])
                    dma(sp, c_mst[s], lambda s=s: nc.sync.dma_start(out=mT_v[:, :, tsl], in_=mch[s][:]), reads=[b_mch[s]])
                barrier()
        barrier()

        with ExitStack() as st:
            sb = lambda n, s_, t: st.enter_context(nc.sbuf_tensor("s_" + n, s_, t))
            Wout = sb("Wout", [128, 8, D], BF16)
            Wq = sb("Wq", [128, 8, D], BF16)
            skT = sb("skT", [128, 8, 128], F32)
            lnp = sb("lnp", [128, 4, D], F32)
            iota16 = sb("iota16", [128, 16], F32)
            mTc = [sb("mTc%d" % i, [128, 8, 512], BF16) for i in range(2)]
            xt = [sb("xt%d" % i, [128, D], F32) for i in range(2)]
            r1 = sb("r1", [128, D], F32)
            x1 = [sb("x1_%d" % i, [128, D], F32) for i in range(2)]
            x1T = sb("x1T", [128, 8, 128], BF16)
            qT = sb("qT", [128, 8, 128], F32)
            stats = sb("stats", [128, 2, 6], F32)
            mv = sb("mv", [128, 2], F32)
            rstd = sb("rstd", [128, 1], F32)
            nmr = sb("nmr", [128, 1], F32)
            sv = sb("sv", [128, 16, 16], F32)
            si_u = sb("si_u", [128, 16, 16], U32)
            si_f = sb("si_f", [128, 16, 16], F32)
            work = sb("work", [128, 128], F32)
            cand = sb("cand", [128, 8, 256], F32)
            work2 = sb("work2", [128, 256], F32)
            top = sb("top", [128, 8, 16], F32)
            pos_u = sb("pos_u", [128, 8, 16], U32)
            ab_u = sb("ab_u", [128, 2, 128], U32)
            ab_f = sb("ab_f", [128, 2, 128], F32)
            oh = sb("oh", [128, 8, 16, 16], BF16)
            ab_b = sb("ab_b", [128, 2, 128], BF16)
            si_b = sb("si_b", [128, 16, 16], BF16)
            iota_b = sb("iota_b", [128, 16], BF16)
            ij = sb("ij", [128, 2, 128], F32)
            eidx_f = sb("eidx_f", [128, 128], F32)
            eidx_u = [sb("eidx_u%d" % i, [128, 128], U32) for i in range(2)]
            gate = [sb("gate%d" % i, [128, 8, 16], F32) for i in range(2)]
            gsum = sb("gsum", [128, 8], F32)
            hpre = bank(4)[:, 0:128]
            gl = sb("gl", [128, 128], F32)
            x1b = [sb("x1b%d" % i, [128, D], BF16) for i in range(2)]
            prod = [sb("prod%d" % i, [128, D], BF16) for i in range(NP)]
            junk = sb("junk", [128, D], BF16)
            G = [sb("G%d" % i, [128, 2 * D], BF16) for i in range(NG)]
            diag = [sb("diag%d" % i, [128, 128], BF16) for i in range(ND)]
            ident_b = sb("ident_b", [128, 128], BF16)
            r2 = sb("r2", [128, D], F32)
            ot = [sb("ot%d" % i, [128, D], F32) for i in range(2)]
            b_w3 = Buf()
            b_mTc, b_xt, b_ot, b_x1, b_eu, b_gate = ([Buf(), Buf()] for _ in range(6))
            b_r1, b_x1T, b_qT, b_st, b_mv, b_rstd, b_nmr = (Buf() for _ in range(7))
            b_sv, b_siu, b_sif, b_work, b_cand, b_work2, b_top, b_pos = (Buf() for _ in range(8))
            b_abu, b_abf, b_oh, b_ij, b_ef, b_gsum, b_r2, b_idb = (Buf() for _ in range(8))
            b_G = [Buf() for _ in range(NG)]
            b_diag = [Buf() for _ in range(ND)]
            b_hp = [Buf() for _ in range(128)]
            b_gl = [Buf() for _ in range(128)]
            b_x1b = [Buf(), Buf()]
            b_prod = [Buf() for _ in range(NP)]
            b_ps = [Buf() for _ in range(8)]
            c_w3 = chan("w3")
            c_mtc, c_xt_, c_ot = [chan("mtc0"), chan("mtc1")], [chan("xt0"), chan("xt1")], [chan("ot0"), chan("ot1")]
            c_g = [chan("g%d" % i) for i in range(NG)]
            c_dbg = chan("dbg")
            dve.do(lambda: nc.vector.tensor_copy(out=ident_b[:], in_=ident[:]), reads=[b_const], writes=[b_idb])
            dma(pool, c_w3, lambda: nc.gpsimd.dma_start(out=Wout[:], in_=w_out_d.rearrange("(kc p) n -> p kc n", p=128)), writes=[b_w3])
            dma(pool, c_w3, lambda: nc.gpsimd.dma_start(out=Wq[:], in_=w_q_d.rearrange("(kc p) n -> p kc n", p=128)), writes=[b_w3])
            dma(sp, c_w3, lambda: nc.sync.dma_start(out=skT[:].rearrange("p a b -> p (a b)"), in_=skT_d[:, :]), writes=[b_w3])
            dma(sp, c_w3, lambda: nc.sync.dma_start(out=lnp[:].rearrange("p a b -> p (a b)"), in_=lnp_d[:, :]), writes=[b_w3])
            dma(sp, c_w3, lambda: nc.sync.dma_start(out=iota16[:], in_=iota_d[:, :]), writes=[b_w3])
            dve.do(lambda: nc.vector.tensor_copy(out=iota_b[:], in_=iota16[:]), reads=[b_w3], writes=[b_w3])
            mT_v = mT_s.rearrange("(c p) t -> p c t", p=128)
            accp = bank(6, 2)
            NTILES = S // 128 if ntiles is None else ntiles
            if upto != "all":
                NTILES = 0

            def layer_norm_steps(L, src, b_src, dst, b_dst, gi):
                def s_stats():
                    for hf in range(2):
                        dve.do(lambda hf=hf: nc.vector.bn_stats(out=stats[:, hf, :], in_=src[:, hf * 512:(hf + 1) * 512]), reads=[b_src], writes=[b_st])
                    dve.do(lambda: nc.vector.bn_aggr(out=mv[:], in_=stats[:].rearrange("p a b -> p (a b)")), reads=[b_st], writes=[b_mv])
                    act.do(lambda: nc.scalar.activation(out=rstd[:], in_=mv[:, 1:2], func=AF.Sqrt, bias=float(LN_EPS), scale=1.0), reads=[b_mv], writes=[b_rstd])
                def s_norm():
                    dve.do(lambda: nc.vector.reciprocal(out=rstd[:], in_=rstd[:]), reads=[], writes=[b_rstd])
                    dve.do(lambda: nc.vector.scalar_tensor_tensor(out=nmr[:], in0=mv[:, 0:1], scalar=-1.0, in1=rstd[:], op0=ALU.mult, op1=ALU.mult),
                           reads=[b_mv, b_rstd], writes=[b_nmr])
                    act.do(lambda: nc.scalar.activation(out=dst[:], in_=src[:], func=AF.Identity, bias=nmr[:, 0:1], scale=rstd[:, 0:1]),
                           reads=[b_src, b_rstd, b_nmr], writes=[b_dst])
                def s_g():
                    dve.do(lambda: nc.vector.tensor_tensor(out=dst[:], in0=dst[:], in1=lnp[:, gi, :], op=ALU.mult), reads=[b_w3], writes=[b_dst])
                def s_b():
                    dve.do(lambda: nc.vector.tensor_tensor(out=dst[:], in0=dst[:], in1=lnp[:, gi + 1, :], op=ALU.add), reads=[b_w3], writes=[b_dst])
                L.extend([s_stats, s_norm, s_g, s_b])

            def load_tile(tt):
                if tt >= NTILES:
                    return
                if tt % 4 == 0:
                    cc = (tt // 4) % 2
                    dma(sp, c_mtc[cc], lambda: nc.sync.dma_start(out=mTc[cc][:], in_=mT_v[:, :, (tt // 4) * 512:(tt // 4 + 1) * 512]),
                        writes=[b_mTc[cc]])
                dma(sp, c_xt_[tt % 2], lambda: nc.sync.dma_start(out=xt[tt % 2][:], in_=x_d[tt * 128:(tt + 1) * 128, :]), writes=[b_xt[tt % 2]])

            def topk16(L, src_fn, b_src, vals, b_vals, idxs, b_idxs, wk, b_wk):
                def f():
                    src = src_fn()
                    dve.do(lambda: nc.vector.max(out=vals[:, 0:8], in_=src), reads=[b_src], writes=[b_vals])
                    dve.do(lambda: nc.vector.max_index(out=idxs[:, 0:8], in_max=vals[:, 0:8], in_values=src), reads=[b_src, b_vals], writes=[b_idxs])
                    dve.do(lambda: nc.vector.match_replace(out=wk, in_to_replace=vals[:, 0:8], in_values=src, imm_value=-1e30),
                           reads=[b_src, b_vals], writes=[b_wk])
                    dve.do(lambda: nc.vector.max(out=vals[:, 8:16], in_=wk), reads=[b_wk], writes=[b_vals])
                    dve.do(lambda: nc.vector.max_index(out=idxs[:, 8:16], in_max=vals[:, 8:16], in_values=wk), reads=[b_wk, b_vals], writes=[b_idxs])
                L.append(f)

            def front_steps(tt):
                L = []
                if tt >= NTILES:
                    return L
                par = tt % 2
                cc = (tt // 4) % 2
                sub = tt % 4
                xs = tt % 2
                X1 = x1[par]
                bX1 = b_x1[par]
                for hf in range(2):
                    for f0 in (0, 4):
                        def s_wout(hf=hf, f0=f0):
                            for f in range(f0, f0 + 4):
                                pe.do(lambda f=f: nc.tensor.matmul(bank(hf), lhsT=mTc[cc][:, f, sub * 128:(sub + 1) * 128],
                                                                  rhs=Wout[:, f, hf * 512:(hf + 1) * 512], start=(f == 0), stop=(f == 7)),
                                      reads=[b_mTc[cc], b_w3], writes=[b_ps[hf]])
                        L.append(s_wout)
                L.append(lambda: dve.do(lambda: nc.vector.scalar_tensor_tensor(out=r1[:], in0=xt[xs][:], scalar=float(ALPHA), in1=bank(0, 2),
                                                                                op0=ALU.mult, op1=ALU.add),
                                        reads=[b_xt[xs], b_ps[0], b_ps[1]], writes=[b_r1]))
                layer_norm_steps(L, r1, b_r1, X1, bX1, 0)
                if debug:
                    L.append(lambda: dma(sp, c_dbg, lambda: nc.sync.dma_start(out=x1_dbg[tt * 128:(tt + 1) * 128, :], in_=X1[:]), reads=[bX1]))
                L.append(lambda: act.do(lambda: nc.scalar.activation(out=x1b[par][:], in_=X1[:], func=AF.Identity), reads=[bX1], writes=[b_x1b[par]]))
                for f0 in (0, 4):
                    def s_tr(f0=f0):
                        for f in range(f0, f0 + 4):
                            pe.do(lambda f=f: nc.tensor.transpose(out=bank(2, 2)[:, f * 128:(f + 1) * 128], in_=X1[:, f * 128:(f + 1) * 128], identity=ident[:]),
                                  reads=[bX1, b_const], writes=[b_ps[2 + f // 4]])
                    L.append(s_tr)
                L.append(lambda: act.do(lambda: nc.scalar.activation(out=x1T[:].rearrange("p a b -> p (a b)"), in_=bank(2, 2), func=AF.Identity),
                                        reads=[b_ps[2], b_ps[3]], writes=[b_x1T]))
                for h in range(8):
                    def s_q(h=h):
                        for kc in range(8):
                            pe.do(lambda kc=kc: nc.tensor.matmul(bank(0, 2)[:, h * 128:(h + 1) * 128], lhsT=Wq[:, kc, h * 128:(h + 1) * 128],
                                                                rhs=x1T[:, kc, :], start=(kc == 0), stop=(kc == 7)),
                                  reads=[b_w3, b_x1T], writes=[b_ps[h // 4]])
                    L.append(s_q)
                L.append(lambda: act.do(lambda: nc.scalar.activation(out=qT[:].rearrange("p a b -> p (a b)"), in_=bank(0, 2), func=AF.Identity),
                                        reads=[b_ps[0], b_ps[1]], writes=[b_qT]))
                for rnd, b0 in ((0, 2), (1, 0)):
                    def s_sc(rnd=rnd, b0=b0):
                        for hh in range(4):
                            h = rnd * 4 + hh
                            for c in range(2):
                                pr = slice(c * 64, (c + 1) * 64)
                                pe.do(lambda h=h, hh=hh, c=c, pr=pr: nc.tensor.matmul(bank(b0 + c)[:, hh * 128:(hh + 1) * 128], lhsT=qT[pr, h, :],
                                                                                    rhs=skT[pr, h, :], start=True, stop=True),
                                      reads=[b_qT, b_w3], writes=[b_ps[b0 + c]])
                    L.append(s_sc)
                    for hh in range(4):
                        for c in range(2):
                            hc = c * 8 + rnd * 4 + hh
                            topk16(L, (lambda b0=b0, c=c, hh=hh: bank(b0 + c)[:, hh * 128:(hh + 1) * 128]), b_ps[b0 + c],
                                   sv[:, hc, :], b_sv, si_u[:, hc, :], b_siu, work[:], b_work)
                sv4 = sv[:].rearrange("p (c h) k -> p c h k", c=2)
                si4 = si_b[:].rearrange("p (c h) k -> p c h k", c=2)

                def s_cand():
                    dve.do(lambda: nc.vector.tensor_copy(out=si_b[:], in_=si_u[:]), reads=[b_siu], writes=[b_sif])
                    dve.do(lambda: nc.vector.tensor_tensor(out=cand[:].rearrange("p h (a b) -> p h a b", b=16),
                                                           in0=sv4[:, 0, :, :].unsqueeze(3).broadcast_to([128, 8, 16, 16]),
                                                           in1=sv4[:, 1, :, :].unsqueeze(2).broadcast_to([128, 8, 16, 16]), op=ALU.add),
                           reads=[b_sv], writes=[b_cand])
                L.append(s_cand)
                for h in range(8):
                    topk16(L, (lambda h=h: cand[:, h, :]), b_cand, top[:, h, :], b_top, pos_u[:, h, :], b_pos, work2[:], b_work2)
                posf = pos_u[:].rearrange("p h k -> p (h k)")

                def s_ab():
                    dve.do(lambda: nc.vector.tensor_single_scalar(out=ab_u[:, 0, :], in_=posf, scalar=4, op=ALU.logical_shift_right), reads=[b_pos], writes=[b_abu])
                    dve.do(lambda: nc.vector.tensor_single_scalar(out=ab_u[:, 1, :], in_=posf, scalar=15, op=ALU.bitwise_and), reads=[b_pos], writes=[b_abu])
                    dve.do(lambda: nc.vector.tensor_copy(out=ab_b[:], in_=ab_u[:]), reads=[b_abu], writes=[b_abf])
                L.append(s_ab)
                for c in range(2):
                    def s_lk(c=c):
                        abv = ab_b[:, c, :].rearrange("p (h k) -> p h k", k=16)
                        dve.do(lambda: nc.vector.tensor_tensor(out=oh[:], in0=abv.unsqueeze(3).broadcast_to([128, 8, 16, 16]),
                                                               in1=iota_b[:].unsqueeze(1).unsqueeze(1).broadcast_to([128, 8, 16, 16]), op=ALU.is_equal),
                               reads=[b_abf, b_w3], writes=[b_oh])
                        dve.do(lambda: nc.vector.tensor_tensor(out=oh[:], in0=oh[:], in1=si4[:, c, :, :].unsqueeze(2).broadcast_to([128, 8, 16, 16]), op=ALU.mult),
                               reads=[b_sif], writes=[b_oh])
                        dve.do(lambda: nc.vector.tensor_reduce(out=ij[:, c, :], in_=oh[:].rearrange("p h k a -> p (h k) a"), axis=AX.X, op=ALU.add),
                               reads=[b_oh], writes=[b_ij])
                    L.append(s_lk)

                def s_eidx():
                    dve.do(lambda: nc.vector.scalar_tensor_tensor(out=eidx_f[:], in0=ij[:, 0, :], scalar=128.0, in1=ij[:, 1, :], op0=ALU.mult, op1=ALU.add),
                           reads=[b_ij], writes=[b_ef])
                    dve.do(lambda: nc.vector.tensor_copy(out=eidx_u[par][:], in_=eidx_f[:]), reads=[b_ef], writes=[b_eu[par]])
                L.append(s_eidx)
                GT = gate[par]
                bGT = b_gate[par]

                def s_gate1():
                    dve.do(lambda: nc.vector.tensor_tensor(out=GT[:], in0=top[:], in1=top[:, :, 0:1].broadcast_to([128, 8, 16]), op=ALU.subtract),
                           reads=[b_top], writes=[bGT])
                    act.do(lambda: nc.scalar.activation(out=GT[:], in_=GT[:], func=AF.Exp), reads=[], writes=[bGT])
                def s_gate2():
                    dve.do(lambda: nc.vector.tensor_reduce(out=gsum[:], in_=GT[:], axis=AX.X, op=ALU.add), reads=[bGT], writes=[b_gsum])
                    dve.do(lambda: nc.vector.reciprocal(out=gsum[:], in_=gsum[:]), reads=[], writes=[b_gsum])
                    dve.do(lambda: nc.vector.tensor_tensor(out=GT[:], in0=GT[:], in1=gsum[:].unsqueeze(2).broadcast_to([128, 8, 16]), op=ALU.mult),
                           reads=[b_gsum], writes=[bGT])
                L.extend([s_gate1, s_gate2])
                return L

            def slot_head(tt, s_):
                par = tt % 2
                g = s_ % NG
                pb = s_ % NP
                dma(pool, c_g[g], lambda: nc.gpsimd.indirect_dma_start(
                    out=G[g][:], out_offset=None, in_=uv_s[:, :], in_offset=bass.IndirectOffsetOnAxis(ap=eidx_u[par][:, s_:s_ + 1], axis=0)),
                    reads=[b_eu[par]], writes=[b_G[g]])
                dve.do(lambda: nc.vector.tensor_tensor(out=prod[pb][:], in0=G[g][:, 0:D], in1=x1b[par][:], op=ALU.mult),
                       reads=[b_G[g], b_x1b[par]], writes=[b_prod[pb]])
                act.do(lambda: nc.scalar.activation(out=junk[:], in_=prod[pb][:], func=AF.Identity, accum_out=hpre[:, s_:s_ + 1]),
                       reads=[b_prod[pb]], writes=[b_hp[s_]])
                act.do(lambda: nc.scalar.activation(out=gl[:, s_:s_ + 1], in_=hpre[:, s_:s_ + 1], func=AF.Gelu), reads=[b_hp[s_]], writes=[b_gl[s_]])

            def slot_tail(tt, s_):
                par = tt % 2
                g = s_ % NG
                d_ = s_ % ND
                gatef = gate[par][:].rearrange("p h k -> p (h k)")
                dve.do(lambda: nc.vector.tensor_scalar(out=diag[d_][:], in0=ident_b[:], scalar1=gl[:, s_:s_ + 1], scalar2=gatef[:, s_:s_ + 1],
                                                       op0=ALU.mult, op1=ALU.mult),
                       reads=[b_gl[s_], b_gate[par], b_idb], writes=[b_diag[d_]])
                for hf in range(2):
                    pe.do(lambda hf=hf: nc.tensor.matmul(bank(6 + hf), lhsT=diag[d_][:], rhs=G[g][:, D + hf * 512:D + (hf + 1) * 512],
                                                        start=(s_ == 0), stop=(s_ == 127)),
                          reads=[b_diag[d_], b_G[g]], writes=[b_ps[6 + hf]])

            def tail(tt):
                par = tt % 2
                os_ = tt % 2
                if debug:
                    dve.do(lambda: nc.vector.tensor_copy(out=r2[:], in_=accp), reads=[b_ps[6], b_ps[7]], writes=[b_r2])
                    dma(sp, c_dbg, lambda: nc.sync.dma_start(out=yp_dbg[tt * 128:(tt + 1) * 128, :], in_=r2[:]), reads=[b_r2])
                dve.do(lambda: nc.vector.scalar_tensor_tensor(out=r2[:], in0=x1[par][:], scalar=float(ALPHA), in1=accp, op0=ALU.mult, op1=ALU.add),
                       reads=[b_x1[par], b_ps[6], b_ps[7]], writes=[b_r2])
                L = []
                layer_norm_steps(L, r2, b_r2, ot[os_], b_ot[os_], 2)
                for f in L:
                    f()
                dma(sp, c_ot[os_], lambda: nc.sync.dma_start(out=out_d[tt * 128:(tt + 1) * 128, :], in_=ot[os_][:]), reads=[b_ot[os_]])

            load_tile(0)
            load_tile(1)
            for f in front_steps(0):
                f()
            for tt in range(NTILES):
                load_tile(tt + 2)
                nxt = front_steps(tt + 1)
                k = 0
                for s_ in range(128 + LAG):
                    if s_ < 128:
                        slot_head(tt, s_)
                    if s_ >= LAG:
                        slot_tail(tt, s_ - LAG)
                    if s_ < 128:
                        tgt = ((s_ + 1) * len(nxt)) // 128
                        while k < tgt:
                            nxt[k]()
                            k += 1
                tail(tt)
            barrier()
    return nc


_NC_CACHE = {}


def _host_inputs(x, mem, rel_bias, w_in, b_gate, w_mem_kv, sinks, w_branch_a, w_branch_b, w_branch_c, w_out, ln1_g, ln1_b,
                 peer_w_query, peer_sub_keys, peer_u, peer_v, ln2_g, ln2_b):
    f = lambda a: np.ascontiguousarray(np.asarray(a, dtype=np.float32))
    pairs, bucket, mask, heads = _bias_tables()
    rb = np.asarray(rel_bias, np.float32)
    biasT = np.zeros((128, 10, 4, 128), np.float32)
    maskT = np.zeros((128, 10, 4, 128), np.float32)
    for p in range(10):
        for hh in range(2):
            for kb in range(2):
                biasT[:, p, 2 * hh + kb, :] = rb[bucket[p, kb], heads[p] + hh]
                maskT[:, p, 2 * hh + kb, :] = mask[p, kb]
    sk = np.asarray(sinks, np.float32)[0]
    sinksP = np.zeros((128, 4), np.float32)
    for p in range(4):
        sinksP[0:64, p] = sk[2 * p]
        sinksP[64:128, p] = sk[2 * p + 1]
    lnp = np.stack([np.asarray(a, np.float32)[0] for a in (ln1_g, ln1_b, ln2_g, ln2_b)], 0)
    lnp = np.ascontiguousarray(np.broadcast_to(lnp.reshape(1, 4 * D), (128, 4 * D)))
    skT = np.asarray(peer_sub_keys, np.float32)[0].transpose(1, 3, 0, 2).reshape(128, 8 * 128)
    shared = {
        "w_in": f(w_in[0]), "w_mem_kv": f(w_mem_kv[0]), "w_a": f(w_branch_a[0]), "w_b": f(w_branch_b[0]), "w_c": f(w_branch_c[0]),
        "w_out": f(w_out[0]), "w_q": f(peer_w_query[0]), "skT": f(skT), "peer_u": f(peer_u[0]), "peer_v": f(peer_v[0]),
        "biasT": f(biasT.reshape(128, 5120)), "maskT": f(maskT.reshape(128, 5120)),
        "bgate": f(np.asarray(b_gate, np.float32)[0].reshape(24, 128).T), "sinksP": f(sinksP), "lnp": f(lnp),
        "ident": np.eye(128, dtype=np.float32), "iota16": f(np.broadcast_to(np.arange(16, dtype=np.float32), (128, 16))),
    }
    x = np.asarray(x, np.float32)
    mem = np.asarray(mem, np.float32)
    in_maps = []
    for b in range(x.shape[0]):
        m = dict(shared)
        m["x"] = f(x[b])
        m["xT"] = f(x[b].T)
        m["memT"] = f(mem[b].T)
        in_maps.append(m)
    return in_maps


def kernel(**inputs):
    in_maps = _host_inputs(**inputs)
    if "nc" not in _NC_CACHE:
        _NC_CACHE["nc"] = build_nc()
    nc = _NC_CACHE["nc"]
    res = run_bass_kernel_spmd(nc, in_maps, core_ids=list(range(NCORES)))
    return np.stack([np.asarray(r["out"], dtype=np.float32) for r in res.results], 0)
```

```python
import numpy as np
import concourse.bass as bass
import concourse.mybir as mybir
from concourse.bass_utils import run_bass_kernel_spmd
from contextlib import ExitStack

F32 = mybir.dt.float32
BF16 = mybir.dt.bfloat16
U32 = mybir.dt.uint32
AF = mybir.ActivationFunctionType
ALU = mybir.AluOpType
AX = mybir.AxisListType

S = 4096
D = 1024
NCORES = 8
ALPHA = 2.0 ** 0.25
LN_EPS = 1e-5
NEG = -30000.0
N_EXP = 16384
NG = 12
ND = 4
LAG = 3
NP = 4


class Buf:
    __slots__ = ("w", "r")

    def __init__(self):
        self.w = None
        self.r = {}


class Q:
    def __init__(self, nc, eng, st, name, is_pe=False):
        self.nc = nc
        self.eng = eng
        self.sem = st.enter_context(nc.semaphore("q_" + name))
        self.n = 0
        self.seen = {}
        self.is_pe = is_pe

    def wait(self, ev):
        if ev is None:
            return
        sem, val = ev
        if sem is self.sem and self.is_pe:
            return
        k = id(sem)
        if self.seen.get(k, -1) >= val:
            return
        self.eng.wait_ge(sem, val)
        self.seen[k] = val

    def deps(self, reads, writes, extra, skip_sem=None):
        for b in reads:
            if b.w is not None and b.w[0] is not skip_sem:
                self.wait(b.w)
        for b in writes:
            if b.w is not None and b.w[0] is not skip_sem:
                self.wait(b.w)
            for ev in b.r.values():
                self.wait(ev)
        for ev in extra:
            self.wait(ev)

    @staticmethod
    def mark(ev, reads, writes):
        for b in reads:
            k = id(ev[0])
            old = b.r.get(k)
            if old is None or old[1] < ev[1]:
                b.r[k] = ev
        for b in writes:
            b.w = ev
            b.r = {}

    def do(self, fn, reads=(), writes=(), extra=()):
        self.deps(reads, writes, extra)
        ins = fn()
        self.n += 1
        ins.then_inc(self.sem, 1)
        ev = (self.sem, self.n)
        self.mark(ev, reads, writes)
        return ev


class DmaChan:
    def __init__(self, nc, st, name):
        self.sem = st.enter_context(nc.semaphore("c_" + name))
        self.n = 0


def dma(q, chan, fn, reads=(), writes=(), extra=()):
    q.deps(reads, writes, extra, skip_sem=chan.sem)
    ins = fn()
    chan.n += 16
    ins.then_inc(chan.sem, 16)
    ev = (chan.sem, chan.n)
    Q.mark(ev, reads, writes)
    return ev


class SpQ(Q):
    def __init__(self, nc, eng):
        self.nc = nc
        self.eng = eng
        self.sem = None
        self.n = 0
        self.seen = {}
        self.is_pe = False


def _t5_bucket(dist):
    n = np.asarray(dist, dtype=np.int32)
    max_exact = 16
    nf = np.maximum(n, 1).astype(np.float32)
    scale = np.float32(np.log(2048 / max_exact))
    large = max_exact + (np.log(nf / np.float32(max_exact)) / scale * np.float32(32 - max_exact)).astype(np.int32)
    large = np.minimum(large, 31)
    return np.where(n < max_exact, n, large).astype(np.int32)


def _bias_tables():
    i = np.arange(128)[None, :]
    j = np.arange(128)[:, None]
    off_prev = 128 + i - j
    off_cur = i - j
    pairs = []
    for g, r in enumerate((1, 4, 16)):
        for m in range(2):
            pairs.append(("A", r, g * 4 + 2 * m))
    for m in range(4):
        pairs.append(("B", 1, 12 + 2 * m))
    bucket = np.zeros((10, 2, 128, 128), np.int32)
    mask = np.zeros((10, 2, 128, 128), np.float32)
    heads = []
    for p, (kind, r, h0) in enumerate(pairs):
        W = 128 if kind == "A" else 127
        for kb, off in enumerate((off_prev, off_cur)):
            bucket[p, kb] = _t5_bucket(np.clip(off, 0, W) * r)
            valid = (off >= 0) & (off <= W)
            mask[p, kb] = np.where(valid, 0.0, NEG)
        heads.append(h0)
    return pairs, bucket, mask, heads


def build_nc(debug=False, upto="all", ntiles=None, njobs=None, do_c=True, stage=99, jobsel=None):
    nc = bass.Bass("TRN2", target_bir_lowering=False)
    dt_in = lambda n, s, t=F32: nc.dram_tensor(n, s, t, kind="ExternalInput").ap()
    xT_d = dt_in("xT", [D, S])
    x_d = dt_in("x", [S, D])
    memT_d = dt_in("memT", [D, 256])
    w_in_d = dt_in("w_in", [D, 6656])
    w_kv_d = dt_in("w_mem_kv", [D, 1024])
    w_a_d = dt_in("w_a", [256, D])
    w_b_d = dt_in("w_b", [512, D])
    w_c_d = dt_in("w_c", [512, D])
    w_out_d = dt_in("w_out", [D, D])
    w_q_d = dt_in("w_q", [D, D])
    skT_d = dt_in("skT", [128, 8 * 128])
    pu_d = dt_in("peer_u", [N_EXP, D])
    pv_d = dt_in("peer_v", [N_EXP, D])
    biasT_d = dt_in("biasT", [128, 10 * 512])
    maskT_d = dt_in("maskT", [128, 10 * 512])
    bgate_d = dt_in("bgate", [128, 24])
    sinks_d = dt_in("sinksP", [128, 4])
    lnp_d = dt_in("lnp", [128, 4 * D])
    ident_d = dt_in("ident", [128, 128])
    iota_d = dt_in("iota16", [128, 16])
    out_d = nc.dram_tensor("out", [S, D], F32, kind="ExternalOutput").ap()
    skind = "ExternalOutput" if debug else "Internal"
    yT_s = nc.dram_tensor("yT_scr", [1280, S], BF16, kind=skind).ap()
    mT_s = nc.dram_tensor("mT_scr", [D, S], BF16, kind=skind).ap()
    uv_s = nc.dram_tensor("uv_scr", [N_EXP, 2 * D], BF16, kind="Internal").ap()
    if debug:
        x1_dbg = nc.dram_tensor("x1_dbg", [S, D], F32, kind="ExternalOutput").ap()
        yp_dbg = nc.dram_tensor("yp_dbg", [S, D], F32, kind="ExternalOutput").ap()

    with ExitStack() as gst:
        pe = Q(nc, nc.tensor, gst, "pe", is_pe=True)
        act = Q(nc, nc.scalar, gst, "act")
        dve = Q(nc, nc.vector, gst, "dve")
        pool = Q(nc, nc.gpsimd, gst, "pool")
        sp = SpQ(nc, nc.sync)
        queues = [pe, act, dve, pool, sp]
        chans = []

        def chan(name):
            c = DmaChan(nc, gst, name)
            chans.append(c)
            return c

        def barrier(skip=()):
            for q in queues:
                for o in (pe, act, dve, pool):
                    if o is not q and o.n > 0:
                        q.wait((o.sem, o.n))
                for c in chans:
                    if c.n > 0 and c not in skip:
                        q.wait((c.sem, c.n))

        psall = gst.enter_context(nc.psum_tensor("psall", [128, 4096], F32))

        def bank(i, n=1):
            return psall[:, i * 512:(i + n) * 512]

        ident = gst.enter_context(nc.sbuf_tensor("s_ident", [128, 128], F32))
        ones_b = gst.enter_context(nc.sbuf_tensor("s_ones_b", [128, 128], BF16))
        c_const = chan("const")
        b_const = Buf()
        dma(sp, c_const, lambda: nc.sync.dma_start(out=ident[:], in_=ident_d[:, :]), writes=[b_const])
        dve.do(lambda: nc.vector.memset(ones_b[:], 1.0), writes=[b_const])

        w_in_v = w_in_d.rearrange("(kc p) n -> p kc n", p=128)

        c_cv = chan("cv")
        cv_list = [(tab, c0, c) for (tab, c0) in ((pu_d, 0), (pv_d, D)) for c in range(16)]

        def convert_some(k):
            for _ in range(k):
                if not cv_list:
                    return
                tab, c0, c = cv_list.pop(0)
                src = tab[c * 1024:(c + 1) * 1024, :].rearrange("(p r) d -> p r d", p=128)
                dst = uv_s[c * 1024:(c + 1) * 1024, c0:c0 + D].rearrange("(p r) d -> p r d", p=128)
                dma(pool, c_cv, lambda: nc.gpsimd.dma_start(out=dst, in_=src))

        with ExitStack() as st:
            sb = lambda n, s, t: st.enter_context(nc.sbuf_tensor("s_" + n, s, t))
            XT = sb("XT", [128, 8, S], BF16)
            b_XTc = [Buf() for _ in range(4)]
            c_xtc = [chan("xc%d" % i) for i in range(4)]
            st01 = st.enter_context(ExitStack())
            sb1 = lambda n, s, t: st01.enter_context(nc.sbuf_tensor("s_" + n, s, t))
            BT = sb1("BT", [128, 10, 512], BF16)
            es = sb1("es", [128, 4], F32)
            KmT = sb1("KmT", [128, 4, 256], BF16)
            Vm = sb1("Vm", [128, 2, 512], BF16)
            b_BT, b_es, b_KmT, b_Vm = Buf(), Buf(), Buf(), Buf()
            b_ps = [Buf() for _ in range(8)]
            c_misc = chan("misc")

            Wt = [sb1("Wt%d" % i, [128, 8, 384], BF16) for i in range(2)]
            b_Wt = [Buf(), Buf()]
            c_wt, c_ys = [chan("wt0"), chan("wt1")], [chan("ys0"), chan("ys1")]
            jobs = []
            for m in range(2):
                for g, r in enumerate((1, 4, 16)):
                    c = g * 256 + 2 * m * 64
                    jobs.append(dict(q=c, k=(768 + c, 768 + c + 64), v=(1536 + c, 1536 + c + 64), r=r, bp=g * 2 + m,
                                     first=(g == 0), last=(g == 2), sink=None, row=m * 128))
            for kv in range(2):
                for m in range(2):
                    hq = kv * 4 + 2 * m
                    jobs.append(dict(q=2304 + hq * 64, k=(2816 + kv * 64,) * 2, v=(2944 + kv * 64,) * 2, r=1, bp=6 + kv * 2 + m,
                                     first=True, last=True, sink=kv * 2 + m, row=256 + (kv * 2 + m) * 128))

            def load_w(ji):
                jb = jobs[ji]
                s = ji % 2
                dma(pool, c_wt[s], lambda: nc.gpsimd.dma_start(out=Wt[s][:, :, 0:128], in_=w_in_v[:, :, jb["q"]:jb["q"] + 128]),
                    writes=[b_Wt[s]])
                for t, key in ((0, "k"), (1, "v")):
                    for hh in range(2):
                        c0 = jb[key][hh]
                        o0 = 128 + t * 128 + hh * 64
                        dma(pool, c_wt[s], lambda c0=c0, o0=o0: nc.gpsimd.dma_start(out=Wt[s][:, :, o0:o0 + 64], in_=w_in_v[:, :, c0:c0 + 64]),
                            writes=[b_Wt[s]])


            xT_v = xT_d.rearrange("(kc p) t -> p kc t", p=128)
            with ExitStack() as st0:
                sb0 = lambda n, s, t: st0.enter_context(nc.sbuf_tensor("s_" + n, s, t))
                bt_f = sb0("bt_f", [128, 5120], F32)
                mk_f = sb0("mk_f", [128, 5120], F32)
                sk_in = sb0("sk_in", [128, 4], F32)
                memTb = sb0("memTb", [128, 8, 256], BF16)
                Wkv = sb0("Wkv", [128, 8, 1024], BF16)
                b_btf = b_mkf = b_skin = b_memTb = b_Wkv = Buf()
                dma(sp, c_misc, lambda: nc.sync.dma_start(out=bt_f[:], in_=biasT_d[:, :]), writes=[b_btf])
                dma(sp, c_misc, lambda: nc.sync.dma_start(out=mk_f[:], in_=maskT_d[:, :]), writes=[b_mkf])
                dma(sp, c_misc, lambda: nc.sync.dma_start(out=sk_in[:], in_=sinks_d[:, :]), writes=[b_skin])
                dma(pool, c_misc, lambda: nc.gpsimd.dma_start(out=memTb[:], in_=memT_d.rearrange("(kc p) m -> p kc m", p=128)),
                    writes=[b_memTb])
                dma(pool, c_misc, lambda: nc.gpsimd.dma_start(out=Wkv[:], in_=w_kv_d.rearrange("(kc p) n -> p kc n", p=128)),
                    writes=[b_Wkv])
                if upto != "p0" and njobs != 0:
                    load_w(0)
                for i in range(4):
                    dma(pool, c_xtc[i], lambda i=i: nc.gpsimd.dma_start(out=XT[:, :, i * 1024:(i + 1) * 1024],
                                                                      in_=xT_v[:, :, i * 1024:(i + 1) * 1024]), writes=[b_XTc[i]])
                dve.do(lambda: nc.vector.tensor_tensor(out=BT[:].rearrange("p a b -> p (a b)"), in0=bt_f[:], in1=mk_f[:], op=ALU.add),
                       reads=[b_btf, b_mkf], writes=[b_BT])
                act.do(lambda: nc.scalar.activation(out=es[:], in_=sk_in[:], func=AF.Exp), reads=[b_skin], writes=[b_es])
                for h in range(4):
                    pb = h % 2
                    for kc in range(8):
                        pe.do(lambda h=h, kc=kc, pb=pb: nc.tensor.matmul(bank(pb)[:, 0:256], lhsT=Wkv[:, kc, h * 128:(h + 1) * 128],
                                                                          rhs=memTb[:, kc, :], start=(kc == 0), stop=(kc == 7)),
                              reads=[b_Wkv, b_memTb], writes=[b_ps[pb]])
                    act.do(lambda h=h, pb=pb: nc.scalar.activation(out=KmT[:, h, :], in_=bank(pb)[:, 0:256], func=AF.Identity),
                           reads=[b_ps[pb]], writes=[b_KmT])
                for kb in range(2):
                    for kc in range(8):
                        pe.do(lambda kb=kb, kc=kc: nc.tensor.matmul(bank(kb), lhsT=memTb[:, kc, kb * 128:(kb + 1) * 128],
                                                                     rhs=Wkv[:, kc, 512:1024], start=(kc == 0), stop=(kc == 7)),
                              reads=[b_Wkv, b_memTb], writes=[b_ps[kb]])
                    act.do(lambda kb=kb: nc.scalar.activation(out=Vm[:, kb, :], in_=bank(kb), func=AF.Identity),
                           reads=[b_ps[kb]], writes=[b_Vm])
                barrier(skip=c_xtc + c_wt)

            QT = sb1("QT", [128, S], BF16)
            KT = sb1("KT", [128, S], BF16)
            Vt = sb1("Vt", [128, 32, 128], BF16)
            Acc = sb1("Acc", [128, 2, S], F32)
            Yt = [sb1("Yt%d" % i, [128, S], BF16) for i in range(2)]
            Pf = [sb1("Pf%d" % i, [128, 512], F32) for i in range(2)]
            PT = [sb1("PT%d" % i, [128, 512], BF16) for i in range(2)]
            b_QT, b_KT, b_Vt, b_Acc = Buf(), Buf(), Buf(), Buf()
            b_Yt = [Buf(), Buf()]
            b_Pf = [Buf(), Buf()]
            b_PT = [Buf(), Buf()]
            b_ps = [Buf() for _ in range(8)]
            b_nz = [[Buf(), Buf()] for _ in range(4)]
            b_S = [Buf(), Buf()]

            if njobs is not None:
                jobs = jobs[:njobs]
            if jobsel is not None:
                jobs = [jobs[i] for i in jobsel]
            if upto == "p0":
                jobs = []
            ycount = 0
            for ji, jb in enumerate(jobs):
                s = ji % 2
                if ji + 1 < len(jobs):
                    load_w(ji + 1)
                convert_some(4)
                r = jb["r"]
                nb = 32 // r
                for which, dst, bdst in ((0, QT, b_QT), (1, KT, b_KT)):
                    for tc in range(8):
                        pb = tc % 2
                        for kc in range(8):
                            pe.do(lambda kc=kc, tc=tc, pb=pb, which=which: nc.tensor.matmul(
                                bank(pb), lhsT=Wt[s][:, kc, which * 128:(which + 1) * 128], rhs=XT[:, kc, tc * 512:(tc + 1) * 512],
                                start=(kc == 0), stop=(kc == 7)), reads=[b_Wt[s], b_XTc[tc // 2]], writes=[b_ps[pb]])
                        act.do(lambda tc=tc, pb=pb, dst=dst: nc.scalar.activation(out=dst[:, tc * 512:(tc + 1) * 512], in_=bank(pb), func=AF.Identity),
                               reads=[b_ps[pb]], writes=[bdst])

                def tokset(blk):
                    rho, n = blk // nb, blk % nb
                    st_ = r * 128 * n + rho
                    return st_, n

                for bg in (range(8) if stage >= 2 else ()):
                    pb = bg % 2
                    for j in range(4):
                        blk = bg * 4 + j
                        st_, n = tokset(blk)
                        for kc in range(8):
                            pe.do(lambda kc=kc, j=j, pb=pb, st_=st_: nc.tensor.matmul(
                                bank(pb)[:, j * 128:(j + 1) * 128], lhsT=XT[:, kc, st_:st_ + 127 * r + 1:r], rhs=Wt[s][:, kc, 256:384],
                                start=(kc == 0), stop=(kc == 7)), reads=[b_Wt[s]] + b_XTc, writes=[b_ps[pb]])
                    act.do(lambda bg=bg, pb=pb: nc.scalar.activation(out=Vt[:, bg * 4:(bg + 1) * 4, :].rearrange("p a b -> p (a b)"),
                                                                   in_=bank(pb), func=AF.Identity), reads=[b_ps[pb]], writes=[b_Vt])
                bp = jb["bp"]

                def qk(blk):
                    st_, n = tokset(blk)
                    pi = blk % 2
                    qs = slice(st_, st_ + 127 * r + 1, r)
                    ks_c = qs
                    ks_p = slice(st_ - 128 * r, st_ - r + 1, r)
                    sb0 = 2 if pi == 0 else 0
                    bS = [b_ps[sb0], b_ps[sb0 + 1]]
                    for hh in range(2):
                        ps_ = slice(hh * 64, (hh + 1) * 64)
                        if n > 0:
                            pe.do(lambda: nc.tensor.matmul(
                                bank(sb0 + hh)[:, 0:128], lhsT=KT[ps_, ks_p], rhs=QT[ps_, qs], start=True, stop=True),
                                reads=[b_KT, b_QT], writes=bS)
                        pe.do(lambda: nc.tensor.matmul(
                            bank(sb0 + hh)[:, 128:256], lhsT=KT[ps_, ks_c], rhs=QT[ps_, qs], start=True, stop=True),
                            reads=[b_KT, b_QT], writes=bS)

                def softmax(blk):
                    st_, n = tokset(blk)
                    pi = blk % 2
                    sb0 = 2 if pi == 0 else 0
                    bS = [b_ps[sb0], b_ps[sb0 + 1]]
                    Sv = bank(sb0, 2).rearrange("p (b c) -> p b c", c=512)[:, :, 0:256]
                    h3 = lambda ap: ap.rearrange("p (b c) -> p b c", c=256)
                    if n > 0:
                        dve.do(lambda: nc.vector.scalar_tensor_tensor(
                            out=h3(Pf[pi][:]), in0=Sv, scalar=0.125, in1=h3(BT[:, bp, :]), op0=ALU.mult, op1=ALU.add),
                            reads=bS + [b_BT], writes=[b_Pf[pi]])
                        act.do(lambda: nc.scalar.activation(out=PT[pi][:], in_=Pf[pi][:], func=AF.Exp),
                               reads=[b_Pf[pi]], writes=[b_PT[pi]])
                    else:
                        dve.do(lambda: nc.vector.scalar_tensor_tensor(
                            out=h3(Pf[pi][:])[:, :, 128:256], in0=Sv[:, :, 128:256], scalar=0.125, in1=h3(BT[:, bp, :])[:, :, 128:256],
                            op0=ALU.mult, op1=ALU.add), reads=bS + [b_BT], writes=[b_Pf[pi]])
                        act.do(lambda: nc.scalar.activation(out=h3(PT[pi][:])[:, :, 128:256], in_=h3(Pf[pi][:])[:, :, 128:256], func=AF.Exp),
                               reads=[b_Pf[pi]], writes=[b_PT[pi]])

                def pv(blk):
                    st_, n = tokset(blk)
                    pi = blk % 2
                    nzb = blk % 4
                    half = 0
                    co = 0
                    for hh in range(2):
                        ps_ = slice(hh * 64, (hh + 1) * 64)
                        for (oc, lhs_fn) in ((co, lambda b_: Vt[:, b_, ps_]), (co + 128, lambda b_: ones_b[:, 0:64])):
                            if n > 0:
                                pe.do(lambda: nc.tensor.matmul(
                                    bank(4 + nzb)[ps_, oc:oc + 128], lhsT=lhs_fn(blk - 1), rhs=PT[pi][:, (2 * hh) * 128:(2 * hh + 1) * 128],
                                    start=True, stop=False), reads=[b_Vt, b_PT[pi]], writes=[b_nz[nzb][half]])
                            pe.do(lambda: nc.tensor.matmul(
                                bank(4 + nzb)[ps_, oc:oc + 128], lhsT=lhs_fn(blk), rhs=PT[pi][:, (2 * hh + 1) * 128:(2 * hh + 2) * 128],
                                start=(n == 0), stop=True), reads=[b_Vt, b_PT[pi]], writes=[b_nz[nzb][half]])

                def evac(blk):
                    st_, n = tokset(blk)
                    nzb = blk % 4
                    half = 0
                    co = 0
                    accv = Acc[:, :, st_:st_ + 127 * r + 1:r]
                    nzv = bank(4 + nzb)[:, co:co + 256].rearrange("p (a q) -> p a q", q=128)
                    if jb["first"]:
                        dve.do(lambda: nc.vector.tensor_copy(out=accv, in_=nzv), reads=[b_nz[nzb][half]], writes=[b_Acc])
                    else:
                        dve.do(lambda: nc.vector.tensor_tensor(out=accv, in0=nzv, in1=accv, op=ALU.add),
                               reads=[b_nz[nzb][half]], writes=[b_Acc])

                qk(0)
                qk(1)
                softmax(0)
                for blk in range(32):
                    if blk + 2 < 32:
                        qk(blk + 2)
                    if blk + 1 < 32:
                        softmax(blk + 1)
                    pv(blk)
                    evac(blk)
                if jb["last"]:
                    ys = ycount % 2
                    ycount += 1
                    if jb["sink"] is not None:
                        sk = jb["sink"]
                        dve.do(lambda sk=sk: nc.vector.tensor_scalar(out=Acc[:, 1, :], in0=Acc[:, 1, :], scalar1=es[:, sk:sk + 1], scalar2=None, op0=ALU.add),
                               reads=[b_es], writes=[b_Acc])
                    dve.do(lambda: nc.vector.reciprocal(out=Acc[:, 1, :], in_=Acc[:, 1, :]), reads=[], writes=[b_Acc])
                    dve.do(lambda ys=ys: nc.vector.tensor_tensor(out=Yt[ys][:], in0=Acc[:, 0, :], in1=Acc[:, 1, :], op=ALU.mult),
                           reads=[b_Acc], writes=[b_Yt[ys]])
                    row = jb["row"]
                    dma(sp, c_ys[ys], lambda ys=ys, row=row: nc.sync.dma_start(out=yT_s[row:row + 128, :], in_=Yt[ys][:]), reads=[b_Yt[ys]])

            convert_some(64)
            barrier()
            for h in (range(4) if (do_c and upto != "p0") else ()):
                s = h % 2
                c0 = 3072 + h * 128
                dma(pool, c_wt[s], lambda s=s, c0=c0: nc.gpsimd.dma_start(out=Wt[s][:, :, 0:128], in_=w_in_v[:, :, c0:c0 + 128]), writes=[b_Wt[s]])
                for tc in range(8):
                    pb = tc % 2
                    for kc in range(8):
                        pe.do(lambda kc=kc, tc=tc, pb=pb, s=s: nc.tensor.matmul(
                            bank(pb), lhsT=Wt[s][:, kc, 0:128], rhs=XT[:, kc, tc * 512:(tc + 1) * 512], start=(kc == 0), stop=(kc == 7)),
                            reads=[b_Wt[s], b_XTc[tc // 2]], writes=[b_ps[pb]])
                    act.do(lambda tc=tc, pb=pb: nc.scalar.activation(out=QT[:, tc * 512:(tc + 1) * 512], in_=bank(pb), func=AF.Identity),
                           reads=[b_ps[pb]], writes=[b_QT])
                ys = ycount % 2
                ycount += 1
                for tc in range(8):
                    for kb in range(2):
                        sbk = 2 + kb
                        pe.do(lambda kb=kb, tc=tc, sbk=sbk, h=h: nc.tensor.matmul(
                            bank(sbk), lhsT=KmT[:, h, kb * 128:(kb + 1) * 128], rhs=QT[:, tc * 512:(tc + 1) * 512], start=True, stop=True),
                            reads=[b_KmT, b_QT], writes=[b_ps[sbk]])
                        act.do(lambda kb=kb, sbk=sbk: nc.scalar.activation(out=PT[kb][:], in_=bank(sbk), func=AF.Exp, scale=float(128 ** -0.5)),
                               reads=[b_ps[sbk]], writes=[b_PT[kb]])
                    nb_ = 4 + (tc % 2) * 2
                    for kb in range(2):
                        pe.do(lambda kb=kb, nb_=nb_, h=h: nc.tensor.matmul(bank(nb_), lhsT=Vm[:, kb, h * 128:(h + 1) * 128], rhs=PT[kb][:],
                                                                        start=(kb == 0), stop=(kb == 1)), reads=[b_Vm, b_PT[kb]], writes=[b_ps[nb_]])
                    for kb in range(2):
                        pe.do(lambda kb=kb, nb_=nb_: nc.tensor.matmul(bank(nb_ + 1), lhsT=ones_b[:, :], rhs=PT[kb][:],
                                                                    start=(kb == 0), stop=(kb == 1)), reads=[b_PT[kb]], writes=[b_ps[nb_ + 1]])
                    pi = tc % 2
                    dve.do(lambda pi=pi, nb_=nb_: nc.vector.reciprocal(out=Pf[pi][:], in_=bank(nb_ + 1)), reads=[b_ps[nb_ + 1]], writes=[b_Pf[pi]])
                    dve.do(lambda pi=pi, nb_=nb_, tc=tc, ys=ys: nc.vector.tensor_tensor(out=Yt[ys][:, tc * 512:(tc + 1) * 512], in0=bank(nb_), in1=Pf[pi][:], op=ALU.mult),
                           reads=[b_ps[nb_], b_Pf[pi]], writes=[b_Yt[ys]])
                row = 768 + h * 128
                dma(sp, c_ys[ys], lambda ys=ys, row=row: nc.sync.dma_start(out=yT_s[row:row + 128, :], in_=Yt[ys][:]), reads=[b_Yt[ys]])
            barrier()
            st01.close()

            with ExitStack() as st2:
                sb2 = lambda n, s_, t: st2.enter_context(nc.sbuf_tensor("s_" + n, s_, t))
                Wg = sb2("Wg", [128, 8, 3072], BF16)
                Wbr = sb2("Wbr", [128, 10, D], BF16)
                bg = sb2("bg", [128, 24], F32)
                Ych = [sb2("Ych%d" % i, [128, 10, 512], BF16) for i in range(2)]
                mch = [sb2("mch%d" % i, [128, 8, 512], BF16) for i in range(2)]
                gt = [sb2("gt%d" % i, [128, 512], F32) for i in range(3)]
                tt_ = [sb2("tt%d" % i, [128, 512], F32) for i in range(3)]
                b_Wg = b_Wbr = b_bg = Buf()
                b_Ych, b_mch = [Buf(), Buf()], [Buf(), Buf()]
                b_gt = [Buf() for _ in range(3)]
                b_tt = [Buf() for _ in range(3)]
                b_ps = [Buf() for _ in range(8)]
                c_w2, c_ych, c_mst = chan("w2"), [chan("ych0"), chan("ych1")], [chan("mst0"), chan("mst1")]
                for br in range(3):
                    dma(pool, c_w2, lambda br=br: nc.gpsimd.dma_start(out=Wg[:, :, br * 1024:(br + 1) * 1024],
                                                                      in_=w_in_v[:, :, 3584 + br * 1024:3584 + (br + 1) * 1024]), writes=[b_Wg])
                dma(pool, c_w2, lambda: nc.gpsimd.dma_start(out=Wbr[:, 0:2, :], in_=w_a_d.rearrange("(kc p) n -> p kc n", p=128)), writes=[b_Wbr])
                dma(pool, c_w2, lambda: nc.gpsimd.dma_start(out=Wbr[:, 2:6, :], in_=w_b_d.rearrange("(kc p) n -> p kc n", p=128)), writes=[b_Wbr])
                dma(pool, c_w2, lambda: nc.gpsimd.dma_start(out=Wbr[:, 6:10, :], in_=w_c_d.rearrange("(kc p) n -> p kc n", p=128)), writes=[b_Wbr])
                dma(sp, c_w2, lambda: nc.sync.dma_start(out=bg[:], in_=bgate_d[:, :]), writes=[b_bg])
                yT_v = yT_s.rearrange("(c p) t -> p c t", p=128)
                mT_v = mT_s.rearrange("(c p) t -> p c t", p=128)
                brk = ((0, 2), (2, 6), (6, 10))

                def load_y(tc):
                    dma(sp, c_ych[tc % 2], lambda: nc.sync.dma_start(out=Ych[tc % 2][:], in_=yT_v[:, :, tc * 512:(tc + 1) * 512]),
                        writes=[b_Ych[tc % 2]])

                if upto not in ("p0", "p1"):
                    load_y(0)
                for tc in (range(8) if upto not in ("p0", "p1") else ()):
                    s = tc % 2
                    if tc + 1 < 8:
                        load_y(tc + 1)
                    tsl = slice(tc * 512, (tc + 1) * 512)
                    for f in range(8):
                        for br in range(3):
                            gb = br
                            for kc in range(8):
                                pe.do(lambda kc=kc, br=br, f=f, gb=gb: nc.tensor.matmul(
                                    bank(gb), lhsT=Wg[:, kc, br * 1024 + f * 128:br * 1024 + (f + 1) * 128], rhs=XT[:, kc, tsl],
                                    start=(kc == 0), stop=(kc == 7)), reads=[b_Wg, b_XTc[tc // 2]], writes=[b_ps[gb]])
                            act.do(lambda br=br, f=f, gb=gb: nc.scalar.activation(out=gt[br][:], in_=bank(gb), func=AF.Sigmoid,
                                                                                   bias=bg[:, br * 8 + f:br * 8 + f + 1]),
                                   reads=[b_ps[gb], b_bg], writes=[b_gt[br]])
                            k0, k1 = brk[br]
                            for kc in range(k0, k1):
                                pe.do(lambda kc=kc, br=br, f=f, k0=k0, k1=k1: nc.tensor.matmul(
                                    bank(3 + br), lhsT=Wbr[:, kc, f * 128:(f + 1) * 128], rhs=Ych[s][:, kc, :],
                                    start=(kc == k0), stop=(kc == k1 - 1)), reads=[b_Wbr, b_Ych[s]], writes=[b_ps[3 + br]])
                            dve.do(lambda br=br: nc.vector.tensor_tensor(out=tt_[br][:], in0=bank(3 + br), in1=gt[br][:], op=ALU.mult),
                                   reads=[b_ps[3 + br], b_gt[br]], writes=[b_tt[br]])
                        pool.do(lambda: nc.gpsimd.tensor_tensor(out=tt_[0][:], in0=tt_[0][:], in1=tt_[1][:], op=ALU.add),
                                reads=[b_tt[1]], writes=[b_tt[0]])
                        pool.do(lambda f=f: nc.gpsimd.tensor_tensor(out=mch[s][:, f, :], in0=tt_[0][:], in1=tt_[2][:], op=ALU.add),
                                reads=[b_tt[0], b_tt[2]], writes=[b_mch[s]])
                    dma(sp, c_mst[s], lambda s=s: nc.sync.dma_start(out=mT_v[:, :, tsl], in_=mch[s][:]), reads=[b_mch[s]])
                barrier()
        barrier()

        with ExitStack() as st:
            sb = lambda n, s_, t: st.enter_context(nc.sbuf_tensor("s_" + n, s_, t))
            Wout = sb("Wout", [128, 8, D], BF16)
            Wq = sb("Wq", [128, 8, D], BF16)
            skT = sb("skT", [128, 8, 128], F32)
            lnp = sb("lnp", [128, 4, D], F32)
            iota16 = sb("iota16", [128, 16], F32)
            mTc = [sb("mTc%d" % i, [128, 8, 512], BF16) for i in range(2)]
            xt = [sb("xt%d" % i, [128, D], F32) for i in range(2)]
            r1 = sb("r1", [128, D], F32)
            x1 = [sb("x1_%d" % i, [128, D], F32) for i in range(2)]
            x1T = sb("x1T", [128, 8, 128], BF16)
            qT = sb("qT", [128, 8, 128], F32)
            stats = sb("stats", [128, 2, 6], F32)
            mv = sb("mv", [128, 2], F32)
            rstd = sb("rstd", [128, 1], F32)
            nmr = sb("nmr", [128, 1], F32)
            sv = sb("sv", [128, 16, 16], F32)
            si_u = sb("si_u", [128, 16, 16], U32)
            si_f = sb("si_f", [128, 16, 16], F32)
            work = sb("work", [128, 128], F32)
            cand = sb("cand", [128, 8, 256], F32)
            work2 = sb("work2", [128, 256], F32)
            top = sb("top", [128, 8, 16], F32)
            pos_u = sb("pos_u", [128, 8, 16], U32)
            ab_u = sb("ab_u", [128, 2, 128], U32)
            ab_f = sb("ab_f", [128, 2, 128], F32)
            oh = sb("oh", [128, 8, 16, 16], BF16)
            ab_b = sb("ab_b", [128, 2, 128], BF16)
            si_b = sb("si_b", [128, 16, 16], BF16)
            iota_b = sb("iota_b", [128, 16], BF16)
            ij = sb("ij", [128, 2, 128], F32)
            eidx_f = sb("eidx_f", [128, 128], F32)
            eidx_u = [sb("eidx_u%d" % i, [128, 128], U32) for i in range(2)]
            gate = [sb("gate%d" % i, [128, 8, 16], F32) for i in range(2)]
            gsum = sb("gsum", [128, 8], F32)
            hpre = bank(4)[:, 0:128]
            gl = sb("gl", [128, 128], F32)
            x1b = [sb("x1b%d" % i, [128, D], BF16) for i in range(2)]
            prod = [sb("prod%d" % i, [128, D], BF16) for i in range(NP)]
            junk = sb("junk", [128, D], BF16)
            G = [sb("G%d" % i, [128, 2 * D], BF16) for i in range(NG)]
            diag = [sb("diag%d" % i, [128, 128], BF16) for i in range(ND)]
            ident_b = sb("ident_b", [128, 128], BF16)
            alphaI = sb("alphaI", [128, 128], F32)
            r2 = sb("r2", [128, D], F32)
            ot = [sb("ot%d" % i, [128, D], F32) for i in range(2)]
            b_w3 = Buf()
            b_mTc, b_xt, b_ot, b_x1, b_eu, b_gate = ([Buf(), Buf()] for _ in range(6))
            b_r1, b_x1T, b_qT, b_st, b_mv, b_rstd, b_nmr = (Buf() for _ in range(7))
            b_sv, b_siu, b_sif, b_work, b_cand, b_work2, b_top, b_pos = (Buf() for _ in range(8))
            b_abu, b_abf, b_oh, b_ij, b_ef, b_gsum, b_r2, b_idb = (Buf() for _ in range(8))
            b_G = [Buf() for _ in range(NG)]
            b_diag = [Buf() for _ in range(ND)]
            b_hp = [Buf() for _ in range(128)]
            b_gl = [Buf() for _ in range(128)]
            b_x1b = [Buf(), Buf()]
            b_prod = [Buf() for _ in range(NP)]
            b_ps = [Buf() for _ in range(8)]
            c_w3 = chan("w3")
            c_mtc, c_xt_, c_ot = [chan("mtc0"), chan("mtc1")], [chan("xt0"), chan("xt1")], [chan("ot0"), chan("ot1")]
            c_g = [chan("g%d" % i) for i in range(NG)]
            c_dbg = chan("dbg")
            dve.do(lambda: nc.vector.tensor_copy(out=ident_b[:], in_=ident[:]), reads=[b_const], writes=[b_idb])
            dve.do(lambda: nc.vector.tensor_scalar(out=alphaI[:], in0=ident[:], scalar1=float(ALPHA), scalar2=None, op0=ALU.mult), reads=[b_const], writes=[b_idb])
            dma(pool, c_w3, lambda: nc.gpsimd.dma_start(out=Wout[:], in_=w_out_d.rearrange("(kc p) n -> p kc n", p=128)), writes=[b_w3])
            dma(pool, c_w3, lambda: nc.gpsimd.dma_start(out=Wq[:], in_=w_q_d.rearrange("(kc p) n -> p kc n", p=128)), writes=[b_w3])
            dma(sp, c_w3, lambda: nc.sync.dma_start(out=skT[:].rearrange("p a b -> p (a b)"), in_=skT_d[:, :]), writes=[b_w3])
            dma(sp, c_w3, lambda: nc.sync.dma_start(out=lnp[:].rearrange("p a b -> p (a b)"), in_=lnp_d[:, :]), writes=[b_w3])
            dma(sp, c_w3, lambda: nc.sync.dma_start(out=iota16[:], in_=iota_d[:, :]), writes=[b_w3])
            dve.do(lambda: nc.vector.tensor_copy(out=iota_b[:], in_=iota16[:]), reads=[b_w3], writes=[b_w3])
            mT_v = mT_s.rearrange("(c p) t -> p c t", p=128)
            accp = bank(6, 2)
            NTILES = S // 128 if ntiles is None else ntiles
            if upto != "all":
                NTILES = 0

            def layer_norm_steps(L, src, b_src, dst, b_dst, gi):
                rs = b_src if isinstance(b_src, list) else [b_src]

                def s_stats():
                    for hf in range(2):
                        dve.do(lambda hf=hf: nc.vector.bn_stats(out=stats[:, hf, :], in_=src[:, hf * 512:(hf + 1) * 512]), reads=rs, writes=[b_st])
                    dve.do(lambda: nc.vector.bn_aggr(out=mv[:], in_=stats[:].rearrange("p a b -> p (a b)")), reads=[b_st], writes=[b_mv])
                    act.do(lambda: nc.scalar.activation(out=rstd[:], in_=mv[:, 1:2], func=AF.Sqrt, bias=float(LN_EPS), scale=1.0), reads=[b_mv], writes=[b_rstd])
                def s_norm():
                    dve.do(lambda: nc.vector.reciprocal(out=rstd[:], in_=rstd[:]), reads=[], writes=[b_rstd])
                    dve.do(lambda: nc.vector.scalar_tensor_tensor(out=nmr[:], in0=mv[:, 0:1], scalar=-1.0, in1=rstd[:], op0=ALU.mult, op1=ALU.mult),
                           reads=[b_mv, b_rstd], writes=[b_nmr])
                    act.do(lambda: nc.scalar.activation(out=dst[:], in_=src[:, 0:D], func=AF.Identity, bias=nmr[:, 0:1], scale=rstd[:, 0:1]),
                           reads=rs + [b_rstd, b_nmr], writes=[b_dst])
                def s_g():
                    dve.do(lambda: nc.vector.tensor_tensor(out=dst[:], in0=dst[:], in1=lnp[:, gi, :], op=ALU.mult), reads=[b_w3], writes=[b_dst])
                def s_b():
                    dve.do(lambda: nc.vector.tensor_tensor(out=dst[:], in0=dst[:], in1=lnp[:, gi + 1, :], op=ALU.add), reads=[b_w3], writes=[b_dst])
                L.extend([s_stats, s_norm, s_g, s_b])

            def load_tile(tt):
                if tt >= NTILES:
                    return
                if tt % 4 == 0:
                    cc = (tt // 4) % 2
                    dma(sp, c_mtc[cc], lambda: nc.sync.dma_start(out=mTc[cc][:], in_=mT_v[:, :, (tt // 4) * 512:(tt // 4 + 1) * 512]),
                        writes=[b_mTc[cc]])
                dma(sp, c_xt_[tt % 2], lambda: nc.sync.dma_start(out=xt[tt % 2][:], in_=x_d[tt * 128:(tt + 1) * 128, :]), writes=[b_xt[tt % 2]])

            def topk16(L, src_fn, b_src, vals, b_vals, idxs, b_idxs, wk, b_wk):
                def f():
                    src = src_fn()
                    dve.do(lambda: nc.vector.max(out=vals[:, 0:8], in_=src), reads=[b_src], writes=[b_vals])
                    dve.do(lambda: nc.vector.max_index(out=idxs[:, 0:8], in_max=vals[:, 0:8], in_values=src), reads=[b_src, b_vals], writes=[b_idxs])
                    dve.do(lambda: nc.vector.match_replace(out=wk, in_to_replace=vals[:, 0:8], in_values=src, imm_value=-1e30),
                           reads=[b_src, b_vals], writes=[b_wk])
                    dve.do(lambda: nc.vector.max(out=vals[:, 8:16], in_=wk), reads=[b_wk], writes=[b_vals])
                    dve.do(lambda: nc.vector.max_index(out=idxs[:, 8:16], in_max=vals[:, 8:16], in_values=wk), reads=[b_wk, b_vals], writes=[b_idxs])
                L.append(f)

            def front_steps(tt):
                L = []
                if tt >= NTILES:
                    return L
                par = tt % 2
                cc = (tt // 4) % 2
                sub = tt % 4
                xs = tt % 2
                X1 = x1[par]
                bX1 = b_x1[par]
                for hf in range(2):
                    for f0 in (0, 4):
                        def s_wout(hf=hf, f0=f0):
                            for f in range(f0, f0 + 4):
                                pe.do(lambda f=f: nc.tensor.matmul(bank(hf), lhsT=mTc[cc][:, f, sub * 128:(sub + 1) * 128],
                                                                  rhs=Wout[:, f, hf * 512:(hf + 1) * 512], start=(f == 0), stop=False),
                                      reads=[b_mTc[cc], b_w3], writes=[b_ps[hf]])
                            if f0 == 4:
                                pe.do(lambda: nc.tensor.matmul(bank(hf), lhsT=alphaI[:], rhs=xt[xs][:, hf * 512:(hf + 1) * 512], start=False, stop=True),
                                      reads=[b_xt[xs], b_idb], writes=[b_ps[hf]])
                        L.append(s_wout)
                layer_norm_steps(L, bank(0, 2), [b_ps[0], b_ps[1]], X1, bX1, 0)
                if debug:
                    L.append(lambda: dma(sp, c_dbg, lambda: nc.sync.dma_start(out=x1_dbg[tt * 128:(tt + 1) * 128, :], in_=X1[:]), reads=[bX1]))
                L.append(lambda: act.do(lambda: nc.scalar.activation(out=x1b[par][:], in_=X1[:], func=AF.Identity), reads=[bX1], writes=[b_x1b[par]]))
                for f0 in (0, 4):
                    def s_tr(f0=f0):
                        for f in range(f0, f0 + 4):
                            pe.do(lambda f=f: nc.tensor.transpose(out=bank(2, 2)[:, f * 128:(f + 1) * 128], in_=X1[:, f * 128:(f + 1) * 128], identity=ident[:]),
                                  reads=[bX1, b_const], writes=[b_ps[2 + f // 4]])
                    L.append(s_tr)
                L.append(lambda: act.do(lambda: nc.scalar.activation(out=x1T[:].rearrange("p a b -> p (a b)"), in_=bank(2, 2), func=AF.Identity),
                                        reads=[b_ps[2], b_ps[3]], writes=[b_x1T]))
                for h in range(8):
                    def s_q(h=h):
                        for kc in range(8):
                            pe.do(lambda kc=kc: nc.tensor.matmul(bank(0, 2)[:, h * 128:(h + 1) * 128], lhsT=Wq[:, kc, h * 128:(h + 1) * 128],
                                                                rhs=x1T[:, kc, :], start=(kc == 0), stop=(kc == 7)),
                                  reads=[b_w3, b_x1T], writes=[b_ps[h // 4]])
                    L.append(s_q)
                L.append(lambda: act.do(lambda: nc.scalar.activation(out=qT[:].rearrange("p a b -> p (a b)"), in_=bank(0, 2), func=AF.Identity),
                                        reads=[b_ps[0], b_ps[1]], writes=[b_qT]))
                for rnd, b0 in ((0, 2), (1, 0)):
                    def s_sc(rnd=rnd, b0=b0):
                        for hh in range(4):
                            h = rnd * 4 + hh
                            for c in range(2):
                                pr = slice(c * 64, (c + 1) * 64)
                                pe.do(lambda h=h, hh=hh, c=c, pr=pr: nc.tensor.matmul(bank(b0 + c)[:, hh * 128:(hh + 1) * 128], lhsT=qT[pr, h, :],
                                                                                    rhs=skT[pr, h, :], start=True, stop=True),
                                      reads=[b_qT, b_w3], writes=[b_ps[b0 + c]])
                    L.append(s_sc)
                    for hh in range(4):
                        for c in range(2):
                            hc = c * 8 + rnd * 4 + hh
                            topk16(L, (lambda b0=b0, c=c, hh=hh: bank(b0 + c)[:, hh * 128:(hh + 1) * 128]), b_ps[b0 + c],
                                   sv[:, hc, :], b_sv, si_u[:, hc, :], b_siu, work[:], b_work)
                sv4 = sv[:].rearrange("p (c h) k -> p c h k", c=2)
                si4 = si_b[:].rearrange("p (c h) k -> p c h k", c=2)

                def s_cand():
                    dve.do(lambda: nc.vector.tensor_copy(out=si_b[:], in_=si_u[:]), reads=[b_siu], writes=[b_sif])
                    dve.do(lambda: nc.vector.tensor_tensor(out=cand[:].rearrange("p h (a b) -> p h a b", b=16),
                                                           in0=sv4[:, 0, :, :].unsqueeze(3).broadcast_to([128, 8, 16, 16]),
                                                           in1=sv4[:, 1, :, :].unsqueeze(2).broadcast_to([128, 8, 16, 16]), op=ALU.add),
                           reads=[b_sv], writes=[b_cand])
                L.append(s_cand)
                for h in range(8):
                    topk16(L, (lambda h=h: cand[:, h, :]), b_cand, top[:, h, :], b_top, pos_u[:, h, :], b_pos, work2[:], b_work2)
                posf = pos_u[:].rearrange("p h k -> p (h k)")

                def s_ab():
                    dve.do(lambda: nc.vector.tensor_single_scalar(out=ab_u[:, 0, :], in_=posf, scalar=4, op=ALU.logical_shift_right), reads=[b_pos], writes=[b_abu])
                    dve.do(lambda: nc.vector.tensor_single_scalar(out=ab_u[:, 1, :], in_=posf, scalar=15, op=ALU.bitwise_and), reads=[b_pos], writes=[b_abu])
                    dve.do(lambda: nc.vector.tensor_copy(out=ab_b[:], in_=ab_u[:]), reads=[b_abu], writes=[b_abf])
                L.append(s_ab)
                for c in range(2):
                    def s_lk(c=c):
                        abv = ab_b[:, c, :].rearrange("p (h k) -> p h k", k=16)
                        dve.do(lambda: nc.vector.tensor_tensor(out=oh[:], in0=abv.unsqueeze(3).broadcast_to([128, 8, 16, 16]),
                                                               in1=iota_b[:].unsqueeze(1).unsqueeze(1).broadcast_to([128, 8, 16, 16]), op=ALU.is_equal),
                               reads=[b_abf, b_w3], writes=[b_oh])
                        dve.do(lambda: nc.vector.tensor_tensor(out=oh[:], in0=oh[:], in1=si4[:, c, :, :].unsqueeze(2).broadcast_to([128, 8, 16, 16]), op=ALU.mult),
                               reads=[b_sif], writes=[b_oh])
                        dve.do(lambda: nc.vector.tensor_reduce(out=ij[:, c, :], in_=oh[:].rearrange("p h k a -> p (h k) a"), axis=AX.X, op=ALU.add),
                               reads=[b_oh], writes=[b_ij])
                    L.append(s_lk)

                def s_eidx():
                    dve.do(lambda: nc.vector.scalar_tensor_tensor(out=eidx_f[:], in0=ij[:, 0, :], scalar=128.0, in1=ij[:, 1, :], op0=ALU.mult, op1=ALU.add),
                           reads=[b_ij], writes=[b_ef])
                    dve.do(lambda: nc.vector.tensor_copy(out=eidx_u[par][:], in_=eidx_f[:]), reads=[b_ef], writes=[b_eu[par]])
                L.append(s_eidx)
                GT = gate[par]
                bGT = b_gate[par]

                def s_gate1():
                    dve.do(lambda: nc.vector.tensor_tensor(out=GT[:], in0=top[:], in1=top[:, :, 0:1].broadcast_to([128, 8, 16]), op=ALU.subtract),
                           reads=[b_top], writes=[bGT])
                    act.do(lambda: nc.scalar.activation(out=GT[:], in_=GT[:], func=AF.Exp), reads=[], writes=[bGT])
                def s_gate2():
                    dve.do(lambda: nc.vector.tensor_reduce(out=gsum[:], in_=GT[:], axis=AX.X, op=ALU.add), reads=[bGT], writes=[b_gsum])
                    dve.do(lambda: nc.vector.reciprocal(out=gsum[:], in_=gsum[:]), reads=[], writes=[b_gsum])
                    dve.do(lambda: nc.vector.tensor_tensor(out=GT[:], in0=GT[:], in1=gsum[:].unsqueeze(2).broadcast_to([128, 8, 16]), op=ALU.mult),
                           reads=[b_gsum], writes=[bGT])
                L.extend([s_gate1, s_gate2])
                return L

            def slot_head(tt, s_):
                par = tt % 2
                g = s_ % NG
                pb = s_ % NP
                dma(pool, c_g[g], lambda: nc.gpsimd.indirect_dma_start(
                    out=G[g][:], out_offset=None, in_=uv_s[:, :], in_offset=bass.IndirectOffsetOnAxis(ap=eidx_u[par][:, s_:s_ + 1], axis=0)),
                    reads=[b_eu[par]], writes=[b_G[g]])
                dve.do(lambda: nc.vector.tensor_tensor(out=prod[pb][:], in0=G[g][:, 0:D], in1=x1b[par][:], op=ALU.mult),
                       reads=[b_G[g], b_x1b[par]], writes=[b_prod[pb]])
                act.do(lambda: nc.scalar.activation(out=junk[:], in_=prod[pb][:], func=AF.Identity, accum_out=hpre[:, s_:s_ + 1]),
                       reads=[b_prod[pb]], writes=[b_hp[s_]])
                act.do(lambda: nc.scalar.activation(out=gl[:, s_:s_ + 1], in_=hpre[:, s_:s_ + 1], func=AF.Gelu), reads=[b_hp[s_]], writes=[b_gl[s_]])

            def slot_tail(tt, s_):
                par = tt % 2
                g = s_ % NG
                d_ = s_ % ND
                gatef = gate[par][:].rearrange("p h k -> p (h k)")
                dve.do(lambda: nc.vector.tensor_scalar(out=diag[d_][:], in0=ident_b[:], scalar1=gl[:, s_:s_ + 1], scalar2=gatef[:, s_:s_ + 1],
                                                       op0=ALU.mult, op1=ALU.mult),
                       reads=[b_gl[s_], b_gate[par], b_idb], writes=[b_diag[d_]])
                for hf in range(2):
                    pe.do(lambda hf=hf: nc.tensor.matmul(bank(6 + hf), lhsT=diag[d_][:], rhs=G[g][:, D + hf * 512:D + (hf + 1) * 512],
                                                        start=(s_ == 0), stop=(s_ == 127)),
                          reads=[b_diag[d_], b_G[g]], writes=[b_ps[6 + hf]])

            def tail(tt):
                par = tt % 2
                os_ = tt % 2
                if debug:
                    dve.do(lambda: nc.vector.tensor_copy(out=r2[:], in_=accp), reads=[b_ps[6], b_ps[7]], writes=[b_r2])
                    dma(sp, c_dbg, lambda: nc.sync.dma_start(out=yp_dbg[tt * 128:(tt + 1) * 128, :], in_=r2[:]), reads=[b_r2])
                dve.do(lambda: nc.vector.scalar_tensor_tensor(out=r2[:], in0=x1[par][:], scalar=float(ALPHA), in1=accp, op0=ALU.mult, op1=ALU.add),
                       reads=[b_x1[par], b_ps[6], b_ps[7]], writes=[b_r2])
                L = []
                layer_norm_steps(L, r2, b_r2, ot[os_], b_ot[os_], 2)
                for f in L:
                    f()
                dma(sp, c_ot[os_], lambda: nc.sync.dma_start(out=out_d[tt * 128:(tt + 1) * 128, :], in_=ot[os_][:]), reads=[b_ot[os_]])

            load_tile(0)
            load_tile(1)
            for f in front_steps(0):
                f()
            for tt in range(NTILES):
                load_tile(tt + 2)
                nxt = front_steps(tt + 1)
                k = 0
                for s_ in range(128 + LAG):
                    if s_ < 128:
                        slot_head(tt, s_)
                    if s_ >= LAG:
                        slot_tail(tt, s_ - LAG)
                    if s_ < 128:
                        tgt = ((s_ + 1) * len(nxt)) // 128
                        while k < tgt:
                            nxt[k]()
                            k += 1
                tail(tt)
            barrier()
    return nc


_NC_CACHE = {}


def _host_inputs(x, mem, rel_bias, w_in, b_gate, w_mem_kv, sinks, w_branch_a, w_branch_b, w_branch_c, w_out, ln1_g, ln1_b,
                 peer_w_query, peer_sub_keys, peer_u, peer_v, ln2_g, ln2_b):
    f = lambda a: np.ascontiguousarray(np.asarray(a, dtype=np.float32))
    pairs, bucket, mask, heads = _bias_tables()
    rb = np.asarray(rel_bias, np.float32)
    biasT = np.zeros((128, 10, 4, 128), np.float32)
    maskT = np.zeros((128, 10, 4, 128), np.float32)
    for p in range(10):
        for hh in range(2):
            for kb in range(2):
                biasT[:, p, 2 * hh + kb, :] = rb[bucket[p, kb], heads[p] + hh]
                maskT[:, p, 2 * hh + kb, :] = mask[p, kb]
    sk = np.asarray(sinks, np.float32)[0]
    sinksP = np.zeros((128, 4), np.float32)
    for p in range(4):
        sinksP[0:64, p] = sk[2 * p]
        sinksP[64:128, p] = sk[2 * p + 1]
    lnp = np.stack([np.asarray(a, np.float32)[0] for a in (ln1_g, ln1_b, ln2_g, ln2_b)], 0)
    lnp = np.ascontiguousarray(np.broadcast_to(lnp.reshape(1, 4 * D), (128, 4 * D)))
    skT = np.asarray(peer_sub_keys, np.float32)[0].transpose(1, 3, 0, 2).reshape(128, 8 * 128)
    shared = {
        "w_in": f(w_in[0]), "w_mem_kv": f(w_mem_kv[0]), "w_a": f(w_branch_a[0]), "w_b": f(w_branch_b[0]), "w_c": f(w_branch_c[0]),
        "w_out": f(w_out[0]), "w_q": f(peer_w_query[0]), "skT": f(skT), "peer_u": f(peer_u[0]), "peer_v": f(peer_v[0]),
        "biasT": f(biasT.reshape(128, 5120)), "maskT": f(maskT.reshape(128, 5120)),
        "bgate": f(np.asarray(b_gate, np.float32)[0].reshape(24, 128).T), "sinksP": f(sinksP), "lnp": f(lnp),
        "ident": np.eye(128, dtype=np.float32), "iota16": f(np.broadcast_to(np.arange(16, dtype=np.float32), (128, 16))),
    }
    x = np.asarray(x, np.float32)
    mem = np.asarray(mem, np.float32)
    in_maps = []
    for b in range(x.shape[0]):
        m = dict(shared)
        m["x"] = f(x[b])
        m["xT"] = f(x[b].T)
        m["memT"] = f(mem[b].T)
        in_maps.append(m)
    return in_maps


def kernel(**inputs):
    in_maps = _host_inputs(**inputs)
    if "nc" not in _NC_CACHE:
        _NC_CACHE["nc"] = build_nc()
    nc = _NC_CACHE["nc"]
    res = run_bass_kernel_spmd(nc, in_maps, core_ids=list(range(NCORES)))
    return np.stack([np.asarray(r["out"], dtype=np.float32) for r in res.results], 0)
```

```python
import numpy as np
import concourse.bass as bass
import concourse.mybir as mybir
from concourse.bass_utils import run_bass_kernel_spmd
from contextlib import ExitStack

F32 = mybir.dt.float32
BF16 = mybir.dt.bfloat16
U32 = mybir.dt.uint32
AF = mybir.ActivationFunctionType
ALU = mybir.AluOpType
AX = mybir.AxisListType

S = 4096
D = 1024
NCORES = 8
ALPHA = 2.0 ** 0.25
LN_EPS = 1e-5
NEG = -30000.0
N_EXP = 16384
NG = 12
ND = 6
LAG = 3
NP = 4


class Buf:
    __slots__ = ("w", "r")

    def __init__(self):
        self.w = None
        self.r = {}


class Q:
    def __init__(self, nc, eng, st, name, is_pe=False):
        self.nc = nc
        self.eng = eng
        self.sem = st.enter_context(nc.semaphore("q_" + name))
        self.n = 0
        self.seen = {}
        self.is_pe = is_pe

    def wait(self, ev):
        if ev is None:
            return
        sem, val = ev
        if sem is self.sem and self.is_pe:
            return
        k = id(sem)
        if self.seen.get(k, -1) >= val:
            return
        self.eng.wait_ge(sem, val)
        self.seen[k] = val

    def deps(self, reads, writes, extra, skip_sem=None):
        for b in reads:
            if b.w is not None and b.w[0] is not skip_sem:
                self.wait(b.w)
        for b in writes:
            if b.w is not None and b.w[0] is not skip_sem:
                self.wait(b.w)
            for ev in b.r.values():
                self.wait(ev)
        for ev in extra:
            self.wait(ev)

    @staticmethod
    def mark(ev, reads, writes):
        for b in reads:
            k = id(ev[0])
            old = b.r.get(k)
            if old is None or old[1] < ev[1]:
                b.r[k] = ev
        for b in writes:
            b.w = ev
            b.r = {}

    def do(self, fn, reads=(), writes=(), extra=()):
        self.deps(reads, writes, extra)
        ins = fn()
        self.n += 1
        ins.then_inc(self.sem, 1)
        ev = (self.sem, self.n)
        self.mark(ev, reads, writes)
        return ev


class DmaChan:
    def __init__(self, nc, st, name):
        self.sem = st.enter_context(nc.semaphore("c_" + name))
        self.n = 0


def dma(q, chan, fn, reads=(), writes=(), extra=()):
    q.deps(reads, writes, extra, skip_sem=chan.sem)
    ins = fn()
    chan.n += 16
    ins.then_inc(chan.sem, 16)
    ev = (chan.sem, chan.n)
    Q.mark(ev, reads, writes)
    return ev


class SpQ(Q):
    def __init__(self, nc, eng):
        self.nc = nc
        self.eng = eng
        self.sem = None
        self.n = 0
        self.seen = {}
        self.is_pe = False


def _t5_bucket(dist):
    n = np.asarray(dist, dtype=np.int32)
    max_exact = 16
    nf = np.maximum(n, 1).astype(np.float32)
    scale = np.float32(np.log(2048 / max_exact))
    large = max_exact + (np.log(nf / np.float32(max_exact)) / scale * np.float32(32 - max_exact)).astype(np.int32)
    large = np.minimum(large, 31)
    return np.where(n < max_exact, n, large).astype(np.int32)


def _bias_tables():
    i = np.arange(128)[None, :]
    j = np.arange(128)[:, None]
    off_prev = 128 + i - j
    off_cur = i - j
    pairs = []
    for g, r in enumerate((1, 4, 16)):
        for m in range(2):
            pairs.append(("A", r, g * 4 + 2 * m))
    for m in range(4):
        pairs.append(("B", 1, 12 + 2 * m))
    bucket = np.zeros((10, 2, 128, 128), np.int32)
    mask = np.zeros((10, 2, 128, 128), np.float32)
    heads = []
    for p, (kind, r, h0) in enumerate(pairs):
        W = 128 if kind == "A" else 127
        for kb, off in enumerate((off_prev, off_cur)):
            bucket[p, kb] = _t5_bucket(np.clip(off, 0, W) * r)
            valid = (off >= 0) & (off <= W)
            mask[p, kb] = np.where(valid, 0.0, NEG)
        heads.append(h0)
    return pairs, bucket, mask, heads


def build_nc(debug=False, upto="all", ntiles=None, njobs=None, do_c=True, stage=99, jobsel=None):
    nc = bass.Bass("TRN2", target_bir_lowering=False)
    dt_in = lambda n, s, t=F32: nc.dram_tensor(n, s, t, kind="ExternalInput").ap()
    xT_d = dt_in("xT", [D, S])
    x_d = dt_in("x", [S, D])
    memT_d = dt_in("memT", [D, 256])
    w_in_d = dt_in("w_in", [D, 6656])
    w_kv_d = dt_in("w_mem_kv", [D, 1024])
    w_a_d = dt_in("w_a", [256, D])
    w_b_d = dt_in("w_b", [512, D])
    w_c_d = dt_in("w_c", [512, D])
    w_out_d = dt_in("w_out", [D, D])
    w_q_d = dt_in("w_q", [D, D])
    skT_d = dt_in("skT", [128, 8 * 128])
    pu_d = dt_in("peer_u", [N_EXP, D])
    pv_d = dt_in("peer_v", [N_EXP, D])
    biasT_d = dt_in("biasT", [128, 10 * 512])
    maskT_d = dt_in("maskT", [128, 10 * 512])
    bgate_d = dt_in("bgate", [128, 24])
    sinks_d = dt_in("sinksP", [128, 4])
    lnp_d = dt_in("lnp", [128, 4 * D])
    ident_d = dt_in("ident", [128, 128])
    iota_d = dt_in("iota16", [128, 16])
    out_d = nc.dram_tensor("out", [S, D], F32, kind="ExternalOutput").ap()
    skind = "ExternalOutput" if debug else "Internal"
    yT_s = nc.dram_tensor("yT_scr", [1280, S], BF16, kind=skind).ap()
    mT_s = nc.dram_tensor("mT_scr", [D, S], BF16, kind=skind).ap()
    uv_s = nc.dram_tensor("uv_scr", [N_EXP, 2 * D], BF16, kind="Internal").ap()
    if debug:
        x1_dbg = nc.dram_tensor("x1_dbg", [S, D], F32, kind="ExternalOutput").ap()
        yp_dbg = nc.dram_tensor("yp_dbg", [S, D], F32, kind="ExternalOutput").ap()

    with ExitStack() as gst:
        pe = Q(nc, nc.tensor, gst, "pe", is_pe=True)
        act = Q(nc, nc.scalar, gst, "act")
        dve = Q(nc, nc.vector, gst, "dve")
        pool = Q(nc, nc.gpsimd, gst, "pool")
        sp = SpQ(nc, nc.sync)
        queues = [pe, act, dve, pool, sp]
        chans = []

        def chan(name):
            c = DmaChan(nc, gst, name)
            chans.append(c)
            return c

        def barrier(skip=()):
            for q in queues:
                for o in (pe, act, dve, pool):
                    if o is not q and o.n > 0:
                        q.wait((o.sem, o.n))
                for c in chans:
                    if c.n > 0 and c not in skip:
                        q.wait((c.sem, c.n))

        psall = gst.enter_context(nc.psum_tensor("psall", [128, 4096], F32))

        def bank(i, n=1):
            return psall[:, i * 512:(i + n) * 512]

        ident = gst.enter_context(nc.sbuf_tensor("s_ident", [128, 128], F32))
        ones_b = gst.enter_context(nc.sbuf_tensor("s_ones_b", [128, 128], BF16))
        c_const = chan("const")
        b_const = Buf()
        dma(sp, c_const, lambda: nc.sync.dma_start(out=ident[:], in_=ident_d[:, :]), writes=[b_const])
        dve.do(lambda: nc.vector.memset(ones_b[:], 1.0), writes=[b_const])

        w_in_v = w_in_d.rearrange("(kc p) n -> p kc n", p=128)

        c_cv = chan("cv")
        cv_list = [(tab, c0, c) for (tab, c0) in ((pu_d, 0), (pv_d, D)) for c in range(16)]

        def convert_some(k):
            for _ in range(k):
                if not cv_list:
                    return
                tab, c0, c = cv_list.pop(0)
                src = tab[c * 1024:(c + 1) * 1024, :].rearrange("(p r) d -> p r d", p=128)
                dst = uv_s[c * 1024:(c + 1) * 1024, c0:c0 + D].rearrange("(p r) d -> p r d", p=128)
                dma(pool, c_cv, lambda: nc.gpsimd.dma_start(out=dst, in_=src))

        with ExitStack() as st:
            sb = lambda n, s, t: st.enter_context(nc.sbuf_tensor("s_" + n, s, t))
            XT = sb("XT", [128, 8, S], BF16)
            b_XTc = [Buf() for _ in range(4)]
            c_xtc = [chan("xc%d" % i) for i in range(4)]
            st01 = st.enter_context(ExitStack())
            sb1 = lambda n, s, t: st01.enter_context(nc.sbuf_tensor("s_" + n, s, t))
            BT = sb1("BT", [128, 10, 512], BF16)
            es = sb1("es", [128, 4], F32)
            KmT = sb1("KmT", [128, 4, 256], BF16)
            Vm = sb1("Vm", [128, 2, 512], BF16)
            b_BT, b_es, b_KmT, b_Vm = Buf(), Buf(), Buf(), Buf()
            b_ps = [Buf() for _ in range(8)]
            c_misc = chan("misc")

            Wt = [sb1("Wt%d" % i, [128, 8, 384], BF16) for i in range(2)]
            b_Wt = [Buf(), Buf()]
            c_wt, c_ys = [chan("wt0"), chan("wt1")], [chan("ys0"), chan("ys1")]
            jobs = []
            for m in range(2):
                for g, r in enumerate((1, 4, 16)):
                    c = g * 256 + 2 * m * 64
                    jobs.append(dict(q=c, k=(768 + c, 768 + c + 64), v=(1536 + c, 1536 + c + 64), r=r, bp=g * 2 + m,
                                     first=(g == 0), last=(g == 2), sink=None, row=m * 128))
            for kv in range(2):
                for m in range(2):
                    hq = kv * 4 + 2 * m
                    jobs.append(dict(q=2304 + hq * 64, k=(2816 + kv * 64,) * 2, v=(2944 + kv * 64,) * 2, r=1, bp=6 + kv * 2 + m,
                                     first=True, last=True, sink=kv * 2 + m, row=256 + (kv * 2 + m) * 128))

            def load_w(ji):
                jb = jobs[ji]
                s = ji % 2
                dma(pool, c_wt[s], lambda: nc.gpsimd.dma_start(out=Wt[s][:, :, 0:128], in_=w_in_v[:, :, jb["q"]:jb["q"] + 128]),
                    writes=[b_Wt[s]])
                for t, key in ((0, "k"), (1, "v")):
                    for hh in range(2):
                        c0 = jb[key][hh]
                        o0 = 128 + t * 128 + hh * 64
                        dma(pool, c_wt[s], lambda c0=c0, o0=o0: nc.gpsimd.dma_start(out=Wt[s][:, :, o0:o0 + 64], in_=w_in_v[:, :, c0:c0 + 64]),
                            writes=[b_Wt[s]])


            xT_v = xT_d.rearrange("(kc p) t -> p kc t", p=128)
            with ExitStack() as st0:
                sb0 = lambda n, s, t: st0.enter_context(nc.sbuf_tensor("s_" + n, s, t))
                bt_f = sb0("bt_f", [128, 5120], F32)
                mk_f = sb0("mk_f", [128, 5120], F32)
                sk_in = sb0("sk_in", [128, 4], F32)
                memTb = sb0("memTb", [128, 8, 256], BF16)
                Wkv = sb0("Wkv", [128, 8, 1024], BF16)
                b_btf = b_mkf = b_skin = b_memTb = b_Wkv = Buf()
                dma(sp, c_misc, lambda: nc.sync.dma_start(out=bt_f[:], in_=biasT_d[:, :]), writes=[b_btf])
                dma(sp, c_misc, lambda: nc.sync.dma_start(out=mk_f[:], in_=maskT_d[:, :]), writes=[b_mkf])
                dma(sp, c_misc, lambda: nc.sync.dma_start(out=sk_in[:], in_=sinks_d[:, :]), writes=[b_skin])
                dma(pool, c_misc, lambda: nc.gpsimd.dma_start(out=memTb[:], in_=memT_d.rearrange("(kc p) m -> p kc m", p=128)),
                    writes=[b_memTb])
                dma(pool, c_misc, lambda: nc.gpsimd.dma_start(out=Wkv[:], in_=w_kv_d.rearrange("(kc p) n -> p kc n", p=128)),
                    writes=[b_Wkv])
                if upto != "p0" and njobs != 0:
                    load_w(0)
                for i in range(4):
                    dma(pool, c_xtc[i], lambda i=i: nc.gpsimd.dma_start(out=XT[:, :, i * 1024:(i + 1) * 1024],
                                                                      in_=xT_v[:, :, i * 1024:(i + 1) * 1024]), writes=[b_XTc[i]])
                dve.do(lambda: nc.vector.tensor_tensor(out=BT[:].rearrange("p a b -> p (a b)"), in0=bt_f[:], in1=mk_f[:], op=ALU.add),
                       reads=[b_btf, b_mkf], writes=[b_BT])
                act.do(lambda: nc.scalar.activation(out=es[:], in_=sk_in[:], func=AF.Exp), reads=[b_skin], writes=[b_es])
                for h in range(4):
                    pb = h % 2
                    for kc in range(8):
                        pe.do(lambda h=h, kc=kc, pb=pb: nc.tensor.matmul(bank(pb)[:, 0:256], lhsT=Wkv[:, kc, h * 128:(h + 1) * 128],
                                                                          rhs=memTb[:, kc, :], start=(kc == 0), stop=(kc == 7)),
                              reads=[b_Wkv, b_memTb], writes=[b_ps[pb]])
                    act.do(lambda h=h, pb=pb: nc.scalar.activation(out=KmT[:, h, :], in_=bank(pb)[:, 0:256], func=AF.Identity),
                           reads=[b_ps[pb]], writes=[b_KmT])
                for kb in range(2):
                    for kc in range(8):
                        pe.do(lambda kb=kb, kc=kc: nc.tensor.matmul(bank(kb), lhsT=memTb[:, kc, kb * 128:(kb + 1) * 128],
                                                                     rhs=Wkv[:, kc, 512:1024], start=(kc == 0), stop=(kc == 7)),
                              reads=[b_Wkv, b_memTb], writes=[b_ps[kb]])
                    act.do(lambda kb=kb: nc.scalar.activation(out=Vm[:, kb, :], in_=bank(kb), func=AF.Identity),
                           reads=[b_ps[kb]], writes=[b_Vm])
                barrier(skip=c_xtc + c_wt)

            QT = sb1("QT", [128, S], BF16)
            KT = sb1("KT", [128, S], BF16)
            Vt = sb1("Vt", [128, 32, 128], BF16)
            Acc = sb1("Acc", [128, 2, S], F32)
            Yt = [sb1("Yt%d" % i, [128, S], BF16) for i in range(2)]
            Pf = [sb1("Pf%d" % i, [128, 512], F32) for i in range(2)]
            PT = [sb1("PT%d" % i, [128, 512], BF16) for i in range(2)]
            b_QT, b_KT, b_Vt, b_Acc = Buf(), Buf(), Buf(), Buf()
            b_Yt = [Buf(), Buf()]
            b_Pf = [Buf(), Buf()]
            b_PT = [Buf(), Buf()]
            b_ps = [Buf() for _ in range(8)]
            b_nz = [[Buf(), Buf()] for _ in range(4)]
            b_S = [Buf(), Buf()]

            if njobs is not None:
                jobs = jobs[:njobs]
            if jobsel is not None:
                jobs = [jobs[i] for i in jobsel]
            if upto == "p0":
                jobs = []
            ycount = 0
            for ji, jb in enumerate(jobs):
                s = ji % 2
                if ji + 1 < len(jobs):
                    load_w(ji + 1)
                convert_some(4)
                r = jb["r"]
                nb = 32 // r
                for which, dst, bdst in ((0, QT, b_QT), (1, KT, b_KT)):
                    for tc in range(8):
                        pb = tc % 2
                        for kc in range(8):
                            pe.do(lambda kc=kc, tc=tc, pb=pb, which=which: nc.tensor.matmul(
                                bank(pb), lhsT=Wt[s][:, kc, which * 128:(which + 1) * 128], rhs=XT[:, kc, tc * 512:(tc + 1) * 512],
                                start=(kc == 0), stop=(kc == 7)), reads=[b_Wt[s], b_XTc[tc // 2]], writes=[b_ps[pb]])
                        act.do(lambda tc=tc, pb=pb, dst=dst: nc.scalar.activation(out=dst[:, tc * 512:(tc + 1) * 512], in_=bank(pb), func=AF.Identity),
                               reads=[b_ps[pb]], writes=[bdst])

                def tokset(blk):
                    rho, n = blk // nb, blk % nb
                    st_ = r * 128 * n + rho
                    return st_, n

                for bg in (range(8) if stage >= 2 else ()):
                    pb = bg % 2
                    for j in range(4):
                        blk = bg * 4 + j
                        st_, n = tokset(blk)
                        for kc in range(8):
                            pe.do(lambda kc=kc, j=j, pb=pb, st_=st_: nc.tensor.matmul(
                                bank(pb)[:, j * 128:(j + 1) * 128], lhsT=XT[:, kc, st_:st_ + 127 * r + 1:r], rhs=Wt[s][:, kc, 256:384],
                                start=(kc == 0), stop=(kc == 7)), reads=[b_Wt[s]] + b_XTc, writes=[b_ps[pb]])
                    act.do(lambda bg=bg, pb=pb: nc.scalar.activation(out=Vt[:, bg * 4:(bg + 1) * 4, :].rearrange("p a b -> p (a b)"),
                                                                   in_=bank(pb), func=AF.Identity), reads=[b_ps[pb]], writes=[b_Vt])
                bp = jb["bp"]

                def qk(blk):
                    st_, n = tokset(blk)
                    pi = blk % 2
                    qs = slice(st_, st_ + 127 * r + 1, r)
                    ks_c = qs
                    ks_p = slice(st_ - 128 * r, st_ - r + 1, r)
                    sb0 = 2 if pi == 0 else 0
                    bS = [b_ps[sb0], b_ps[sb0 + 1]]
                    for hh in range(2):
                        ps_ = slice(hh * 64, (hh + 1) * 64)
                        if n > 0:
                            pe.do(lambda: nc.tensor.matmul(
                                bank(sb0 + hh)[:, 0:128], lhsT=KT[ps_, ks_p], rhs=QT[ps_, qs], start=True, stop=True),
                                reads=[b_KT, b_QT], writes=bS)
                        pe.do(lambda: nc.tensor.matmul(
                            bank(sb0 + hh)[:, 128:256], lhsT=KT[ps_, ks_c], rhs=QT[ps_, qs], start=True, stop=True),
                            reads=[b_KT, b_QT], writes=bS)

                def softmax(blk):
                    st_, n = tokset(blk)
                    pi = blk % 2
                    sb0 = 2 if pi == 0 else 0
                    bS = [b_ps[sb0], b_ps[sb0 + 1]]
                    Sv = bank(sb0, 2).rearrange("p (b c) -> p b c", c=512)[:, :, 0:256]
                    h3 = lambda ap: ap.rearrange("p (b c) -> p b c", c=256)
                    if n > 0:
                        dve.do(lambda: nc.vector.scalar_tensor_tensor(
                            out=h3(Pf[pi][:]), in0=Sv, scalar=0.125, in1=h3(BT[:, bp, :]), op0=ALU.mult, op1=ALU.add),
                            reads=bS + [b_BT], writes=[b_Pf[pi]])
                        act.do(lambda: nc.scalar.activation(out=PT[pi][:], in_=Pf[pi][:], func=AF.Exp),
                               reads=[b_Pf[pi]], writes=[b_PT[pi]])
                    else:
                        dve.do(lambda: nc.vector.scalar_tensor_tensor(
                            out=h3(Pf[pi][:])[:, :, 128:256], in0=Sv[:, :, 128:256], scalar=0.125, in1=h3(BT[:, bp, :])[:, :, 128:256],
                            op0=ALU.mult, op1=ALU.add), reads=bS + [b_BT], writes=[b_Pf[pi]])
                        act.do(lambda: nc.scalar.activation(out=h3(PT[pi][:])[:, :, 128:256], in_=h3(Pf[pi][:])[:, :, 128:256], func=AF.Exp),
                               reads=[b_Pf[pi]], writes=[b_PT[pi]])

                def pv(blk):
                    st_, n = tokset(blk)
                    pi = blk % 2
                    nzb = blk % 4
                    half = 0
                    co = 0
                    for hh in range(2):
                        ps_ = slice(hh * 64, (hh + 1) * 64)
                        for (oc, lhs_fn) in ((co, lambda b_: Vt[:, b_, ps_]), (co + 128, lambda b_: ones_b[:, 0:64])):
                            if n > 0:
                                pe.do(lambda: nc.tensor.matmul(
                                    bank(4 + nzb)[ps_, oc:oc + 128], lhsT=lhs_fn(blk - 1), rhs=PT[pi][:, (2 * hh) * 128:(2 * hh + 1) * 128],
                                    start=True, stop=False), reads=[b_Vt, b_PT[pi]], writes=[b_nz[nzb][half]])
                            pe.do(lambda: nc.tensor.matmul(
                                bank(4 + nzb)[ps_, oc:oc + 128], lhsT=lhs_fn(blk), rhs=PT[pi][:, (2 * hh + 1) * 128:(2 * hh + 2) * 128],
                                start=(n == 0), stop=True), reads=[b_Vt, b_PT[pi]], writes=[b_nz[nzb][half]])

                def evac(blk):
                    st_, n = tokset(blk)
                    nzb = blk % 4
                    half = 0
                    co = 0
                    accv = Acc[:, :, st_:st_ + 127 * r + 1:r]
                    nzv = bank(4 + nzb)[:, co:co + 256].rearrange("p (a q) -> p a q", q=128)
                    if jb["first"]:
                        dve.do(lambda: nc.vector.tensor_copy(out=accv, in_=nzv), reads=[b_nz[nzb][half]], writes=[b_Acc])
                    else:
                        dve.do(lambda: nc.vector.tensor_tensor(out=accv, in0=nzv, in1=accv, op=ALU.add),
                               reads=[b_nz[nzb][half]], writes=[b_Acc])

                qk(0)
                qk(1)
                softmax(0)
                for blk in range(32):
                    if blk + 2 < 32:
                        qk(blk + 2)
                    if blk + 1 < 32:
                        softmax(blk + 1)
                    pv(blk)
                    evac(blk)
                if jb["last"]:
                    ys = ycount % 2
                    ycount += 1
                    if jb["sink"] is not None:
                        sk = jb["sink"]
                        dve.do(lambda sk=sk: nc.vector.tensor_scalar(out=Acc[:, 1, :], in0=Acc[:, 1, :], scalar1=es[:, sk:sk + 1], scalar2=None, op0=ALU.add),
                               reads=[b_es], writes=[b_Acc])
                    dve.do(lambda: nc.vector.reciprocal(out=Acc[:, 1, :], in_=Acc[:, 1, :]), reads=[], writes=[b_Acc])
                    dve.do(lambda ys=ys: nc.vector.tensor_tensor(out=Yt[ys][:], in0=Acc[:, 0, :], in1=Acc[:, 1, :], op=ALU.mult),
                           reads=[b_Acc], writes=[b_Yt[ys]])
                    row = jb["row"]
                    dma(sp, c_ys[ys], lambda ys=ys, row=row: nc.sync.dma_start(out=yT_s[row:row + 128, :], in_=Yt[ys][:]), reads=[b_Yt[ys]])

            convert_some(64)
            barrier()
            for h in (range(4) if (do_c and upto != "p0") else ()):
                s = h % 2
                c0 = 3072 + h * 128
                dma(pool, c_wt[s], lambda s=s, c0=c0: nc.gpsimd.dma_start(out=Wt[s][:, :, 0:128], in_=w_in_v[:, :, c0:c0 + 128]), writes=[b_Wt[s]])
                for tc in range(8):
                    pb = tc % 2
                    for kc in range(8):
                        pe.do(lambda kc=kc, tc=tc, pb=pb, s=s: nc.tensor.matmul(
                            bank(pb), lhsT=Wt[s][:, kc, 0:128], rhs=XT[:, kc, tc * 512:(tc + 1) * 512], start=(kc == 0), stop=(kc == 7)),
                            reads=[b_Wt[s], b_XTc[tc // 2]], writes=[b_ps[pb]])
                    act.do(lambda tc=tc, pb=pb: nc.scalar.activation(out=QT[:, tc * 512:(tc + 1) * 512], in_=bank(pb), func=AF.Identity),
                           reads=[b_ps[pb]], writes=[b_QT])
                ys = ycount % 2
                ycount += 1
                for tc in range(8):
                    for kb in range(2):
                        sbk = 2 + kb
                        pe.do(lambda kb=kb, tc=tc, sbk=sbk, h=h: nc.tensor.matmul(
                            bank(sbk), lhsT=KmT[:, h, kb * 128:(kb + 1) * 128], rhs=QT[:, tc * 512:(tc + 1) * 512], start=True, stop=True),
                            reads=[b_KmT, b_QT], writes=[b_ps[sbk]])
                        act.do(lambda kb=kb, sbk=sbk: nc.scalar.activation(out=PT[kb][:], in_=bank(sbk), func=AF.Exp, scale=float(128 ** -0.5)),
                               reads=[b_ps[sbk]], writes=[b_PT[kb]])
                    nb_ = 4 + (tc % 2) * 2
                    for kb in range(2):
                        pe.do(lambda kb=kb, nb_=nb_, h=h: nc.tensor.matmul(bank(nb_), lhsT=Vm[:, kb, h * 128:(h + 1) * 128], rhs=PT[kb][:],
                                                                        start=(kb == 0), stop=(kb == 1)), reads=[b_Vm, b_PT[kb]], writes=[b_ps[nb_]])
                    for kb in range(2):
                        pe.do(lambda kb=kb, nb_=nb_: nc.tensor.matmul(bank(nb_ + 1), lhsT=ones_b[:, :], rhs=PT[kb][:],
                                                                    start=(kb == 0), stop=(kb == 1)), reads=[b_PT[kb]], writes=[b_ps[nb_ + 1]])
                    pi = tc % 2
                    dve.do(lambda pi=pi, nb_=nb_: nc.vector.reciprocal(out=Pf[pi][:], in_=bank(nb_ + 1)), reads=[b_ps[nb_ + 1]], writes=[b_Pf[pi]])
                    dve.do(lambda pi=pi, nb_=nb_, tc=tc, ys=ys: nc.vector.tensor_tensor(out=Yt[ys][:, tc * 512:(tc + 1) * 512], in0=bank(nb_), in1=Pf[pi][:], op=ALU.mult),
                           reads=[b_ps[nb_], b_Pf[pi]], writes=[b_Yt[ys]])
                row = 768 + h * 128
                dma(sp, c_ys[ys], lambda ys=ys, row=row: nc.sync.dma_start(out=yT_s[row:row + 128, :], in_=Yt[ys][:]), reads=[b_Yt[ys]])
            barrier()
            st01.close()

            with ExitStack() as st2:
                sb2 = lambda n, s_, t: st2.enter_context(nc.sbuf_tensor("s_" + n, s_, t))
                Wg = sb2("Wg", [128, 8, 3072], BF16)
                Wbr = sb2("Wbr", [128, 10, D], BF16)
                bg = sb2("bg", [128, 24], F32)
                Ych = [sb2("Ych%d" % i, [128, 10, 512], BF16) for i in range(2)]
                mch = [sb2("mch%d" % i, [128, 8, 512], BF16) for i in range(2)]
                gt = [sb2("gt%d" % i, [128, 512], F32) for i in range(3)]
                tt_ = [sb2("tt%d" % i, [128, 512], F32) for i in range(3)]
                b_Wg = b_Wbr = b_bg = Buf()
                b_Ych, b_mch = [Buf(), Buf()], [Buf(), Buf()]
                b_gt = [Buf() for _ in range(3)]
                b_tt = [Buf() for _ in range(3)]
                b_ps = [Buf() for _ in range(8)]
                c_w2, c_ych, c_mst = chan("w2"), [chan("ych0"), chan("ych1")], [chan("mst0"), chan("mst1")]
                for br in range(3):
                    dma(pool, c_w2, lambda br=br: nc.gpsimd.dma_start(out=Wg[:, :, br * 1024:(br + 1) * 1024],
                                                                      in_=w_in_v[:, :, 3584 + br * 1024:3584 + (br + 1) * 1024]), writes=[b_Wg])
                dma(pool, c_w2, lambda: nc.gpsimd.dma_start(out=Wbr[:, 0:2, :], in_=w_a_d.rearrange("(kc p) n -> p kc n", p=128)), writes=[b_Wbr])
                dma(pool, c_w2, lambda: nc.gpsimd.dma_start(out=Wbr[:, 2:6, :], in_=w_b_d.rearrange("(kc p) n -> p kc n", p=128)), writes=[b_Wbr])
                dma(pool, c_w2, lambda: nc.gpsimd.dma_start(out=Wbr[:, 6:10, :], in_=w_c_d.rearrange("(kc p) n -> p kc n", p=128)), writes=[b_Wbr])
                dma(sp, c_w2, lambda: nc.sync.dma_start(out=bg[:], in_=bgate_d[:, :]), writes=[b_bg])
                yT_v = yT_s.rearrange("(c p) t -> p c t", p=128)
                mT_v = mT_s.rearrange("(c p) t -> p c t", p=128)
                brk = ((0, 2), (2, 6), (6, 10))

                def load_y(tc):
                    dma(sp, c_ych[tc % 2], lambda: nc.sync.dma_start(out=Ych[tc % 2][:], in_=yT_v[:, :, tc * 512:(tc + 1) * 512]),
                        writes=[b_Ych[tc % 2]])

                if upto not in ("p0", "p1"):
                    load_y(0)
                for tc in (range(8) if upto not in ("p0", "p1") else ()):
                    s = tc % 2
                    if tc + 1 < 8:
                        load_y(tc + 1)
                    tsl = slice(tc * 512, (tc + 1) * 512)
                    for f in range(8):
                        for br in range(3):
                            gb = br
                            for kc in range(8):
                                pe.do(lambda kc=kc, br=br, f=f, gb=gb: nc.tensor.matmul(
                                    bank(gb), lhsT=Wg[:, kc, br * 1024 + f * 128:br * 1024 + (f + 1) * 128], rhs=XT[:, kc, tsl],
                                    start=(kc == 0), stop=(kc == 7)), reads=[b_Wg, b_XTc[tc // 2]], writes=[b_ps[gb]])
                            act.do(lambda br=br, f=f, gb=gb: nc.scalar.activation(out=gt[br][:], in_=bank(gb), func=AF.Sigmoid,
                                                                                   bias=bg[:, br * 8 + f:br * 8 + f + 1]),
                                   reads=[b_ps[gb], b_bg], writes=[b_gt[br]])
                            k0, k1 = brk[br]
                            for kc in range(k0, k1):
                                pe.do(lambda kc=kc, br=br, f=f, k0=k0, k1=k1: nc.tensor.matmul(
                                    bank(3 + br), lhsT=Wbr[:, kc, f * 128:(f + 1) * 128], rhs=Ych[s][:, kc, :],
                                    start=(kc == k0), stop=(kc == k1 - 1)), reads=[b_Wbr, b_Ych[s]], writes=[b_ps[3 + br]])
                            dve.do(lambda br=br: nc.vector.tensor_tensor(out=tt_[br][:], in0=bank(3 + br), in1=gt[br][:], op=ALU.mult),
                                   reads=[b_ps[3 + br], b_gt[br]], writes=[b_tt[br]])
                        pool.do(lambda: nc.gpsimd.tensor_tensor(out=tt_[0][:], in0=tt_[0][:], in1=tt_[1][:], op=ALU.add),
                                reads=[b_tt[1]], writes=[b_tt[0]])
                        pool.do(lambda f=f: nc.gpsimd.tensor_tensor(out=mch[s][:, f, :], in0=tt_[0][:], in1=tt_[2][:], op=ALU.add),
                                reads=[b_tt[0], b_tt[2]], writes=[b_mch[s]])
                    dma(sp, c_mst[s], lambda s=s: nc.sync.dma_start(out=mT_v[:, :, tsl], in_=mch[s][:]), reads=[b_mch[s]])
                barrier()
        barrier()

        with ExitStack() as st:
            sb = lambda n, s_, t: st.enter_context(nc.sbuf_tensor("s_" + n, s_, t))
            Wout = sb("Wout", [128, 8, D], BF16)
            Wq = sb("Wq", [128, 8, D], BF16)
            skT = sb("skT", [128, 8, 128], F32)
            lnp = sb("lnp", [128, 4, D], F32)
            iota16 = sb("iota16", [128, 16], F32)
            mTc = [sb("mTc%d" % i, [128, 8, 512], BF16) for i in range(2)]
            xt = [sb("xt%d" % i, [128, D], F32) for i in range(2)]
            r1 = sb("r1", [128, D], F32)
            x1 = [sb("x1_%d" % i, [128, D], F32) for i in range(2)]
            x1T = sb("x1T", [128, 8, 128], BF16)
            qT = sb("qT", [128, 8, 128], F32)
            stats = sb("stats", [128, 2, 6], F32)
            mv = sb("mv", [128, 2], F32)
            rstd = sb("rstd", [128, 1], F32)
            nmr = sb("nmr", [128, 1], F32)
            sv = sb("sv", [128, 16, 16], F32)
            si_u = sb("si_u", [128, 16, 16], U32)
            si_f = sb("si_f", [128, 16, 16], F32)
            work = sb("work", [128, 128], F32)
            cand = sb("cand", [128, 8, 256], F32)
            work2 = sb("work2", [128, 256], F32)
            top = sb("top", [128, 8, 16], F32)
            pos_u = sb("pos_u", [128, 8, 16], U32)
            ab_u = sb("ab_u", [128, 2, 128], U32)
            ab_f = sb("ab_f", [128, 2, 128], F32)
            oh = sb("oh", [128, 8, 16, 16], BF16)
            ab_b = sb("ab_b", [128, 2, 128], BF16)
            si_b = sb("si_b", [128, 16, 16], BF16)
            iota_b = sb("iota_b", [128, 16], BF16)
            ij = sb("ij", [128, 2, 128], F32)
            eidx_f = sb("eidx_f", [128, 128], F32)
            eidx_u = [sb("eidx_u%d" % i, [128, 128], U32) for i in range(2)]
            gate = [sb("gate%d" % i, [128, 8, 16], F32) for i in range(2)]
            gsum = sb("gsum", [128, 8], F32)
            hpre = bank(4)[:, 0:128]
            gl = sb("gl", [128, 128], F32)
            x1b = [sb("x1b%d" % i, [128, D], BF16) for i in range(2)]
            prod = [sb("prod%d" % i, [128, D], BF16) for i in range(NP)]
            junk = sb("junk", [128, D], BF16)
            G = [sb("G%d" % i, [128, 2 * D], BF16) for i in range(NG)]
            diag = [sb("diag%d" % i, [128, 128], BF16) for i in range(ND)]
            ident_b = sb("ident_b", [128, 128], BF16)
            alphaI = sb("alphaI", [128, 128], F32)
            r2 = sb("r2", [128, D], F32)
            ot = [sb("ot%d" % i, [128, D], F32) for i in range(2)]
            b_w3 = Buf()
            b_mTc, b_xt, b_ot, b_x1, b_eu, b_gate = ([Buf(), Buf()] for _ in range(6))
            b_r1, b_x1T, b_qT, b_st, b_mv, b_rstd, b_nmr = (Buf() for _ in range(7))
            b_sv, b_siu, b_sif, b_work, b_cand, b_work2, b_top, b_pos = (Buf() for _ in range(8))
            b_abu, b_abf, b_oh, b_ij, b_ef, b_gsum, b_r2, b_idb = (Buf() for _ in range(8))
            b_G = [Buf() for _ in range(NG)]
            b_diag = [Buf() for _ in range(ND)]
            b_hp = [Buf() for _ in range(128)]
            b_gl = [Buf() for _ in range(128)]
            b_x1b = [Buf(), Buf()]
            b_prod = [Buf() for _ in range(NP)]
            b_ps = [Buf() for _ in range(8)]
            c_w3 = chan("w3")
            c_mtc, c_xt_, c_ot = [chan("mtc0"), chan("mtc1")], [chan("xt0"), chan("xt1")], [chan("ot0"), chan("ot1")]
            c_g = [chan("g%d" % i) for i in range(NG)]
            c_dbg = chan("dbg")
            dve.do(lambda: nc.vector.tensor_copy(out=ident_b[:], in_=ident[:]), reads=[b_const], writes=[b_idb])
            dve.do(lambda: nc.vector.tensor_scalar(out=alphaI[:], in0=ident[:], scalar1=float(ALPHA), scalar2=None, op0=ALU.mult), reads=[b_const], writes=[b_idb])
            dma(pool, c_w3, lambda: nc.gpsimd.dma_start(out=Wout[:], in_=w_out_d.rearrange("(kc p) n -> p kc n", p=128)), writes=[b_w3])
            dma(pool, c_w3, lambda: nc.gpsimd.dma_start(out=Wq[:], in_=w_q_d.rearrange("(kc p) n -> p kc n", p=128)), writes=[b_w3])
            dma(sp, c_w3, lambda: nc.sync.dma_start(out=skT[:].rearrange("p a b -> p (a b)"), in_=skT_d[:, :]), writes=[b_w3])
            dma(sp, c_w3, lambda: nc.sync.dma_start(out=lnp[:].rearrange("p a b -> p (a b)"), in_=lnp_d[:, :]), writes=[b_w3])
            dma(sp, c_w3, lambda: nc.sync.dma_start(out=iota16[:], in_=iota_d[:, :]), writes=[b_w3])
            dve.do(lambda: nc.vector.tensor_copy(out=iota_b[:], in_=iota16[:]), reads=[b_w3], writes=[b_w3])
            mT_v = mT_s.rearrange("(c p) t -> p c t", p=128)
            accp = bank(6, 2)
            NTILES = S // 128 if ntiles is None else ntiles
            if upto != "all":
                NTILES = 0

            def layer_norm_steps(L, src, b_src, dst, b_dst, gi):
                rs = b_src if isinstance(b_src, list) else [b_src]

                def s_stats():
                    for hf in range(2):
                        dve.do(lambda hf=hf: nc.vector.bn_stats(out=stats[:, hf, :], in_=src[:, hf * 512:(hf + 1) * 512]), reads=rs, writes=[b_st])
                    dve.do(lambda: nc.vector.bn_aggr(out=mv[:], in_=stats[:].rearrange("p a b -> p (a b)")), reads=[b_st], writes=[b_mv])
                    act.do(lambda: nc.scalar.activation(out=rstd[:], in_=mv[:, 1:2], func=AF.Sqrt, bias=float(LN_EPS), scale=1.0), reads=[b_mv], writes=[b_rstd])
                def s_norm():
                    dve.do(lambda: nc.vector.reciprocal(out=rstd[:], in_=rstd[:]), reads=[], writes=[b_rstd])
                    dve.do(lambda: nc.vector.scalar_tensor_tensor(out=nmr[:], in0=mv[:, 0:1], scalar=-1.0, in1=rstd[:], op0=ALU.mult, op1=ALU.mult),
                           reads=[b_mv, b_rstd], writes=[b_nmr])
                    act.do(lambda: nc.scalar.activation(out=dst[:], in_=src[:, 0:D], func=AF.Identity, bias=nmr[:, 0:1], scale=rstd[:, 0:1]),
                           reads=rs + [b_rstd, b_nmr], writes=[b_dst])
                def s_g():
                    dve.do(lambda: nc.vector.tensor_tensor(out=dst[:], in0=dst[:], in1=lnp[:, gi, :], op=ALU.mult), reads=[b_w3], writes=[b_dst])
                def s_b():
                    dve.do(lambda: nc.vector.tensor_tensor(out=dst[:], in0=dst[:], in1=lnp[:, gi + 1, :], op=ALU.add), reads=[b_w3], writes=[b_dst])
                L.extend([s_stats, s_norm, s_g, s_b])

            def load_tile(tt):
                if tt >= NTILES:
                    return
                if tt % 4 == 0:
                    cc = (tt // 4) % 2
                    dma(sp, c_mtc[cc], lambda: nc.sync.dma_start(out=mTc[cc][:], in_=mT_v[:, :, (tt // 4) * 512:(tt // 4 + 1) * 512]),
                        writes=[b_mTc[cc]])
                dma(sp, c_xt_[tt % 2], lambda: nc.sync.dma_start(out=xt[tt % 2][:], in_=x_d[tt * 128:(tt + 1) * 128, :]), writes=[b_xt[tt % 2]])

            def topk16(L, src_fn, b_src, vals, b_vals, idxs, b_idxs, wk, b_wk):
                def f():
                    src = src_fn()
                    dve.do(lambda: nc.vector.max(out=vals[:, 0:8], in_=src), reads=[b_src], writes=[b_vals])
                    dve.do(lambda: nc.vector.max_index(out=idxs[:, 0:8], in_max=vals[:, 0:8], in_values=src), reads=[b_src, b_vals], writes=[b_idxs])
                    dve.do(lambda: nc.vector.match_replace(out=wk, in_to_replace=vals[:, 0:8], in_values=src, imm_value=-1e30),
                           reads=[b_src, b_vals], writes=[b_wk])
                    dve.do(lambda: nc.vector.max(out=vals[:, 8:16], in_=wk), reads=[b_wk], writes=[b_vals])
                    dve.do(lambda: nc.vector.max_index(out=idxs[:, 8:16], in_max=vals[:, 8:16], in_values=wk), reads=[b_wk, b_vals], writes=[b_idxs])
                L.append(f)

            def front_steps(tt):
                L = []
                if tt >= NTILES:
                    return L
                par = tt % 2
                cc = (tt // 4) % 2
                sub = tt % 4
                xs = tt % 2
                X1 = x1[par]
                bX1 = b_x1[par]
                for hf in range(2):
                    for f0 in (0, 4):
                        def s_wout(hf=hf, f0=f0):
                            for f in range(f0, f0 + 4):
                                pe.do(lambda f=f: nc.tensor.matmul(bank(hf), lhsT=mTc[cc][:, f, sub * 128:(sub + 1) * 128],
                                                                  rhs=Wout[:, f, hf * 512:(hf + 1) * 512], start=(f == 0), stop=False),
                                      reads=[b_mTc[cc], b_w3], writes=[b_ps[hf]])
                            if f0 == 4:
                                pe.do(lambda: nc.tensor.matmul(bank(hf), lhsT=alphaI[:], rhs=xt[xs][:, hf * 512:(hf + 1) * 512], start=False, stop=True),
                                      reads=[b_xt[xs], b_idb], writes=[b_ps[hf]])
                        L.append(s_wout)
                layer_norm_steps(L, bank(0, 2), [b_ps[0], b_ps[1]], X1, bX1, 0)
                if debug:
                    L.append(lambda: dma(sp, c_dbg, lambda: nc.sync.dma_start(out=x1_dbg[tt * 128:(tt + 1) * 128, :], in_=X1[:]), reads=[bX1]))
                L.append(lambda: act.do(lambda: nc.scalar.activation(out=x1b[par][:], in_=X1[:], func=AF.Identity), reads=[bX1], writes=[b_x1b[par]]))
                for f0 in (0, 4):
                    def s_tr(f0=f0):
                        for f in range(f0, f0 + 4):
                            pe.do(lambda f=f: nc.tensor.transpose(out=bank(2, 2)[:, f * 128:(f + 1) * 128], in_=X1[:, f * 128:(f + 1) * 128], identity=ident[:]),
                                  reads=[bX1, b_const], writes=[b_ps[2 + f // 4]])
                    L.append(s_tr)
                L.append(lambda: act.do(lambda: nc.scalar.activation(out=x1T[:].rearrange("p a b -> p (a b)"), in_=bank(2, 2), func=AF.Identity),
                                        reads=[b_ps[2], b_ps[3]], writes=[b_x1T]))
                for h in range(8):
                    def s_q(h=h):
                        for kc in range(8):
                            pe.do(lambda kc=kc: nc.tensor.matmul(bank(0, 2)[:, h * 128:(h + 1) * 128], lhsT=Wq[:, kc, h * 128:(h + 1) * 128],
                                                                rhs=x1T[:, kc, :], start=(kc == 0), stop=(kc == 7)),
                                  reads=[b_w3, b_x1T], writes=[b_ps[h // 4]])
                    L.append(s_q)
                L.append(lambda: act.do(lambda: nc.scalar.activation(out=qT[:].rearrange("p a b -> p (a b)"), in_=bank(0, 2), func=AF.Identity),
                                        reads=[b_ps[0], b_ps[1]], writes=[b_qT]))
                for rnd, b0 in ((0, 2), (1, 0)):
                    def s_sc(rnd=rnd, b0=b0):
                        for hh in range(4):
                            h = rnd * 4 + hh
                            for c in range(2):
                                pr = slice(c * 64, (c + 1) * 64)
                                pe.do(lambda h=h, hh=hh, c=c, pr=pr: nc.tensor.matmul(bank(b0 + c)[:, hh * 128:(hh + 1) * 128], lhsT=qT[pr, h, :],
                                                                                    rhs=skT[pr, h, :], start=True, stop=True),
                                      reads=[b_qT, b_w3], writes=[b_ps[b0 + c]])
                    L.append(s_sc)
                    for hh in range(4):
                        for c in range(2):
                            hc = c * 8 + rnd * 4 + hh
                            topk16(L, (lambda b0=b0, c=c, hh=hh: bank(b0 + c)[:, hh * 128:(hh + 1) * 128]), b_ps[b0 + c],
                                   sv[:, hc, :], b_sv, si_u[:, hc, :], b_siu, work[:], b_work)
                sv4 = sv[:].rearrange("p (c h) k -> p c h k", c=2)
                si4 = si_b[:].rearrange("p (c h) k -> p c h k", c=2)

                def s_cand():
                    dve.do(lambda: nc.vector.tensor_copy(out=si_b[:], in_=si_u[:]), reads=[b_siu], writes=[b_sif])
                    dve.do(lambda: nc.vector.tensor_tensor(out=cand[:].rearrange("p h (a b) -> p h a b", b=16),
                                                           in0=sv4[:, 0, :, :].unsqueeze(3).broadcast_to([128, 8, 16, 16]),
                                                           in1=sv4[:, 1, :, :].unsqueeze(2).broadcast_to([128, 8, 16, 16]), op=ALU.add),
                           reads=[b_sv], writes=[b_cand])
                L.append(s_cand)
                for h in range(8):
                    topk16(L, (lambda h=h: cand[:, h, :]), b_cand, top[:, h, :], b_top, pos_u[:, h, :], b_pos, work2[:], b_work2)
                posf = pos_u[:].rearrange("p h k -> p (h k)")

                def s_ab():
                    dve.do(lambda: nc.vector.tensor_single_scalar(out=ab_u[:, 0, :], in_=posf, scalar=4, op=ALU.logical_shift_right), reads=[b_pos], writes=[b_abu])
                    dve.do(lambda: nc.vector.tensor_single_scalar(out=ab_u[:, 1, :], in_=posf, scalar=15, op=ALU.bitwise_and), reads=[b_pos], writes=[b_abu])
                    dve.do(lambda: nc.vector.tensor_copy(out=ab_b[:], in_=ab_u[:]), reads=[b_abu], writes=[b_abf])
                L.append(s_ab)
                for c in range(2):
                    def s_lk(c=c):
                        abv = ab_b[:, c, :].rearrange("p (h k) -> p h k", k=16)
                        dve.do(lambda: nc.vector.tensor_tensor(out=oh[:], in0=abv.unsqueeze(3).broadcast_to([128, 8, 16, 16]),
                                                               in1=iota_b[:].unsqueeze(1).unsqueeze(1).broadcast_to([128, 8, 16, 16]), op=ALU.is_equal),
                               reads=[b_abf, b_w3], writes=[b_oh])
                        dve.do(lambda: nc.vector.tensor_tensor(out=oh[:], in0=oh[:], in1=si4[:, c, :, :].unsqueeze(2).broadcast_to([128, 8, 16, 16]), op=ALU.mult),
                               reads=[b_sif], writes=[b_oh])
                        dve.do(lambda: nc.vector.tensor_reduce(out=ij[:, c, :], in_=oh[:].rearrange("p h k a -> p (h k) a"), axis=AX.X, op=ALU.add),
                               reads=[b_oh], writes=[b_ij])
                    L.append(s_lk)

                def s_eidx():
                    dve.do(lambda: nc.vector.scalar_tensor_tensor(out=eidx_f[:], in0=ij[:, 0, :], scalar=128.0, in1=ij[:, 1, :], op0=ALU.mult, op1=ALU.add),
                           reads=[b_ij], writes=[b_ef])
                    dve.do(lambda: nc.vector.tensor_copy(out=eidx_u[par][:], in_=eidx_f[:]), reads=[b_ef], writes=[b_eu[par]])
                L.append(s_eidx)
                GT = gate[par]
                bGT = b_gate[par]

                def s_gate1():
                    dve.do(lambda: nc.vector.tensor_tensor(out=GT[:], in0=top[:], in1=top[:, :, 0:1].broadcast_to([128, 8, 16]), op=ALU.subtract),
                           reads=[b_top], writes=[bGT])
                    act.do(lambda: nc.scalar.activation(out=GT[:], in_=GT[:], func=AF.Exp), reads=[], writes=[bGT])
                def s_gate2():
                    dve.do(lambda: nc.vector.tensor_reduce(out=gsum[:], in_=GT[:], axis=AX.X, op=ALU.add), reads=[bGT], writes=[b_gsum])
                    dve.do(lambda: nc.vector.reciprocal(out=gsum[:], in_=gsum[:]), reads=[], writes=[b_gsum])
                    dve.do(lambda: nc.vector.tensor_tensor(out=GT[:], in0=GT[:], in1=gsum[:].unsqueeze(2).broadcast_to([128, 8, 16]), op=ALU.mult),
                           reads=[b_gsum], writes=[bGT])
                L.extend([s_gate1, s_gate2])
                return L

            def slot_head(tt, s_):
                par = tt % 2
                g = s_ % NG
                pb = s_ % NP
                dma(pool, c_g[g], lambda: nc.gpsimd.indirect_dma_start(
                    out=G[g][:], out_offset=None, in_=uv_s[:, :], in_offset=bass.IndirectOffsetOnAxis(ap=eidx_u[par][:, s_:s_ + 1], axis=0)),
                    reads=[b_eu[par]], writes=[b_G[g]])
                dve.do(lambda: nc.vector.tensor_tensor(out=prod[pb][:], in0=G[g][:, 0:D], in1=x1b[par][:], op=ALU.mult),
                       reads=[b_G[g], b_x1b[par]], writes=[b_prod[pb]])
                act.do(lambda: nc.scalar.activation(out=junk[:], in_=prod[pb][:], func=AF.Identity, accum_out=hpre[:, s_:s_ + 1]),
                       reads=[b_prod[pb]], writes=[b_hp[s_]])
                act.do(lambda: nc.scalar.activation(out=gl[:, s_:s_ + 1], in_=hpre[:, s_:s_ + 1], func=AF.Gelu), reads=[b_hp[s_]], writes=[b_gl[s_]])

            def slot_tail(tt, s_):
                par = tt % 2
                g = s_ % NG
                d_ = s_ % ND
                gatef = gate[par][:].rearrange("p h k -> p (h k)")
                dve.do(lambda: nc.vector.tensor_scalar(out=diag[d_][:], in0=ident_b[:], scalar1=gl[:, s_:s_ + 1], scalar2=gatef[:, s_:s_ + 1],
                                                       op0=ALU.mult, op1=ALU.mult),
                       reads=[b_gl[s_], b_gate[par], b_idb], writes=[b_diag[d_]])
                for hf in range(2):
                    pe.do(lambda hf=hf: nc.tensor.matmul(bank(6 + hf), lhsT=diag[d_][:], rhs=G[g][:, D + hf * 512:D + (hf + 1) * 512],
                                                        start=(s_ == 0), stop=(s_ == 127)),
                          reads=[b_diag[d_], b_G[g]], writes=[b_ps[6 + hf]])

            def tail(tt):
                par = tt % 2
                os_ = tt % 2
                if debug:
                    dve.do(lambda: nc.vector.tensor_copy(out=r2[:], in_=accp), reads=[b_ps[6], b_ps[7]], writes=[b_r2])
                    dma(sp, c_dbg, lambda: nc.sync.dma_start(out=yp_dbg[tt * 128:(tt + 1) * 128, :], in_=r2[:]), reads=[b_r2])
                dve.do(lambda: nc.vector.scalar_tensor_tensor(out=r2[:], in0=x1[par][:], scalar=float(ALPHA), in1=accp, op0=ALU.mult, op1=ALU.add),
                       reads=[b_x1[par], b_ps[6], b_ps[7]], writes=[b_r2])
                L = []
                layer_norm_steps(L, r2, b_r2, ot[os_], b_ot[os_], 2)
                for f in L:
                    f()
                dma(sp, c_ot[os_], lambda: nc.sync.dma_start(out=out_d[tt * 128:(tt + 1) * 128, :], in_=ot[os_][:]), reads=[b_ot[os_]])

            load_tile(0)
            load_tile(1)
            for f in front_steps(0):
                f()
            for tt in range(NTILES):
                load_tile(tt + 2)
                nxt = front_steps(tt + 1)
                k = 0
                for s_ in range(128 + LAG):
                    if s_ < 128:
                        slot_head(tt, s_)
                    if s_ >= LAG:
                        slot_tail(tt, s_ - LAG)
                    if s_ < 128:
                        tgt = ((s_ + 1) * len(nxt)) // 128
                        while k < tgt:
                            nxt[k]()
                            k += 1
                tail(tt)
            barrier()
    return nc


_NC_CACHE = {}


def _host_inputs(x, mem, rel_bias, w_in, b_gate, w_mem_kv, sinks, w_branch_a, w_branch_b, w_branch_c, w_out, ln1_g, ln1_b,
                 peer_w_query, peer_sub_keys, peer_u, peer_v, ln2_g, ln2_b):
    f = lambda a: np.ascontiguousarray(np.asarray(a, dtype=np.float32))
    pairs, bucket, mask, heads = _bias_tables()
    rb = np.asarray(rel_bias, np.float32)
    biasT = np.zeros((128, 10, 4, 128), np.float32)
    maskT = np.zeros((128, 10, 4, 128), np.float32)
    for p in range(10):
        for hh in range(2):
            for kb in range(2):
                biasT[:, p, 2 * hh + kb, :] = rb[bucket[p, kb], heads[p] + hh]
                maskT[:, p, 2 * hh + kb, :] = mask[p, kb]
    sk = np.asarray(sinks, np.float32)[0]
    sinksP = np.zeros((128, 4), np.float32)
    for p in range(4):
        sinksP[0:64, p] = sk[2 * p]
        sinksP[64:128, p] = sk[2 * p + 1]
    lnp = np.stack([np.asarray(a, np.float32)[0] for a in (ln1_g, ln1_b, ln2_g, ln2_b)], 0)
    lnp = np.ascontiguousarray(np.broadcast_to(lnp.reshape(1, 4 * D), (128, 4 * D)))
    skT = np.asarray(peer_sub_keys, np.float32)[0].transpose(1, 3, 0, 2).reshape(128, 8 * 128)
    shared = {
        "w_in": f(w_in[0]), "w_mem_kv": f(w_mem_kv[0]), "w_a": f(w_branch_a[0]), "w_b": f(w_branch_b[0]), "w_c": f(w_branch_c[0]),
        "w_out": f(w_out[0]), "w_q": f(peer_w_query[0]), "skT": f(skT), "peer_u": f(peer_u[0]), "peer_v": f(peer_v[0]),
        "biasT": f(biasT.reshape(128, 5120)), "maskT": f(maskT.reshape(128, 5120)),
        "bgate": f(np.asarray(b_gate, np.float32)[0].reshape(24, 128).T), "sinksP": f(sinksP), "lnp": f(lnp),
        "ident": np.eye(128, dtype=np.float32), "iota16": f(np.broadcast_to(np.arange(16, dtype=np.float32), (128, 16))),
    }
    x = np.asarray(x, np.float32)
    mem = np.asarray(mem, np.float32)
    in_maps = []
    for b in range(x.shape[0]):
        m = dict(shared)
        m["x"] = f(x[b])
        m["xT"] = f(x[b].T)
        m["memT"] = f(mem[b].T)
        in_maps.append(m)
    return in_maps


def kernel(**inputs):
    in_maps = _host_inputs(**inputs)
    if "nc" not in _NC_CACHE:
        _NC_CACHE["nc"] = build_nc()
    nc = _NC_CACHE["nc"]
    res = run_bass_kernel_spmd(nc, in_maps, core_ids=list(range(NCORES)))
    return np.stack([np.asarray(r["out"], dtype=np.float32) for r in res.results], 0)
```
